# Optimizing a Trainium2 kernel written in Bass

```python
import math
import jax, jax.numpy as jnp
from jax import lax
import numpy as np

D_MODEL = 1024
BATCH = 8
SEQ = 2048
DEPTH = 2
DEC_BATCH = 128
DEC_SEQ = 4
PAST_LEN = 16384
PAGE_SIZE = 128

N_EVEN = (DEPTH + 1) // 2
N_ODD = DEPTH // 2
EPS = 1e-6
CHUNK = 128
A_GROUPS = 8
A_DIM = D_MODEL
A_HEAD = A_DIM // A_GROUPS
SSM_HEAD_DIM = 64
SSM_DIM = D_MODEL
SSM_HEADS = SSM_DIM // SSM_HEAD_DIM
SSM_GROUPS = 2
SSM_HPG = SSM_HEADS // SSM_GROUPS
SSM_STATE = 128
SSM_CONV = 4
SSM_CONV_DIM = SSM_DIM + 2 * SSM_GROUPS * SSM_STATE
PROJ_AB = 2 * A_DIM + SSM_DIM + SSM_CONV_DIM + SSM_HEADS
SC_DIM = D_MODEL
SC_CONV = 3
N_MEM = 256
MEM_HEADS = 4
MEM_HEAD_DIM = D_MODEL // MEM_HEADS
D_FF = 2816
N_EXPERTS = 8
TOP_K = 2
D_EXP = 2816

kernel_name = 'hybrid_sgmlp_ssd_shortconv_step'


def rmsnorm(x, g):
    xf = x.astype(jnp.float32)
    y = xf * lax.rsqrt(jnp.mean(xf * xf, axis=-1, keepdims=True) + EPS)
    return (y * g.astype(jnp.float32)).astype(x.dtype)


def causal_dwconv(x, buf, w):
    K = w.shape[0]
    L = x.shape[1]
    xp = jnp.concatenate([buf.astype(x.dtype), x], axis=1)
    y = xp[:, 0:L] * w[0]
    for k in range(1, K):
        y = y + xp[:, k:k + L] * w[k]
    return y, xp[:, xp.shape[1] - (K - 1):]


def spatial_gate(u, v, w_s, b_s, T):
    bsz, L, G, d = v.shape
    vc = v.reshape(bsz, L // T, T, G, d)
    mask = jnp.tril(jnp.ones((T, T), bool))
    w = jnp.where(mask[None], w_s[:, :T, :T], 0)
    s = jnp.einsum('gts,bcsgd->bctgd', w, vc) + b_s[:, :T].T[None, None, :, :, None]
    return u * s.reshape(bsz, L, G, d)


def ssd_chunk_scan(x, dt, a, bm, cm, h0, Q):
    bsz, L = x.shape[0], x.shape[1]
    nc = L // Q

    def to_chunks(t):
        return jnp.moveaxis(t.reshape((bsz, nc, Q) + t.shape[2:]), 1, 0)

    causal = jnp.tril(jnp.ones((Q, Q), bool))[None, :, :, None, None]

    def step(h, inp):
        xc, dtc, bc, cc = inp
        la = jnp.cumsum(dtc * a, axis=1)
        seg = la[:, :, None] - la[:, None, :]
        decay = jnp.exp(jnp.where(causal, seg, -jnp.inf))
        cb = jnp.einsum('btgn,bsgn->btsg', cc, bc).astype(jnp.float32)
        wts = cb[..., None] * decay * dtc[:, None]
        y = jnp.einsum('btsgh,bsghp->btghp', wts, xc.astype(jnp.float32))
        y = y + jnp.einsum('btgn,bghpn->btghp', cc.astype(jnp.float32), h) * jnp.exp(la)[..., None]
        last = la[:, -1]
        wend = jnp.exp(last[:, None] - la) * dtc
        h_new = h * jnp.exp(last)[..., None, None] + jnp.einsum('bsgh,bsgn,bsghp->bghpn', wend, bc.astype(jnp.float32), xc.astype(jnp.float32))
        return h_new, y

    h, ys = lax.scan(step, h0.astype(jnp.float32), (to_chunks(x), to_chunks(dt), to_chunks(bm), to_chunks(cm)))
    y = jnp.moveaxis(ys, 0, 1).reshape(x.shape)
    return y, h.astype(h0.dtype)


def mixer_ab(h, conv_buf, ssm_h, w_in, w_s, b_s, conv_w, conv_b, dt_bias, a_log, d_skip, g_norm, w_out, T):
    bsz, L, _ = h.shape
    proj = h @ w_in
    cuts = [A_DIM, 2 * A_DIM, 2 * A_DIM + SSM_DIM, 2 * A_DIM + SSM_DIM + SSM_CONV_DIM]
    u, v, z, xbc, dt_raw = jnp.split(proj, cuts, axis=-1)
    u = jax.nn.gelu(u)
    v = jax.nn.gelu(v)
    y_a = spatial_gate(u.reshape(bsz, L, A_GROUPS, A_HEAD), v.reshape(bsz, L, A_GROUPS, A_HEAD), w_s, b_s, T).reshape(bsz, L, A_DIM)
    xbc, conv_buf_new = causal_dwconv(xbc, conv_buf, conv_w)
    xbc = jax.nn.silu(xbc + conv_b)
    xs, bm, cm = jnp.split(xbc, [SSM_DIM, SSM_DIM + SSM_GROUPS * SSM_STATE], axis=-1)
    xs = xs.reshape(bsz, L, SSM_GROUPS, SSM_HPG, SSM_HEAD_DIM)
    bm = bm.reshape(bsz, L, SSM_GROUPS, SSM_STATE)
    cm = cm.reshape(bsz, L, SSM_GROUPS, SSM_STATE)
    dt = jax.nn.softplus(dt_raw.astype(jnp.float32) + dt_bias.astype(jnp.float32)).reshape(bsz, L, SSM_GROUPS, SSM_HPG)
    a = -jnp.exp(a_log.astype(jnp.float32)).reshape(SSM_GROUPS, SSM_HPG)
    h0 = ssm_h.reshape(bsz, SSM_GROUPS, SSM_HPG, SSM_HEAD_DIM, SSM_STATE)
    ys, h_new = ssd_chunk_scan(xs, dt, a, bm, cm, h0, T)
    ys = (ys + xs.astype(jnp.float32) * d_skip.astype(jnp.float32).reshape(SSM_GROUPS, SSM_HPG)[..., None]).astype(h.dtype)
    gated = (ys.reshape(bsz, L, SSM_DIM) * jax.nn.silu(z)).reshape(bsz, L, SSM_GROUPS, SSM_DIM // SSM_GROUPS)
    y_b = rmsnorm(gated, g_norm.reshape(SSM_GROUPS, SSM_DIM // SSM_GROUPS)).reshape(bsz, L, SSM_DIM)
    out = jnp.concatenate([y_a, y_b], axis=-1) @ w_out
    return out, conv_buf_new, h_new.reshape(bsz, SSM_HEADS, SSM_HEAD_DIM, SSM_STATE), v


def mixer_c(h, buf, w_in, conv_w, w_out):
    bg, cg, hx = jnp.split(h @ w_in, 3, axis=-1)
    y, buf_new = causal_dwconv(cg * hx, buf, conv_w)
    return (bg * y) @ w_out, buf_new


def mem_attend(h, mk, mv, wq, wo):
    bsz, L, _ = h.shape
    q = (h @ wq).reshape(bsz, L, MEM_HEADS, MEM_HEAD_DIM)
    s = jnp.einsum('blhe,bmhe->bhlm', q, mk).astype(jnp.float32) * (MEM_HEAD_DIM ** -0.5)
    pr = jax.nn.softmax(s, axis=-1).astype(h.dtype)
    o = jnp.einsum('bhlm,bmhe->blhe', pr, mv).reshape(bsz, L, D_MODEL)
    return o @ wo


def swiglu(h, wg, wu, wd):
    return (jax.nn.silu(h @ wg) * (h @ wu)) @ wd


def moe_swiglu(h, w_router, wg, wu, wd):
    logits = (h @ w_router).astype(jnp.float32)
    top_v, top_i = lax.top_k(logits, TOP_K)
    gates = jax.nn.softmax(top_v, axis=-1)
    combine = jnp.sum(jax.nn.one_hot(top_i, N_EXPERTS, dtype=jnp.float32) * gates[..., None], axis=-2).astype(h.dtype)
    out = jnp.zeros_like(h)
    for e in range(N_EXPERTS):
        out = out + combine[..., e:e + 1] * swiglu(h, wg[e], wu[e], wd[e])
    return out


def trunk(x, mem_k, mem_v, ssm_h, ssm_conv, sc_conv, T, p):
    ssm_out, ssm_conv_out, sc_out, v_out = [], [], [], []
    for l in range(DEPTH):
        i = l // 2
        h = rmsnorm(x, p['g_mix'][l])
        if l % 2 == 0:
            mix, cbuf, hs, v = mixer_ab(h, ssm_conv[i], ssm_h[i], p['w_in_ab'][i], p['w_spatial'][i], p['b_spatial'][i],
                                        p['conv_w_ssm'][i], p['conv_b_ssm'][i], p['dt_bias'][i], p['a_log'][i],
                                        p['d_skip'][i], p['g_ssm_norm'][i], p['w_out_ab'][i], T)
            ssm_out.append(hs)
            ssm_conv_out.append(cbuf)
            v_out.append(v)
        else:
            mix, sbuf = mixer_c(h, sc_conv[i], p['w_in_c'][i], p['conv_w_c'][i], p['w_out_c'][i])
            sc_out.append(sbuf)
        x = x + mix
        x = x + mem_attend(rmsnorm(x, p['g_mem'][l]), mem_k[l], mem_v[l], p['w_mem_q'][l], p['w_mem_o'][l])
        hf = rmsnorm(x, p['g_ffn'][l])
        if l % 2 == 0:
            x = x + swiglu(hf, p['w_ffn_gate'][i], p['w_ffn_up'][i], p['w_ffn_down'][i])
        else:
            x = x + moe_swiglu(hf, p['w_router'][i], p['w_exp_gate'][i], p['w_exp_up'][i], p['w_exp_down'][i])
    return rmsnorm(x, p['g_final']), jnp.stack(ssm_out), jnp.stack(ssm_conv_out), jnp.stack(sc_out), jnp.stack(v_out)


def setup_inputs(seed: int = 0) -> dict:
    key = jax.random.key(seed)
    ks = iter(jax.random.split(key, 48))

    def nrm(shape, scale):
        return jax.random.normal(next(ks), shape, jnp.float32) * scale

    def gain(shape):
        return 1.0 + nrm(shape, 0.02)

    dt0 = jnp.exp(jax.random.uniform(next(ks), (N_EVEN, SSM_HEADS), jnp.float32, math.log(1e-3), math.log(1e-1)))
    return {
        'x_prompt': nrm((BATCH, SEQ, D_MODEL), 1.0),
        'x_sample': nrm((DEC_BATCH, DEC_SEQ, D_MODEL), 1.0),
        'mem_prompt': nrm((BATCH, N_MEM, D_MODEL), 1.0),
        'state_ssm': nrm((N_EVEN, DEC_BATCH, SSM_HEADS, SSM_HEAD_DIM, SSM_STATE), 0.5),
        'state_ssm_conv': nrm((N_EVEN, DEC_BATCH, SSM_CONV - 1, SSM_CONV_DIM), 1.0),
        'state_sconv': nrm((N_ODD, DEC_BATCH, SC_CONV - 1, SC_DIM), 1.0),
        'cache_mem_k': nrm((DEPTH, DEC_BATCH, N_MEM, MEM_HEADS, MEM_HEAD_DIM), 1.0),
        'cache_mem_v': nrm((DEPTH, DEC_BATCH, N_MEM, MEM_HEADS, MEM_HEAD_DIM), 1.0),
        'g_mix': gain((DEPTH, D_MODEL)),
        'g_mem': gain((DEPTH, D_MODEL)),
        'g_ffn': gain((DEPTH, D_MODEL)),
        'g_final': gain((D_MODEL,)),
        'w_in_ab': nrm((N_EVEN, D_MODEL, PROJ_AB), D_MODEL ** -0.5),
        'w_spatial': nrm((N_EVEN, A_GROUPS, CHUNK, CHUNK), CHUNK ** -0.5),
        'b_spatial': 1.0 + nrm((N_EVEN, A_GROUPS, CHUNK), 0.02),
        'conv_w_ssm': nrm((N_EVEN, SSM_CONV, SSM_CONV_DIM), SSM_CONV ** -0.5),
        'conv_b_ssm': nrm((N_EVEN, SSM_CONV_DIM), 0.02),
        'dt_bias': dt0 + jnp.log(-jnp.expm1(-dt0)),
        'a_log': jnp.log(jax.random.uniform(next(ks), (N_EVEN, SSM_HEADS), jnp.float32, 1.0, 16.0)),
        'd_skip': 1.0 + nrm((N_EVEN, SSM_HEADS), 0.1),
        'g_ssm_norm': gain((N_EVEN, SSM_DIM)),
        'w_out_ab': nrm((N_EVEN, A_DIM + SSM_DIM, D_MODEL), (A_DIM + SSM_DIM) ** -0.5),
        'w_ffn_gate': nrm((N_EVEN, D_MODEL, D_FF), D_MODEL ** -0.5),
        'w_ffn_up': nrm((N_EVEN, D_MODEL, D_FF), D_MODEL ** -0.5),
        'w_ffn_down': nrm((N_EVEN, D_FF, D_MODEL), D_FF ** -0.5),
        'w_in_c': nrm((N_ODD, D_MODEL, 3 * SC_DIM), D_MODEL ** -0.5),
        'conv_w_c': nrm((N_ODD, SC_CONV, SC_DIM), SC_CONV ** -0.5),
        'w_out_c': nrm((N_ODD, SC_DIM, D_MODEL), SC_DIM ** -0.5),
        'w_router': nrm((N_ODD, D_MODEL, N_EXPERTS), D_MODEL ** -0.5),
        'w_exp_gate': nrm((N_ODD, N_EXPERTS, D_MODEL, D_EXP), D_MODEL ** -0.5),
        'w_exp_up': nrm((N_ODD, N_EXPERTS, D_MODEL, D_EXP), D_MODEL ** -0.5),
        'w_exp_down': nrm((N_ODD, N_EXPERTS, D_EXP, D_MODEL), D_EXP ** -0.5),
        'w_mem_q': nrm((DEPTH, D_MODEL, D_MODEL), D_MODEL ** -0.5),
        'w_mem_k': nrm((DEPTH, D_MODEL, D_MODEL), D_MODEL ** -0.5),
        'w_mem_v': nrm((DEPTH, D_MODEL, D_MODEL), D_MODEL ** -0.5),
        'w_mem_o': nrm((DEPTH, D_MODEL, D_MODEL), D_MODEL ** -0.5),
    }


def reference(x_prompt, x_sample, mem_prompt, state_ssm, state_ssm_conv, state_sconv, cache_mem_k, cache_mem_v,
              g_mix, g_mem, g_ffn, g_final, w_in_ab, w_spatial, b_spatial, conv_w_ssm, conv_b_ssm, dt_bias, a_log,
              d_skip, g_ssm_norm, w_out_ab, w_ffn_gate, w_ffn_up, w_ffn_down, w_in_c, conv_w_c, w_out_c, w_router,
              w_exp_gate, w_exp_up, w_exp_down, w_mem_q, w_mem_k, w_mem_v, w_mem_o):
    p = dict(g_mix=g_mix, g_mem=g_mem, g_ffn=g_ffn, g_final=g_final, w_in_ab=w_in_ab, w_spatial=w_spatial,
             b_spatial=b_spatial, conv_w_ssm=conv_w_ssm, conv_b_ssm=conv_b_ssm, dt_bias=dt_bias, a_log=a_log,
             d_skip=d_skip, g_ssm_norm=g_ssm_norm, w_out_ab=w_out_ab, w_ffn_gate=w_ffn_gate, w_ffn_up=w_ffn_up,
             w_ffn_down=w_ffn_down, w_in_c=w_in_c, conv_w_c=conv_w_c, w_out_c=w_out_c, w_router=w_router,
             w_exp_gate=w_exp_gate, w_exp_up=w_exp_up, w_exp_down=w_exp_down, w_mem_q=w_mem_q, w_mem_o=w_mem_o)
    bp = x_prompt.shape[0]
    dtype = x_prompt.dtype
    mem_k_p = jnp.einsum('bmd,lde->lbme', mem_prompt, w_mem_k).reshape(DEPTH, bp, N_MEM, MEM_HEADS, MEM_HEAD_DIM)
    mem_v_p = jnp.einsum('bmd,lde->lbme', mem_prompt, w_mem_v).reshape(DEPTH, bp, N_MEM, MEM_HEADS, MEM_HEAD_DIM)
    ssm0 = jnp.zeros((N_EVEN, bp, SSM_HEADS, SSM_HEAD_DIM, SSM_STATE), dtype)
    ssmconv0 = jnp.zeros((N_EVEN, bp, SSM_CONV - 1, SSM_CONV_DIM), dtype)
    sconv0 = jnp.zeros((N_ODD, bp, SC_CONV - 1, SC_DIM), dtype)
    y_prompt, ssm_p, ssmconv_p, sconv_p, _ = trunk(x_prompt, mem_k_p, mem_v_p, ssm0, ssmconv0, sconv0, CHUNK, p)
    y_sample, ssm_s, ssmconv_s, sconv_s, v_s = trunk(x_sample, cache_mem_k, cache_mem_v, state_ssm, state_ssm_conv,
                                                     state_sconv, x_sample.shape[1], p)
    return (y_prompt, y_sample, ssm_p, ssm_s, ssmconv_p, ssmconv_s, sconv_p, sconv_s, mem_k_p, mem_v_p, v_s)
```

```python
import os
import types
import numpy as np
from contextlib import ExitStack
import concourse.bass as bass
import concourse.mybir as mybir
from concourse.bass_utils import run_bass_kernel_spmd

F32 = mybir.dt.float32
BF16 = mybir.dt.bfloat16
AF = mybir.ActivationFunctionType
ALU = mybir.AluOpType
AX = mybir.AxisListType

NCORES = 8
D = 1024
NP = 2048
NS = 64
NT = NP + NS
EPS = 1e-6
DFF = 2816
NEXP = 8

C_GMIX, C_GMEM, C_GFFN, C_GFIN = 0, 16, 32, 48
C_CWS, C_CBS, C_GN, C_CWC = 56, 104, 116, 124
C_DTB, C_ALOG, C_DSK = 148, 149, 150
NPCOL = 166


class Buf:
    __slots__ = ("name", "last_w", "readers", "dsem", "dcount", "excl")

    def __init__(self, name, excl=False):
        self.name = name
        self.excl = excl
        self.last_w = None
        self.readers = {}
        self.dsem = None
        self.dcount = 0


def _freeze(fn):
    if fn.__closure__ is None:
        return fn
    cells = []
    for c in fn.__closure__:
        try:
            cells.append(types.CellType(c.cell_contents))
        except ValueError:
            cells.append(c)
    g = types.FunctionType(fn.__code__, fn.__globals__, fn.__name__, fn.__defaults__, tuple(cells))
    g.__kwdefaults__ = fn.__kwdefaults__
    return g


class Sched:
    ENG = ("pe", "act", "dve", "pool", "sp")

    def __init__(self, nc, stack):
        self.nc = nc
        self.prog = {e: [] for e in self.ENG}
        self.count = {e: 0 for e in self.ENG}
        self.seen = {e: {} for e in self.ENG}
        self.sems = {}
        self.dbufs = []
        self._stack = stack
        self.nops = 0

    def _sem(self, key):
        if key not in self.sems:
            self.sems[key] = self._stack.enter_context(self.nc.semaphore("s%d" % len(self.sems)))
        return self.sems[key]

    def _deps(self, eng, reads, writes):
        need = {}

        def want(k, v):
            if v > need.get(k, 0):
                need[k] = v
        me = ("e", eng)
        for b in reads:
            if b.last_w is not None:
                want(*b.last_w)
            if b.excl:
                for k, v in b.readers.items():
                    if k != me:
                        want(k, v)
        for b in writes:
            if b.last_w is not None:
                want(*b.last_w)
            for k, v in b.readers.items():
                want(k, v)
        out = []
        seen = self.seen[eng]
        for k, v in need.items():
            if seen.get(k, 0) < v:
                seen[k] = v
                out.append((self._sem(k), v))
        return out

    def op(self, eng, fn, reads=(), writes=()):
        fn = _freeze(fn)
        waits = self._deps(eng, reads, writes)
        self.count[eng] += 1
        tick = self.count[eng]
        key = ("e", eng)
        sem = self._sem(key)
        self.nops += 1

        def run(e, fn=fn, waits=waits, sem=sem):
            for s, v in waits:
                e.wait_ge(s, v)
            ins = fn(e)
            ins.then_inc(sem, 1)
        self.prog[eng].append(run)
        for b in writes:
            b.last_w = (key, tick)
            b.readers = {}
        for b in reads:
            if b not in writes:
                b.readers[key] = tick

    def dma(self, queue, fn, sbuf, reads=(), writes=(), n=1):
        fn = _freeze(fn)
        waits = self._deps(queue, reads, writes)
        if sbuf.dsem is None:
            sbuf.dsem = ("d", len(self.dbufs))
            self.dbufs.append(sbuf)
        key = sbuf.dsem
        sem = self._sem(key)
        sbuf.dcount += 16 * n
        val = sbuf.dcount

        def run(e, fn=fn, waits=waits, sem=sem, n=n):
            for s, v in waits:
                e.wait_ge(s, v)
            inss = fn(e)
            assert len(inss) == n
            for ins in inss:
                ins.then_inc(sem, 16)
        self.prog[queue].append(run)
        for b in writes:
            b.last_w = (key, val)
            b.readers = {}
        for b in reads:
            if b not in writes:
                b.readers[key] = val

    def barrier(self, engines=None):
        targets = [(("e", e), self.count[e]) for e in ("pe", "act", "dve") if self.count[e] > 0]
        targets += [(b.dsem, b.dcount) for b in self.dbufs]
        for eng in (engines or self.ENG):
            seen = self.seen[eng]
            waits = []
            for k, v in targets:
                if seen.get(k, 0) < v:
                    seen[k] = v
                    waits.append((self._sem(k), v))

            def run(e, waits=waits):
                for s, v in waits:
                    e.wait_ge(s, v)
            self.prog[eng].append(run)

    def emit(self, block):
        prog = self.prog

        @block.tensor
        def _(e):
            for f in prog["pe"]:
                f(e)

        @block.scalar
        def _(e):
            for f in prog["act"]:
                f(e)

        @block.vector
        def _(e):
            for f in prog["dve"]:
                f(e)

        @block.gpsimd
        def _(e):
            for f in prog["pool"]:
                f(e)

        @block.sync
        def _(e):
            for f in prog["sp"]:
                f(e)


class Ctx:
    def __init__(self, nc, stack):
        self.nc = nc
        self.st = stack
        self.S = Sched(nc, stack)
        self.arena_words = 52224
        self.arena = stack.enter_context(nc.sbuf_tensor("arena", [128, self.arena_words], F32))
        self.off = 0
        self.banks = []
        for i in range(8):
            t = stack.enter_context(nc.psum_tensor("bank%d" % i, [128, 512], F32))
            self.banks.append((t, Buf("bank%d" % i, excl=True)))
        self.bi = 0
        self.evi = 0

    def alloc(self, name, shape, dt):
        esz = 4 if dt == F32 else 2
        n = 1
        for s in shape[1:]:
            n *= s
        nbytes = (n * esz + 3) // 4 * 4
        w0 = self.off // 4
        nw = nbytes // 4
        assert w0 + nw <= self.arena_words, ("SBUF arena overflow", name, self.off, nbytes)
        ap = self.arena[:, w0:w0 + nw]
        if dt != F32:
            ap = ap.bitcast(dt)
        if shape[0] != 128:
            ap = ap[0:shape[0]]
        if len(shape) == 3:
            ap = ap.rearrange("p (a b) -> p a b", a=shape[1])
        elif len(shape) == 4:
            ap = ap.rearrange("p (a b c) -> p a b c", a=shape[1], b=shape[2])
        self.off += nbytes
        return ap, Buf(name)

    def bank(self):
        t, b = self.banks[self.bi]
        self.bi = (self.bi + 1) % 8
        return t, b


def _lay_pcol(inp):
    pc = np.zeros((128, NPCOL), np.float32)

    def cols(v):
        return np.ascontiguousarray(v.reshape(-1, 128).T)
    for l in range(2):
        pc[:, C_GMIX + 8 * l:C_GMIX + 8 * l + 8] = cols(inp["g_mix"][l])
        pc[:, C_GMEM + 8 * l:C_GMEM + 8 * l + 8] = cols(inp["g_mem"][l])
        pc[:, C_GFFN + 8 * l:C_GFFN + 8 * l + 8] = cols(inp["g_ffn"][l])
    pc[:, C_GFIN:C_GFIN + 8] = cols(inp["g_final"])
    for k in range(4):
        pc[:, C_CWS + 12 * k:C_CWS + 12 * k + 12] = cols(inp["conv_w_ssm"][0, k])
    pc[:, C_CBS:C_CBS + 12] = cols(inp["conv_b_ssm"][0])
    pc[:, C_GN:C_GN + 8] = cols(inp["g_ssm_norm"][0])
    for k in range(3):
        pc[:, C_CWC + 8 * k:C_CWC + 8 * k + 8] = cols(inp["conv_w_c"][0, k])
    pc[0:16, C_DTB] = inp["dt_bias"][0]
    pc[0:16, C_ALOG] = inp["a_log"][0]
    pc[:, C_DSK:C_DSK + 16] = np.broadcast_to(inp["d_skip"][0][None, :], (128, 16))
    return pc


def _consts():
    ident = np.eye(128, dtype=np.float32)
    tri = np.triu(np.ones((128, 128), np.float32))
    R = np.zeros((16, 8, 128), np.float32)
    for j in range(8):
        R[2 * j, j, 0:64] = 1.0
        R[2 * j + 1, j, 64:128] = 1.0
    sel = np.zeros((8, 8, 128), np.float32)
    for e in range(8):
        sel[e, e, :] = 1.0
    return ident, tri, R.reshape(16, 1024), sel.reshape(8, 1024)


def build_program(stop_after=99, dbg=False):
    nc = bass.Bass("TRN2", target_bir_lowering=False)

    def din(name, shape):
        return nc.dram_tensor(name, list(shape), F32, kind="ExternalInput").ap()

    def dout(name, shape):
        return nc.dram_tensor(name, list(shape), F32, kind="ExternalOutput").ap()

    xp_d = din("xp", [NP, D])
    xs_d = din("xs", [NS, D])
    memp_d = din("memp", [256, D])
    sssm_d = din("sssm", [16 * 1024, 128])
    scv_d = din("scv", [48, 1536])
    ssc_d = din("ssc", [32, 1024])
    ck_d = din("ck", [2 * 16 * 256, 1024])
    cv_d = din("cv", [2 * 16 * 256, 1024])
    pcol_d = din("pcol", [128, NPCOL])
    bsp_d = din("bsp", [1, 1024])
    wsT_d = din("wsT", [128, 1024])
    ident_d = din("ident", [128, 128])
    tri_d = din("tri", [128, 128])
    R_d = din("Rm", [16, 1024])
    sel_d = din("selm", [8, 1024])
    wblk_d = din("wblk", [64, 512])
    tri4_d = din("tri4", [64, 64])
    bsps_d = din("bsps", [1, 512])
    w_in_ab = din("w_in_ab", [1024, 4624])
    w_out_ab = din("w_out_ab", [2048, 1024])
    w_ffn_gate = din("w_ffn_gate", [1024, DFF])
    w_ffn_up = din("w_ffn_up", [1024, DFF])
    w_ffn_down = din("w_ffn_down", [DFF, 1024])
    w_in_c = din("w_in_c", [1024, 3072])
    w_out_c = din("w_out_c", [1024, 1024])
    w_router = din("w_router", [1024, 8])
    w_exp_gate = din("w_exp_gate", [8 * 1024, DFF])
    w_exp_up = din("w_exp_up", [8 * 1024, DFF])
    w_exp_down = din("w_exp_down", [8 * DFF, 1024])
    w_mem_q = din("w_mem_q", [2 * 1024, 1024])
    w_mem_k = din("w_mem_k", [2 * 1024, 1024])
    w_mem_v = din("w_mem_v", [2 * 1024, 1024])
    w_mem_o = din("w_mem_o", [2 * 1024, 1024])

    yp_d = dout("yp", [NP, D])
    ys_d = dout("ys", [NS, D])
    ssmp_d = dout("ssmp", [1024, 128])
    ssms_d = dout("ssms", [16 * 1024, 128])
    scvp_d = dout("scvp", [3, 1536])
    scvs_d = dout("scvs", [48, 1536])
    sccp_d = dout("sccp", [2, 1024])
    sccs_d = dout("sccs", [32, 1024])
    mkp_d = dout("mkp", [2 * 256, 1024])
    mvp_d = dout("mvp", [2 * 256, 1024])
    vs_d = dout("vs", [NS, 1024])
    if dbg:
        dbg_d = dout("dbgx", [128, 8 * NT])
    w_in_ab_b = nc.dram_tensor("w_in_ab_b", [1024, 4624], BF16, kind="Internal").ap()
    w_out_ab_b = nc.dram_tensor("w_out_ab_b", [2048, 1024], BF16, kind="Internal").ap()

    with ExitStack() as st:
        K = Ctx(nc, st)
        S = K.S
        outbufs = []

        x, Bx = K.alloc("x", [128, 8, NT], F32)
        pcol, Bpc = K.alloc("pcol", [128, NPCOL], F32)
        identf, Bidf = K.alloc("identf", [128, 128], F32)
        identb, Bidb = K.alloc("identb", [128, 128], BF16)
        trif, Btrf = K.alloc("trif", [128, 128], F32)
        onesf, Bonf = K.alloc("onesf", [128, 128], F32)
        onesb, Bonb = K.alloc("onesb", [128, 128], BF16)
        acol, Bacol = K.alloc("acol", [16, 2], F32)
        PERSIST = K.off

        Bwinb = [Buf("w_in_ab_b%d" % i) for i in range(10)]
        Bwoutb = [Buf("w_out_ab_b%d" % i) for i in range(8)]
        for cb in range(10):
            c0, c1 = cb * 512, min(4624, cb * 512 + 512)
            S.dma("pool", lambda e, c0=c0, c1=c1: [e.dma_start(out=w_in_ab_b[:, c0:c1], in_=w_in_ab[:, c0:c1])], Bwinb[cb], writes=[Bwinb[cb]])
        for cb in range(8):
            S.dma("pool", lambda e, cb=cb: [e.dma_start(out=w_out_ab_b[:, cb * 128:(cb + 1) * 128], in_=w_out_ab[:, cb * 128:(cb + 1) * 128])], Bwoutb[cb], writes=[Bwoutb[cb]])
        S.dma("sp", lambda e: [e.dma_start(out=pcol, in_=pcol_d)], Bpc, writes=[Bpc])
        S.dma("sp", lambda e: [e.dma_start(out=identf, in_=ident_d)], Bidf, writes=[Bidf])
        S.dma("pool", lambda e: [e.dma_start(out=identb, in_=ident_d)], Bidb, writes=[Bidb])
        S.dma("sp", lambda e: [e.dma_start(out=trif, in_=tri_d)], Btrf, writes=[Btrf])
        S.op("dve", lambda e: e.memset(onesf, 1.0), writes=[Bonf])
        S.op("dve", lambda e: e.memset(onesb, 1.0), writes=[Bonb])
        S.op("act", lambda e: e.activation(out=acol[:, 0:1], in_=pcol[0:16, C_ALOG:C_ALOG + 1], func=AF.Exp), reads=[Bpc], writes=[Bacol])
        S.op("dve", lambda e: e.tensor_scalar(out=acol[:, 1:2], in0=acol[:, 0:1], scalar1=-1.0, scalar2=None, op0=ALU.mult), reads=[Bacol], writes=[Bacol])

        def evac_copy(dst, src, rb, wb, extra_reads=()):
            K.evi += 1
            if K.evi % 2 == 0:
                S.op("act", lambda e: e.activation(out=dst, in_=src, func=AF.Copy), reads=[rb] + list(extra_reads), writes=[wb])
            else:
                S.op("dve", lambda e: e.tensor_copy(out=dst, in_=src), reads=[rb] + list(extra_reads), writes=[wb])

        def mm_group(out_ap, pairs, bankbuf, reads):
            n = len(pairs)

            def fn(e):
                ins = None
                for i, (l, r) in enumerate(pairs):
                    ins = e.matmul(out_ap, lhsT=l, rhs=r, start=(i == 0), stop=(i == n - 1))
                return ins
            S.op("pe", fn, reads=reads, writes=[bankbuf])

        def wload(dst, dbuf, src):
            S.dma("pool", lambda e: [e.dma_start(out=dst, in_=src)], dbuf, writes=[dbuf])

        def wview(w2d, r0, nrows, c0, ncols):
            return w2d[r0:r0 + nrows, c0:c0 + ncols].rearrange("(kc p) n -> p kc n", p=128)

        K.off = PERSIST
        xin = [K.alloc("xin%d" % i, [128, 4, D], F32) for i in range(2)]
        for ti in range(4):
            xi, Bxi = xin[ti % 2]
            S.dma("sp", lambda e, xi=xi, ti=ti: [e.dma_start(out=xi, in_=xp_d[ti * 512:(ti + 1) * 512, :].rearrange("(c p) f -> p c f", p=128))],
                  Bxi, writes=[Bxi])
            for kc in range(8):
                bk, Bbk = K.bank()

                def tr(e, xi=xi, bk=bk, kc=kc):
                    ins = None
                    for c in range(4):
                        ins = e.transpose(bk[:, c * 128:(c + 1) * 128], xi[:, c, kc * 128:(kc + 1) * 128], identf)
                    return ins
                S.op("pe", tr, reads=[Bxi, Bidf], writes=[Bbk])
                evac_copy(x[:, kc, ti * 512:(ti + 1) * 512], bk[:, :], Bbk, Bx)
        xi, Bxi = xin[0]
        S.dma("sp", lambda e: [e.dma_start(out=xi[0:64, 0, :], in_=xs_d)], Bxi, writes=[Bxi])
        bk, Bbk = K.bank()

        def trs(e, xi=xi, bk=bk):
            ins = None
            for kc in range(8):
                ins = e.transpose(bk[:, kc * 64:(kc + 1) * 64], xi[0:64, 0, kc * 128:(kc + 1) * 128], identf[0:64, 0:64])
            return ins
        S.op("pe", trs, reads=[Bxi, Bidf], writes=[Bbk])
        evac_copy(x[:, :, NP:NT], bk[:, :].rearrange("p (k t) -> p k t", k=8), Bbk, Bx)

        def rmsnorm(t0, W, gcol, hn, Bhn, sq, Bsq, rstd, Brstd, hnf=None, Bhnf=None):
            S.op("act", lambda e: e.activation(out=sq[:, :, 0:W], in_=x[:, :, t0:t0 + W], func=AF.Square), reads=[Bx], writes=[Bsq])
            bk, Bbk = K.bank()
            mm_group(bk[:, 0:W], [(onesb, sq[:, kc, 0:W]) for kc in range(8)], Bbk, [Bonb, Bsq])
            S.op("act", lambda e: e.activation(out=rstd[:, 0:W], in_=bk[:, 0:W], func=AF.Sqrt, bias=EPS, scale=1.0 / D), reads=[Bbk], writes=[Brstd])
            S.op("dve", lambda e: e.reciprocal(out=rstd[:, 0:W], in_=rstd[:, 0:W]), reads=[Brstd], writes=[Brstd])
            for kc in range(8):
                if hn is not None:
                    S.op("dve", lambda e, kc=kc: e.scalar_tensor_tensor(out=hn[:, kc, 0:W], in0=x[:, kc, t0:t0 + W], scalar=pcol[:, gcol + kc:gcol + kc + 1],
                                                                      in1=rstd[:, 0:W], op0=ALU.mult, op1=ALU.mult), reads=[Bx, Bpc, Brstd], writes=[Bhn])
                if hnf is not None:
                    S.op("dve", lambda e, kc=kc: e.scalar_tensor_tensor(out=hnf[:, kc, 0:W], in0=x[:, kc, t0:t0 + W], scalar=pcol[:, gcol + kc:gcol + kc + 1],
                                                                      in1=rstd[:, 0:W], op0=ALU.mult, op1=ALU.mult), reads=[Bx, Bpc, Brstd], writes=[Bhnf])

        def add_to_x(oc, t0, W, bk, Bbk):
            S.op("dve", lambda e: e.tensor_tensor(out=x[:, oc, t0:t0 + W], in0=bk[:, 0:W], in1=x[:, oc, t0:t0 + W], op=ALU.add), reads=[Bbk, Bx], writes=[Bx])

        def dump_x():
            S.barrier()
            S.dma("sp", lambda e: [e.dma_start(out=dbg_d, in_=x.rearrange("p k t -> p (k t)"))], Bx, reads=[Bx])
            outbufs.append(Bx)

        TILES512 = [(i * 512, 512) for i in range(4)] + [(NP, NS)]
        TILES_E = [(i * 448, 448) for i in range(4)] + [(1792, 320)]

        def phase1():
            S.barrier()
            K.off = PERSIST
            WT = 256
            hn, Bhn = K.alloc("hn", [128, 8, WT], BF16)
            sq, Bsq = K.alloc("sq", [128, 8, WT], BF16)
            rstd, Brstd = K.alloc("rstd", [128, WT], F32)
            wbufs = [K.alloc("w1_%d" % i, [128, 8, 512], BF16) for i in range(2)]
            wobufs = [K.alloc("wo1_%d" % i, [128, 16, 128], BF16) for i in range(2)]
            wdt, Bwdt = K.alloc("wdt", [128, 8, 16], BF16)
            u_fm, Bu = K.alloc("u_fm", [128, 8, WT], BF16)
            z_fm, Bz = K.alloc("z_fm", [128, 8, WT], BF16)
            v_tm, Bv = K.alloc("v_tm", [128, 2, 1024], BF16)
            v_f32, Bvf = K.alloc("v_f32", [64, 1024], F32)
            xlo, Bxlo = v_f32[0:48, 0:512], Bvf
            xbc, Bxbc = K.alloc("xbc", [128, 12, 3 + WT], BF16)
            xbcs, Bxbcs = K.alloc("xbcs", [128, 12, 16, 7], BF16)
            xlast, Bxl = K.alloc("xlast", [128, 12, 48], F32)
            xc, Bxc = K.alloc("xc", [128, 12, WT], BF16)
            dtf, Bdtf = K.alloc("dtf", [16, 2, WT], F32)
            _mk = K.off
            sci, Bsci = K.alloc("sci", [48, 1536], F32)
            K.off = _mk
            ycat, Bycat = K.alloc("ycat", [128, 16, WT], BF16)
            Bsci = Bycat
            diag, Bdiag = K.alloc("diag", [128, 48, 128], BF16)
            dI, BdI = K.alloc("dI", [128, 16, 128], BF16)
            wsm, Bwsm = K.alloc("wsm", [128, 8, 128], BF16)
            wsf, Bwsf = K.alloc("wsf", [128, 8, 128], BF16)
            bsp, Bbsp = K.alloc("bsp", [1, 1024], BF16)
            onesr, Bonesr = K.alloc("onesr", [1, 128], BF16)
            Sst = [K.alloc("Sst%d" % i, [128, 8, 128], F32) for i in range(2)]
            Rm, BRm = K.alloc("Rm", [16, 8, 128], F32)
            S.dma("sp", lambda e: [e.dma_start(out=Rm, in_=R_d.rearrange("h (j q) -> h j q", j=8))], BRm, writes=[BRm])
            STb, BSTb = K.alloc("STb", [128, 1024], BF16)
            xs_tm, Bxstm = K.alloc("xs_tm", [128, 1024], BF16)
            xdt_tm, Bxdt = K.alloc("xdt_tm", [128, 1024], BF16)
            xw_tm, Bxw = xdt_tm, Bxdt
            B_tm, BBtm = K.alloc("B_tm", [128, 2, 128], BF16)
            dtt, Bdtt = K.alloc("dtt", [128, 2, 16], F32)
            la_tm, Blat = K.alloc("la_tm", [128, 16], F32)
            wend, Bwend = K.alloc("wend", [128, 16], F32)
            sumd, Bsumd = K.alloc("sumd", [16, 1], F32)
            elc, Belc = K.alloc("elc", [128, 8], F32)
            rhsall, Brhs = K.alloc("rhsall", [128, 8, 128], F32)
            seg, Bseg = rhsall, Brhs
            Eb, BEb = K.alloc("Eb", [128, 8, 128], BF16)
            ELb, BELb = K.alloc("ELb", [128, 8, 128], BF16)
            CEb, BCEb = ELb, BELb
            wts, Bwts = Eb, BEb
            cbm, Bcbm = K.alloc("cbm", [128, 128], BF16)
            gated, Bgat = K.alloc("gated", [128, 4, 128], F32)
            gsq, Bgsq = K.alloc("gsq", [128, 4, 128], BF16)
            grs, Bgrs = K.alloc("grs", [128, 128], F32)
            wbf, Bwbf = K.alloc("wbf", [64, 8, 64], BF16)
            wbs, Bwbs = K.alloc("wbs", [64, 8, 64], BF16)
            tri4, Btri4 = K.alloc("tri4", [64, 64], F32)
            bsps, Bbsps = K.alloc("bsps", [1, 512], BF16)
            print("phase1 SBUF bytes/partition:", K.off)

            for k in range(4):
                for j in range(12):
                    idx = k * 12 + j
                    S.op("dve", lambda e, idx=idx: e.tensor_scalar(out=diag[:, idx, :], in0=identf, scalar1=pcol[:, C_CWS + idx:C_CWS + idx + 1], scalar2=None, op0=ALU.mult),
                         reads=[Bidf, Bpc], writes=[Bdiag])
            for h in range(16):
                S.op("dve", lambda e, h=h: e.tensor_scalar(out=dI[:, h, :], in0=identf, scalar1=pcol[:, C_DSK + h:C_DSK + h + 1], scalar2=None, op0=ALU.mult),
                     reads=[Bidf, Bpc], writes=[BdI])
            S.dma("pool", lambda e: [e.dma_start(out=wsf, in_=wsT_d.rearrange("s (g t) -> s g t", g=8))], Bwsf, writes=[Bwsf])
            S.op("dve", lambda e: e.tensor_tensor(out=wsm, in0=wsf, in1=trif.unsqueeze(1).to_broadcast([128, 8, 128]), op=ALU.mult), reads=[Bwsf, Btrf], writes=[Bwsm])
            S.dma("pool", lambda e: [e.dma_start(out=bsp, in_=bsp_d)], Bbsp, writes=[Bbsp])
            S.op("dve", lambda e: e.memset(onesr, 1.0), writes=[Bonesr])
            S.dma("pool", lambda e: [e.dma_start(out=wbf, in_=wblk_d.rearrange("r (g c) -> r g c", g=8))], Bwbf, writes=[Bwbf])
            S.dma("sp", lambda e: [e.dma_start(out=tri4, in_=tri4_d)], Btri4, writes=[Btri4])
            S.op("dve", lambda e: e.tensor_tensor(out=wbs, in0=wbf, in1=tri4.unsqueeze(1).to_broadcast([64, 8, 64]), op=ALU.mult), reads=[Bwbf, Btri4], writes=[Bwbs])
            S.dma("pool", lambda e: [e.dma_start(out=bsps, in_=bsps_d)], Bbsps, writes=[Bbsps])
            S.op("dve", lambda e: e.memset(xbc[:, :, 0:3], 0.0), writes=[Bxbc])
            S.dma("sp", lambda e: [e.dma_start(out=wdt, in_=wview(w_in_ab_b, 0, 1024, 4608, 16))], Bwdt, reads=[Bwinb[9]], writes=[Bwdt])
            S.dma("sp", lambda e: [e.dma_start(out=sci, in_=scv_d)], Bsci, writes=[Bsci])
            for j4 in range(3):
                bk, Bbk = K.bank()

                def trc(e, bk=bk, j4=j4):
                    ins = None
                    for jj in range(4):
                        j = j4 * 4 + jj
                        ins = e.transpose(bk[:, jj * 48:(jj + 1) * 48], sci[:, j * 128:(j + 1) * 128], identf[0:48, 0:48])
                    return ins
                S.op("pe", trc, reads=[Bsci, Bidf], writes=[Bbk])
                evac_copy(xbcs[:, j4 * 4:(j4 + 1) * 4, :, 0:3], bk[:, 0:192].rearrange("p (j b k) -> p j b k", j=4, b=16), Bbk, Bxbcs)

            sample_g1_bufs = tuple(Buf("sg1_%d" % i) for i in range(7))
            wi = [0]

            def next_w():
                r = wbufs[wi[0] % 2]
                wi[0] += 1
                return r
            woi = [0]

            tiles = [(i * WT, WT, True) for i in range(NP // WT)] + [(NP, NS, False)]
            sidx = [0]
            for (t0, W, is_p) in tiles:
                Q = 128 if is_p else 4
                nch = W // Q
                rmsnorm(t0, W, C_GMIX, hn, Bhn, sq, Bsq, rstd, Brstd)
                for blk in range(9):
                    wt, Bwt = next_w()
                    S.dma("sp", lambda e, wt=wt, blk=blk: [e.dma_start(out=wt, in_=wview(w_in_ab_b, 0, 1024, blk * 512, 512))], Bwt, reads=[Bwinb[blk]], writes=[Bwt])
                    if blk in (2, 3):
                        vb = blk - 2
                        for c in range((W + 127) // 128):
                            rows = min(128, W - c * 128)
                            bk, Bbk = K.bank()
                            mm_group(bk[0:rows, :], [(hn[:, kc, c * 128:c * 128 + rows], wt[:, kc, :]) for kc in range(8)], Bbk, [Bhn, Bwt])
                            if is_p:
                                S.op("act", lambda e, bk=bk, c=c, vb=vb: e.activation(out=v_tm[:, c, vb * 512:(vb + 1) * 512], in_=bk[:, :], func=AF.Gelu_apprx_tanh),
                                     reads=[Bbk], writes=[Bv])
                            else:
                                S.op("act", lambda e, bk=bk, vb=vb: e.activation(out=v_f32[:, vb * 512:(vb + 1) * 512], in_=bk[0:64, :], func=AF.Gelu_apprx_tanh),
                                     reads=[Bbk], writes=[Bvf])
                                S.op("dve", lambda e, vb=vb: e.tensor_copy(out=v_tm[0:64, 0, vb * 512:(vb + 1) * 512], in_=v_f32[:, vb * 512:(vb + 1) * 512]),
                                     reads=[Bvf], writes=[Bv])
                        continue
                    for oc in range(4):
                        col = blk * 4 + oc
                        bk, Bbk = K.bank()
                        mm_group(bk[:, 0:W], [(wt[:, kc, oc * 128:(oc + 1) * 128], hn[:, kc, 0:W]) for kc in range(8)], Bbk, [Bhn, Bwt])
                        if col < 8:
                            S.op("act", lambda e, bk=bk, col=col: e.activation(out=u_fm[:, col, 0:W], in_=bk[:, 0:W], func=AF.Gelu_apprx_tanh), reads=[Bbk], writes=[Bu])
                        elif col < 24:
                            j = col - 16
                            S.op("act", lambda e, bk=bk, j=j: e.activation(out=z_fm[:, j, 0:W], in_=bk[:, 0:W], func=AF.Silu), reads=[Bbk], writes=[Bz])
                        else:
                            j = col - 24
                            if is_p:
                                S.op("act", lambda e, bk=bk, j=j: e.activation(out=xbc[:, j, 3:3 + W], in_=bk[:, 0:W], func=AF.Copy), reads=[Bbk], writes=[Bxbc])
                                if t0 + W == NP:
                                    S.op("dve", lambda e, bk=bk, j=j: e.tensor_copy(out=xlast[:, j, 0:3], in_=bk[:, W - 3:W]), reads=[Bbk], writes=[Bxl])
                            else:
                                S.op("act", lambda e, bk=bk, j=j: e.activation(out=xbcs[:, j, :, 3:7], in_=bk[:, 0:64].rearrange("p (b l) -> p b l", b=16), func=AF.Copy),
                                     reads=[Bbk], writes=[Bxbcs])
                                S.op("dve", lambda e, bk=bk, j=j: e.tensor_copy(out=xlast[:, j, :].rearrange("p (b k) -> p b k", b=16),
                                                                                 in_=bk[:, 0:64].rearrange("p (b l) -> p b l", b=16)[:, :, 1:4]), reads=[Bbk], writes=[Bxl])
                bk, Bbk = K.bank()
                mm_group(bk[0:16, 0:W], [(wdt[:, kc, :], hn[:, kc, 0:W]) for kc in range(8)], Bbk, [Bhn, Bwdt])
                S.op("act", lambda e, bk=bk: e.activation(out=dtf[:, 0, 0:W], in_=bk[0:16, 0:W], func=AF.Exp, bias=pcol[0:16, C_DTB:C_DTB + 1]), reads=[Bbk, Bpc], writes=[Bdtf])
                S.op("act", lambda e: e.activation(out=dtf[:, 0, 0:W], in_=dtf[:, 0, 0:W], func=AF.Ln, bias=1.0), reads=[Bdtf], writes=[Bdtf])
                S.op("dve", lambda e: e.tensor_scalar(out=dtf[:, 1, 0:W], in0=dtf[:, 0, 0:W], scalar1=acol[:, 1:2], scalar2=None, op0=ALU.mult), reads=[Bdtf, Bacol], writes=[Bdtf])
                for j in range(12):
                    bk, Bbk = K.bank()
                    if is_p:
                        pairs = [(diag[:, k * 12 + j, :], xbc[:, j, k:k + W]) for k in range(4)]
                        mm_group(bk[:, 0:W], pairs, Bbk, [Bdiag, Bxbc])
                    else:
                        pairs = [(diag[:, k * 12 + j, :], xbcs[:, j, :, k:k + 4]) for k in range(4)]
                        mm_group(bk[:, 0:W], pairs, Bbk, [Bdiag, Bxbcs])
                    S.op("act", lambda e, bk=bk, j=j: e.activation(out=xc[:, j, 0:W], in_=bk[:, 0:W], func=AF.Silu, bias=pcol[:, C_CBS + j:C_CBS + j + 1]), reads=[Bbk, Bpc], writes=[Bxc])
                if is_p:
                    S.op("dve", lambda e: e.tensor_copy(out=xbc[:, :, 0:3], in_=xbc[:, :, W:W + 3]), reads=[Bxbc], writes=[Bxbc])

                for c in range(nch):
                    o = c * Q
                    first = is_p and (t0 == 0 and c == 0)
                    if is_p:
                        Sc, BSc = Sst[0]
                    else:
                        b = c

                        def loadS(bb):
                            Sl, BSl = Sst[bb % 2]
                            S.dma("sp", lambda e, Sl=Sl, bb=bb: [e.dma_start(out=Sl, in_=sssm_d[bb * 1024:(bb + 1) * 1024, :].rearrange("(j q) n -> q j n", q=128))], BSl, writes=[BSl])
                        if b == 0:
                            loadS(0)
                        if b + 1 < 16:
                            loadS(b + 1)
                        Sc, BSc = Sst[b % 2]
                    for g in range(8):
                        bk, Bbk = K.bank()
                        if is_p:
                            vsl = v_tm[0:Q, c, g * 128:(g + 1) * 128]
                        else:
                            vsl = None
                        if is_p:
                            pairs = [(vsl, wsm[0:Q, g, 0:Q]), (onesr[0:1, 0:128], bsp[0:1, g * 128:g * 128 + Q])]
                            mm_group(bk[:, 0:Q], pairs, Bbk, [Bv, Bwsm, Bonesr, Bbsp])
                            S.op("dve", lambda e, bk=bk, g=g, o=o: e.tensor_tensor(out=ycat[:, g, o:o + Q], in0=bk[:, 0:Q], in1=u_fm[:, g, o:o + Q], op=ALU.mult),
                                 reads=[Bbk, Bu], writes=[Bycat])
                    bkx, Bbkx = K.bank()
                    bkxb = bkx[:, :].bitcast(BF16)

                    def trx(e, bkxb=bkxb, o=o, Q=Q):
                        ins = None
                        for j in range(8):
                            ins = e.transpose(bkxb[0:Q, j * 128:(j + 1) * 128], xc[:, j, o:o + Q], identb)
                        return ins
                    S.op("pe", trx, reads=[Bxc, Bidb], writes=[Bbkx])
                    S.op("act", lambda e, bkxb=bkxb, Q=Q: e.activation(out=xs_tm[0:Q, :], in_=bkxb[0:Q, :], func=AF.Copy), reads=[Bbkx], writes=[Bxstm])
                    bkb, Bbkb = K.bank()
                    bkbb = bkb[:, :].bitcast(BF16)

                    def trb(e, bkbb=bkbb, o=o, Q=Q):
                        ins = None
                        for g in range(2):
                            ins = e.transpose(bkbb[0:Q, g * 128:(g + 1) * 128], xc[:, 8 + g, o:o + Q], identb)
                        return ins
                    S.op("pe", trb, reads=[Bxc, Bidb], writes=[Bbkb])
                    S.op("dve", lambda e, bkbb=bkbb, Q=Q: e.tensor_copy(out=B_tm[0:Q, :, :], in_=bkbb[0:Q, 0:256].rearrange("p (g n) -> p g n", g=2)), reads=[Bbkb], writes=[BBtm])
                    bkd, Bbkd = K.bank()

                    def trd(e, bkd=bkd, o=o, Q=Q):
                        e.transpose(bkd[0:Q, 0:16], dtf[:, 0, o:o + Q], identf[0:16, 0:16])
                        return e.transpose(bkd[0:Q, 16:32], dtf[:, 1, o:o + Q], identf[0:16, 0:16])
                    S.op("pe", trd, reads=[Bdtf, Bidf], writes=[Bbkd])
                    S.op("dve", lambda e, bkd=bkd, Q=Q: e.tensor_copy(out=dtt[0:Q, :, :], in_=bkd[0:Q, 0:32].rearrange("p (a h) -> p a h", a=2)), reads=[Bbkd], writes=[Bdtt])
                    bkl, Bbkl = K.bank()

                    def mla(e, bkl=bkl, Q=Q):
                        e.matmul(bkl[0:Q, 0:16], lhsT=trif[0:Q, 0:Q], rhs=dtt[0:Q, 1, :], start=True, stop=True)
                        return e.matmul(bkl[0:Q, 16:32], lhsT=onesf[0:Q, 0:Q], rhs=dtt[0:Q, 1, :], start=True, stop=True)
                    S.op("pe", mla, reads=[Btrf, Bonf, Bdtt], writes=[Bbkl])
                    S.op("dve", lambda e, bkl=bkl, Q=Q: e.tensor_copy(out=la_tm[0:Q, :], in_=bkl[0:Q, 0:16]), reads=[Bbkl], writes=[Blat])
                    S.op("dve", lambda e, bkl=bkl, Q=Q: e.tensor_tensor(out=wend[0:Q, :], in0=bkl[0:Q, 16:32], in1=la_tm[0:Q, :], op=ALU.subtract), reads=[Bbkl, Blat], writes=[Bwend])
                    S.op("act", lambda e, Q=Q: e.activation(out=wend[0:Q, :], in_=wend[0:Q, :], func=AF.Exp), reads=[Bwend], writes=[Bwend])
                    S.op("dve", lambda e, o=o, Q=Q: e.tensor_reduce(out=sumd, in_=dtf[:, 1, o:o + Q], axis=AX.X, op=ALU.add), reads=[Bdtf], writes=[Bsumd])
                    bke, Bbke = K.bank()

                    def mel(e, bke=bke):
                        ins = None
                        for j in range(8):
                            ins = e.matmul(bke[:, j:j + 1], lhsT=Rm[:, j, :], rhs=sumd, start=True, stop=True)
                        return ins
                    S.op("pe", mel, reads=[BRm, Bsumd], writes=[Bbke])
                    S.op("act", lambda e, bke=bke: e.activation(out=elc, in_=bke[:, 0:8], func=AF.Exp), reads=[Bbke], writes=[Belc])
                    S.op("dve", lambda e, Q=Q: e.tensor_tensor(out=xdt_tm[0:Q, :].rearrange("p (h d) -> p h d", h=16), in0=xs_tm[0:Q, :].rearrange("p (h d) -> p h d", h=16),
                                                               in1=dtt[0:Q, 0, :].unsqueeze(2).to_broadcast([Q, 16, 64]), op=ALU.mult), reads=[Bxstm, Bdtt], writes=[Bxdt])
                    if not first:
                        for half in range(2):
                            bks, Bbks = K.bank()

                            def trS(e, bks=bks, half=half, Sc=Sc):
                                ins = None
                                for jj in range(4):
                                    ins = e.transpose(bks[:, jj * 128:(jj + 1) * 128], Sc[:, half * 4 + jj, :], identf)
                                return ins
                            S.op("pe", trS, reads=[BSc, Bidf], writes=[Bbks])
                            evac_copy(STb[:, half * 512:(half + 1) * 512], bks[:, :], Bbks, BSTb)
                    GB = []
                    for g in range(2):
                        c0 = 0 if is_p else g * 64
                        if is_p or g == 0:
                            bufs = (Brhs, BEb, BELb, Bcbm, Bgat, Bgsq, Bgrs)
                        else:
                            bufs = sample_g1_bufs
                        GB.append(dict(
                            rh=rhsall[0:Q, :, c0:c0 + Q], E=Eb[0:Q, :, c0:c0 + Q], EL=ELb[:, :, c0:c0 + Q], cb=cbm[0:Q, c0:c0 + Q],
                            ga=gated[:, :, c0:c0 + Q], gs=gsq[:, :, c0:c0 + Q], gr=grs[:, c0:c0 + Q],
                            Brh=bufs[0], BE=bufs[1], BEL=bufs[2], Bcb=bufs[3], Bga=bufs[4], Bgs=bufs[5], Bgr=bufs[6]))

                    def s1(g, G_):
                        rh, Brh = G_["rh"], G_["Brh"]
                        S.op("dve", lambda e, g=g, Q=Q, rh=rh: e.tensor_tensor(out=rh, in0=trif[0:Q, 0:Q].unsqueeze(1).to_broadcast([Q, 8, Q]),
                                                                              in1=dtt[0:Q, 1, g * 8:(g + 1) * 8].unsqueeze(2).to_broadcast([Q, 8, Q]), op=ALU.mult),
                             reads=[Btrf, Bdtt], writes=[Brh])
                        bkA, BbkA = K.bank()
                        if Q == 128:
                            bkB, BbkB = K.bank()

                            def mlab(e, bkA=bkA, bkB=bkB, rh=rh):
                                e.matmul(bkA[:, :], lhsT=onesf, rhs=rh[:, 0:4, :], start=True, stop=True)
                                return e.matmul(bkB[:, :], lhsT=onesf, rhs=rh[:, 4:8, :], start=True, stop=True)
                            S.op("pe", mlab, reads=[Bonf, Brh], writes=[BbkA, BbkB])
                            G_["lab"] = [(bkA[:, :].rearrange("p (h t) -> p h t", h=4), 0, 4, BbkA), (bkB[:, :].rearrange("p (h t) -> p h t", h=4), 4, 8, BbkB)]
                        else:
                            S.op("pe", lambda e, bkA=bkA, rh=rh: e.matmul(bkA[:, 0:32], lhsT=onesf[0:4, :], rhs=rh, start=True, stop=True), reads=[Bonf, Brh], writes=[BbkA])
                            G_["lab"] = [(bkA[:, 0:32].rearrange("p (h t) -> p h t", h=8), 0, 8, BbkA)]
                        bkc, Bbkc = K.bank()
                        S.op("pe", lambda e, bkc=bkc, g=g, o=o, Q=Q: e.matmul(bkc[0:Q, 0:Q], lhsT=xc[:, 8 + g, o:o + Q], rhs=xc[:, 10 + g, o:o + Q], start=True, stop=True),
                             reads=[Bxc], writes=[Bbkc])
                        G_["bkc"] = (bkc, Bbkc)

                    def s2(g, G_):
                        rh, Brh, EL, BEL, cb, Bcb = G_["rh"], G_["Brh"], G_["EL"], G_["BEL"], G_["cb"], G_["Bcb"]
                        for (lb, h0, h1, Bb_) in G_["lab"]:
                            nh = h1 - h0
                            S.op("act", lambda e, lb=lb, h0=h0, h1=h1, EL=EL: e.activation(out=EL[:, h0:h1, :], in_=lb, func=AF.Exp), reads=[Bb_], writes=[BEL])
                            S.op("dve", lambda e, lb=lb, h0=h0, h1=h1, g=g, Q=Q, nh=nh, rh=rh: e.tensor_tensor(
                                out=rh[:, h0:h1, :], in0=lb[0:Q], in1=la_tm[0:Q, g * 8 + h0:g * 8 + h1].unsqueeze(2).to_broadcast([Q, nh, Q]), op=ALU.subtract),
                                reads=[Bb_, Blat], writes=[Brh])
                        S.op("dve", lambda e, rh=rh: e.tensor_scalar(out=rh, in0=rh, scalar1=0.0, scalar2=None, op0=ALU.min), reads=[Brh], writes=[Brh])
                        bkc, Bbkc = G_["bkc"]
                        S.op("dve", lambda e, bkc=bkc, Q=Q, cb=cb: e.tensor_tensor(out=cb, in0=bkc[0:Q, 0:Q], in1=trif[0:Q, 0:Q], op=ALU.mult), reads=[Bbkc, Btrf], writes=[Bcb])

                    def s3(g, G_):
                        rh, Brh, E, BE, EL, BEL, cb, Bcb = G_["rh"], G_["Brh"], G_["E"], G_["BE"], G_["EL"], G_["BEL"], G_["cb"], G_["Bcb"]
                        S.op("act", lambda e, E=E, rh=rh: e.activation(out=E, in_=rh, func=AF.Exp), reads=[Brh], writes=[BE])
                        S.op("dve", lambda e, g=g, o=o, Q=Q, EL=EL: e.tensor_tensor(out=EL, in0=EL, in1=xc[:, 10 + g, o:o + Q].unsqueeze(1).to_broadcast([128, 8, Q]), op=ALU.mult),
                             reads=[BEL, Bxc], writes=[BEL])
                        S.op("dve", lambda e, Q=Q, E=E, cb=cb: e.tensor_tensor(out=E, in0=E, in1=cb.unsqueeze(1).to_broadcast([Q, 8, Q]), op=ALU.mult),
                             reads=[BE, Bcb], writes=[BE])

                    def s4(g, G_):
                        E, BE, EL, BEL, ga, Bga, gs, Bgs = G_["E"], G_["BE"], G_["EL"], G_["BEL"], G_["ga"], G_["Bga"], G_["gs"], G_["Bgs"]
                        bky, Bbky = K.bank()
                        for jj in range(4):
                            j = g * 4 + jj

                            def my(e, bky=bky, jj=jj, j=j, Q=Q, first=first, E=E, EL=EL):
                                ins = None
                                for hh in range(2):
                                    hl = jj * 2 + hh
                                    h = j * 2 + hh
                                    outp = bky[hh * 64:(hh + 1) * 64, jj * Q:(jj + 1) * Q]
                                    e.matmul(outp, lhsT=xdt_tm[0:Q, h * 64:(h + 1) * 64], rhs=E[:, hl, :], start=True, stop=False)
                                    ins = e.matmul(outp, lhsT=xs_tm[0:Q, h * 64:(h + 1) * 64], rhs=dI[0:Q, h, 0:Q], start=False, stop=first)
                                    if not first:
                                        ins = e.matmul(outp, lhsT=STb[:, h * 64:(h + 1) * 64], rhs=EL[:, hl, :], start=False, stop=True)
                                return ins
                            S.op("pe", my, reads=[Bxdt, BE, Bxstm, BdI, BSTb, BEL], writes=[Bbky])
                        S.op("dve", lambda e, bky=bky, g=g, o=o, Q=Q, ga=ga: e.tensor_tensor(out=ga, in0=bky[:, 0:4 * Q].rearrange("p (j t) -> p j t", j=4),
                                                                                          in1=z_fm[:, g * 4:(g + 1) * 4, o:o + Q], op=ALU.mult), reads=[Bbky, Bz], writes=[Bga])
                        S.op("act", lambda e, gs=gs, ga=ga: e.activation(out=gs, in_=ga, func=AF.Square), reads=[Bga], writes=[Bgs])

                    def s5(g, G_):
                        ga, Bga, gs, Bgs, gr, Bgr = G_["ga"], G_["Bga"], G_["gs"], G_["Bgs"], G_["gr"], G_["Bgr"]
                        bkr, Bbkr = K.bank()
                        mm_group(bkr[:, 0:Q], [(onesb, gs[:, jj, :]) for jj in range(4)], Bbkr, [Bonb, Bgs])
                        S.op("act", lambda e, bkr=bkr, Q=Q, gr=gr: e.activation(out=gr, in_=bkr[:, 0:Q], func=AF.Sqrt, bias=EPS, scale=1.0 / 512.0), reads=[Bbkr], writes=[Bgr])
                        S.op("dve", lambda e, gr=gr: e.reciprocal(out=gr, in_=gr), reads=[Bgr], writes=[Bgr])
                        for jj in range(4):
                            j = g * 4 + jj
                            S.op("dve", lambda e, jj=jj, j=j, o=o, Q=Q, ga=ga, gr=gr: e.scalar_tensor_tensor(out=ycat[:, 8 + j, o:o + Q], in0=ga[:, jj, :], scalar=pcol[:, C_GN + j:C_GN + j + 1],
                                                                                                            in1=gr, op0=ALU.mult, op1=ALU.mult), reads=[Bga, Bpc, Bgr], writes=[Bycat])
                    stages = (s1, s2, s3, s4, s5)
                    if is_p:
                        for g in range(2):
                            for st_ in stages:
                                st_(g, GB[g])
                    else:
                        for st_ in stages:
                            for g in range(2):
                                st_(g, GB[g])
                    S.op("dve", lambda e, Q=Q: e.tensor_tensor(out=xw_tm[0:Q, :].rearrange("p (h d) -> p h d", h=16), in0=xdt_tm[0:Q, :].rearrange("p (h d) -> p h d", h=16),
                                                               in1=wend[0:Q, :].unsqueeze(2).to_broadcast([Q, 16, 64]), op=ALU.mult), reads=[Bxdt, Bwend], writes=[Bxw])
                    for half in range(2):
                        bku, Bbku = K.bank()

                        def mu(e, bku=bku, half=half, Q=Q):
                            ins = None
                            for jj in range(4):
                                j = half * 4 + jj
                                ins = e.matmul(bku[:, jj * 128:(jj + 1) * 128], lhsT=xw_tm[0:Q, j * 128:(j + 1) * 128], rhs=B_tm[0:Q, j // 4, :], start=True, stop=True)
                            return ins
                        S.op("pe", mu, reads=[Bxw, BBtm], writes=[Bbku])
                        for jj in range(4):
                            j = half * 4 + jj
                            if first:
                                S.op("dve", lambda e, bku=bku, jj=jj, j=j, Sc=Sc: e.tensor_copy(out=Sc[:, j, :], in_=bku[:, jj * 128:(jj + 1) * 128]), reads=[Bbku], writes=[BSc])
                            else:
                                S.op("dve", lambda e, bku=bku, jj=jj, j=j, Sc=Sc: e.scalar_tensor_tensor(out=Sc[:, j, :], in0=Sc[:, j, :], scalar=elc[:, j:j + 1], in1=bku[:, jj * 128:(jj + 1) * 128],
                                                                                                        op0=ALU.mult, op1=ALU.add), reads=[BSc, Belc, Bbku], writes=[BSc])
                    if not is_p:
                        S.dma("sp", lambda e, Sc=Sc, b=b: [e.dma_start(out=ssms_d[b * 1024:(b + 1) * 1024, :].rearrange("(j q) n -> q j n", q=128), in_=Sc)], BSc, reads=[BSc])
                        outbufs.append(BSc)
                    elif t0 + o + Q == NP:
                        S.dma("sp", lambda e, Sc=Sc: [e.dma_start(out=ssmp_d.rearrange("(j q) n -> q j n", q=128), in_=Sc)], BSc, reads=[BSc])
                        outbufs.append(BSc)

                if not is_p:
                    for g in range(8):
                        bk, Bbk = K.bank()
                        pairs = [(v_tm[0:64, 0, g * 128:(g + 1) * 128], wbs[:, g, :]), (onesr[0:1, 0:128], bsps[0:1, g * 64:(g + 1) * 64])]
                        mm_group(bk[:, 0:64], pairs, Bbk, [Bv, Bwbs, Bonesr, Bbsps])
                        S.op("dve", lambda e, bk=bk, g=g: e.tensor_tensor(out=ycat[:, g, 0:64], in0=bk[:, 0:64], in1=u_fm[:, g, 0:64], op=ALU.mult),
                             reads=[Bbk, Bu], writes=[Bycat])
                    S.dma("sp", lambda e: [e.dma_start(out=vs_d, in_=v_f32)], Bvf, reads=[Bvf])
                    outbufs.append(Bvf)
                if (t0 + W == NP) or not is_p:
                    n = 3 if is_p else 48
                    dst = scvp_d if is_p else scvs_d
                    for j4 in range(3):
                        bk, Bbk = K.bank()

                        def trl(e, bk=bk, j4=j4, n=n):
                            ins = None
                            for jj in range(4):
                                ins = e.transpose(bk[0:n, jj * 128:(jj + 1) * 128], xlast[:, j4 * 4 + jj, 0:n], identf)
                            return ins
                        S.op("pe", trl, reads=[Bxl, Bidf], writes=[Bbk])
                        S.op("dve", lambda e, bk=bk, n=n: e.tensor_copy(out=xlo[0:n, :], in_=bk[0:n, :]), reads=[Bbk], writes=[Bxlo])
                        S.dma("sp", lambda e, n=n, dst=dst, j4=j4: [e.dma_start(out=dst[:, j4 * 512:(j4 + 1) * 512], in_=xlo[0:n, :])], Bxlo, reads=[Bxlo])
                    outbufs.append(Bxlo)

                for oc in range(8):
                    wo, Bwo = wobufs[woi[0] % 2]
                    woi[0] += 1
                    S.dma("sp", lambda e, wo=wo, oc=oc: [e.dma_start(out=wo, in_=wview(w_out_ab_b, 0, 2048, oc * 128, 128))], Bwo, reads=[Bwoutb[oc]], writes=[Bwo])
                    bk, Bbk = K.bank()
                    mm_group(bk[:, 0:W], [(wo[:, kc, :], ycat[:, kc, 0:W]) for kc in range(16)], Bbk, [Bwo, Bycat])
                    add_to_x(oc, t0, W, bk, Bbk)
            return dict(v_f32=(v_f32, Bvf), xlast=(xlast, Bxl))

        if stop_after >= 1:
            p1 = phase1()
        elif dbg:
            dump_x()

        if dbg and stop_after == 1:
            dump_x()

        def phase_attn(l):
            S.barrier()
            K.off = PERSIST
            hn, Bhn = K.alloc("hn", [128, 8, 512], BF16)
            sq, Bsq = K.alloc("sq", [128, 8, 512], BF16)
            rstd, Brstd = K.alloc("rstd", [128, 512], F32)
            wq, Bwq = K.alloc("wq", [128, 8, 1024], BF16)
            wo, Bwo = K.alloc("wo", [128, 8, 1024], BF16)
            wkv = [K.alloc("wkv%d" % i, [128, 8, 512], BF16) for i in range(2)]
            Kfm, BKfm = K.alloc("Kfm", [128, 8, 256], BF16)
            Vb, BVb = K.alloc("Vb", [128, 2, 1024], BF16)
            rden2 = [K.alloc("rden%d" % i, [128, 512], F32) for i in range(2)]
            o_fm, Bo = K.alloc("o_fm", [128, 8, 512], BF16)
            _ov = K.off
            kvo = [K.alloc("kvo%d" % i, [128, 2, 1024], F32) for i in range(2)]
            mi, Bmi = K.alloc("mi", [128, 2, 1024], F32)
            _ov_end = K.off
            K.off = _ov
            q_fm, Bq = K.alloc("q_fm", [128, 8, 512], BF16)
            ET = [K.alloc("ET%d" % i, [128, 2, 512], BF16) for i in range(2)]
            Ks = [K.alloc("Ks%d" % i, [128, 2, 1024], BF16) for i in range(2)]
            Vs = [K.alloc("Vs%d" % i, [128, 2, 1024], BF16) for i in range(2)]
            K.off = max(K.off, _ov_end)
            Kfs2 = [K.alloc("Kfs%d" % i, [128, 8, 256], BF16) for i in range(2)]
            ETs2 = [K.alloc("ETs%d" % i, [128, 32], BF16) for i in range(2)]
            rds2 = [K.alloc("rds%d" % i, [128, 16], F32) for i in range(2)]
            memT, BmemT = K.alloc("memT", [128, 8, 256], BF16)
            print("attn SBUF bytes/partition:", K.off)
            S.dma("sp", lambda e: [e.dma_start(out=mi, in_=memp_d.rearrange("(c p) f -> p c f", p=128))], Bmi, writes=[Bmi])
            for kc2 in range(4):
                bk, Bbk = K.bank()

                def trm(e, bk=bk, kc2=kc2):
                    ins = None
                    for kk in range(2):
                        for mc in range(2):
                            kc = kc2 * 2 + kk
                            ins = e.transpose(bk[:, kk * 256 + mc * 128: kk * 256 + mc * 128 + 128], mi[:, mc, kc * 128:(kc + 1) * 128], identf)
                    return ins
                S.op("pe", trm, reads=[Bmi, Bidf], writes=[Bbk])
                evac_copy(memT[:, kc2 * 2:kc2 * 2 + 2, :], bk[:, :].rearrange("p (k m) -> p k m", k=2), Bbk, BmemT)

            SC = 1.0 / 16.0
            wload(wq, Bwq, wview(w_mem_q, l * 1024, 1024, 0, 1024))
            for which in range(2):
                wsrc = w_mem_k if which == 0 else w_mem_v
                dstd = mkp_d if which == 0 else mvp_d
                ko, Bko = kvo[which]
                for cb in range(2):
                    wt, Bwt = wkv[cb]
                    wload(wt, Bwt, wview(wsrc, l * 1024, 1024, cb * 512, 512))
                    for mc in range(2):
                        bk, Bbk = K.bank()
                        mm_group(bk[:, :], [(memT[:, kc, mc * 128:(mc + 1) * 128], wt[:, kc, :]) for kc in range(8)], Bbk, [BmemT, Bwt])
                        evac_copy(ko[:, mc, cb * 512:(cb + 1) * 512], bk[:, :], Bbk, Bko)
                        if which == 1:
                            S.op("act", lambda e, bk=bk, mc=mc, cb=cb: e.activation(out=Vb[:, mc, cb * 512:(cb + 1) * 512], in_=bk[:, :], func=AF.Copy), reads=[Bbk], writes=[BVb])
                    if which == 0:
                        for oc in range(4):
                            bk, Bbk = K.bank()
                            mm_group(bk[:, 0:256], [(wt[:, kc, oc * 128:(oc + 1) * 128], memT[:, kc, :]) for kc in range(8)], Bbk, [BmemT, Bwt])
                            evac_copy(Kfm[:, cb * 4 + oc, :], bk[:, 0:256], Bbk, BKfm)
                S.dma("sp", lambda e, ko=ko, dstd=dstd: [e.dma_start(out=dstd[l * 256:(l + 1) * 256, :].rearrange("(mc p) d -> p mc d", p=128), in_=ko)], Bko, reads=[Bko])
                outbufs.append(Bko)
            wload(wo, Bwo, wview(w_mem_o, l * 1024, 1024, 0, 1024))
            S.barrier()

            for (t0, W) in TILES512:
                is_p = t0 < NP
                rmsnorm(t0, W, C_GMEM + 8 * l, hn, Bhn, sq, Bsq, rstd, Brstd)
                for oc in range(8):
                    bk, Bbk = K.bank()
                    mm_group(bk[:, 0:W], [(wq[:, kc, oc * 128:(oc + 1) * 128], hn[:, kc, 0:W]) for kc in range(8)], Bbk, [Bwq, Bhn])
                    evac_copy(q_fm[:, oc, 0:W], bk[:, 0:W], Bbk, Bq)
                if is_p:
                    def scores(h):
                        Et, BEt = ET[h % 2]
                        for mc in range(2):
                            bk, Bbk = K.bank()
                            mm_group(bk[:, 0:W], [(Kfm[:, 2 * h + ec, mc * 128:(mc + 1) * 128], q_fm[:, 2 * h + ec, 0:W]) for ec in range(2)], Bbk, [BKfm, Bq])
                            S.op("act", lambda e, bk=bk, Et=Et, mc=mc: e.activation(out=Et[:, mc, 0:W], in_=bk[:, 0:W], func=AF.Exp, scale=SC), reads=[Bbk], writes=[BEt])
                    scores(0)
                    for h in range(4):
                        Et, BEt = ET[h % 2]
                        rden, Brden = rden2[h % 2]
                        if h + 1 < 4:
                            scores(h + 1)
                        bk, Bbk = K.bank()
                        mm_group(bk[:, 0:W], [(onesb, Et[:, mc, 0:W]) for mc in range(2)], Bbk, [Bonb, BEt])
                        S.op("dve", lambda e, bk=bk: e.reciprocal(out=rden[:, 0:W], in_=bk[:, 0:W]), reads=[Bbk], writes=[Brden])
                        for ec in range(2):
                            bk, Bbk = K.bank()
                            mm_group(bk[:, 0:W], [(Vb[:, mc, (2 * h + ec) * 128:(2 * h + ec + 1) * 128], Et[:, mc, 0:W]) for mc in range(2)], Bbk, [BVb, BEt])
                            S.op("dve", lambda e, bk=bk, h=h, ec=ec: e.tensor_tensor(out=o_fm[:, 2 * h + ec, 0:W], in0=bk[:, 0:W], in1=rden[:, 0:W], op=ALU.mult),
                                 reads=[Bbk, Brden], writes=[Bo])
                else:
                    def prep(b):
                        Kb, BKb = Ks[b % 2]
                        Vs_, BVs = Vs[b % 2]
                        Kfs, BKfs = Kfs2[b % 2]
                        r0 = (l * 16 + b) * 256
                        wload(Kb, BKb, ck_d[r0:r0 + 256, :].rearrange("(mc p) d -> p mc d", p=128))
                        wload(Vs_, BVs, cv_d[r0:r0 + 256, :].rearrange("(mc p) d -> p mc d", p=128))
                        for kc4 in range(2):
                            bk, Bbk = K.bank()
                            bkb = bk[:, :].bitcast(BF16)

                            def trk(e, bkb=bkb, Kb=Kb, kc4=kc4):
                                ins = None
                                for kk in range(4):
                                    for mc in range(2):
                                        kc = kc4 * 4 + kk
                                        ins = e.transpose(bkb[:, kk * 256 + mc * 128:kk * 256 + mc * 128 + 128], Kb[:, mc, kc * 128:(kc + 1) * 128], identb)
                                return ins
                            S.op("pe", trk, reads=[BKb, Bidb], writes=[Bbk])
                            evac_copy(Kfs[:, kc4 * 4:(kc4 + 1) * 4, :], bkb.rearrange("p (k m) -> p k m", k=4), Bbk, BKfs)

                    def score(b):
                        Kfs, BKfs = Kfs2[b % 2]
                        ETs, BETs = ETs2[b % 2]
                        bk, Bbk = K.bank()

                        def msc(e, bk=bk, b=b, Kfs=Kfs):
                            ins = None
                            for h in range(4):
                                for mc in range(2):
                                    c0 = (h * 2 + mc) * 4
                                    for ec in range(2):
                                        ins = e.matmul(bk[:, c0:c0 + 4], lhsT=Kfs[:, 2 * h + ec, mc * 128:(mc + 1) * 128], rhs=q_fm[:, 2 * h + ec, b * 4:b * 4 + 4],
                                                       start=(ec == 0), stop=(ec == 1))
                            return ins
                        S.op("pe", msc, reads=[BKfs, Bq], writes=[Bbk])
                        S.op("act", lambda e, bk=bk, ETs=ETs: e.activation(out=ETs, in_=bk[:, 0:32], func=AF.Exp, scale=SC), reads=[Bbk], writes=[BETs])

                    def pv(b):
                        Vs_, BVs = Vs[b % 2]
                        ETs, BETs = ETs2[b % 2]
                        rds, Brds = rds2[b % 2]
                        bk2, Bbk2 = K.bank()

                        def mpv(e, bk2=bk2, Vs_=Vs_, ETs=ETs):
                            ins = None
                            for h in range(4):
                                for mc in range(2):
                                    c0 = (h * 2 + mc) * 4
                                    ins = e.matmul(bk2[:, h * 4:h * 4 + 4], lhsT=onesb, rhs=ETs[:, c0:c0 + 4], start=(mc == 0), stop=(mc == 1))
                                for ec in range(2):
                                    for mc in range(2):
                                        c0 = (h * 2 + mc) * 4
                                        o0 = 16 + (2 * h + ec) * 4
                                        ins = e.matmul(bk2[:, o0:o0 + 4], lhsT=Vs_[:, mc, (2 * h + ec) * 128:(2 * h + ec + 1) * 128], rhs=ETs[:, c0:c0 + 4],
                                                       start=(mc == 0), stop=(mc == 1))
                            return ins
                        S.op("pe", mpv, reads=[Bonb, BETs, BVs], writes=[Bbk2])
                        S.op("dve", lambda e, bk2=bk2, rds=rds: e.reciprocal(out=rds, in_=bk2[:, 0:16]), reads=[Bbk2], writes=[Brds])
                        S.op("dve", lambda e, bk2=bk2, b=b, rds=rds: e.tensor_tensor(out=o_fm[:, :, b * 4:b * 4 + 4].rearrange("p (h c) l -> p h c l", h=4),
                                                                                     in0=bk2[:, 16:48].rearrange("p (h c l) -> p h c l", h=4, c=2),
                                                                                     in1=rds.rearrange("p (h l) -> p h l", h=4).unsqueeze(2).to_broadcast([128, 4, 2, 4]), op=ALU.mult),
                             reads=[Bbk2, Brds], writes=[Bo])

                    prep(0)
                    for b in range(16):
                        if b >= 1:
                            pv(b - 1)
                        if b + 1 < 16:
                            prep(b + 1)
                        score(b)
                    pv(15)
                for oc in range(8):
                    bk, Bbk = K.bank()
                    mm_group(bk[:, 0:W], [(wo[:, kc, oc * 128:(oc + 1) * 128], o_fm[:, kc, 0:W]) for kc in range(8)], Bbk, [Bwo, Bo])
                    add_to_x(oc, t0, W, bk, Bbk)

        def phase_ffn(moe):
            S.barrier()
            K.off = PERSIST
            hn, Bhn = K.alloc("hnall", [128, 8, NT], BF16)
            G = 4
            wg = [K.alloc("wg%d" % i, [128, 8, G * 128], BF16) for i in range(2)]
            wu = [K.alloc("wu%d" % i, [128, 8, G * 128], BF16) for i in range(2)]
            wd = [K.alloc("wd%d" % i, [128, G, 1024], BF16) for i in range(2)]
            sg = [K.alloc("sg%d" % i, [128, 512], BF16) for i in range(2)]
            hh = [K.alloc("hh%d" % i, [128, G, 512], BF16) for i in range(2)]
            sq, Bsq = K.alloc("sq", [128, 8, 512], BF16)
            rstd, Brstd = K.alloc("rstd", [128, 512], F32)
            hnf = Bhnf = None
            if moe:
                wr, Bwr = K.alloc("wr", [128, 8, 8], F32)
                comb, Bcomb = K.alloc("comb", [128, 17, 8], F32)
                lg3, Blg = K.alloc("lg3", [128, 4, 8], F32)
                m13, Bm1 = K.alloc("m13", [128, 5, 4], F32)
                mk13, Bmk1 = K.alloc("mk13", [128, 4, 8], F32)
                mk23, Bmk2 = K.alloc("mk23", [128, 4, 8], F32)
                l23, Bl2 = K.alloc("l23", [128, 4, 8], F32)
                combT, BcombT = K.alloc("combT", [8, NT], BF16)
                selb, Bselb = K.alloc("selb", [8, 8, 128], BF16)
                cbc, Bcbc = K.alloc("cbc", [128, NT], BF16)
                hnf, Bhnf = K.alloc("hnf", [128, 8, 512], F32)
                S.dma("sp", lambda e: [e.dma_start(out=wr, in_=w_router.rearrange("(kc p) n -> p kc n", p=128))], Bwr, writes=[Bwr])
                S.dma("pool", lambda e: [e.dma_start(out=selb, in_=sel_d.rearrange("a (b p) -> a b p", b=8))], Bselb, writes=[Bselb])
            print("ffn SBUF bytes/partition:", K.off)
            gcol = C_GFFN + (8 if moe else 0)
            for (t0, W) in TILES512:
                rmsnorm(t0, W, gcol, hn[:, :, t0:t0 + W], Bhn, sq, Bsq, rstd, Brstd, hnf, Bhnf)
                if moe:
                    nb = (W + 127) // 128
                    rows = min(128, W)
                    blk0 = t0 // 128
                    bk, Bbk = K.bank()

                    def mrt(e, bk=bk, nb=nb, rows=rows):
                        ins = None
                        for c in range(nb):
                            for kc in range(8):
                                ins = e.matmul(bk[0:rows, c * 8:(c + 1) * 8], lhsT=hnf[:, kc, c * 128:c * 128 + rows], rhs=wr[:, kc, :], start=(kc == 0), stop=(kc == 7))
                        return ins
                    S.op("pe", mrt, reads=[Bhnf, Bwr], writes=[Bbk])
                    L3 = lg3[0:rows, 0:nb, :]
                    M1 = mk13[0:rows, 0:nb, :]
                    M2 = mk23[0:rows, 0:nb, :]
                    L2 = l23[0:rows, 0:nb, :]
                    mx = lambda i: m13[0:rows, i, 0:nb]
                    mxb = lambda i: m13[0:rows, i, 0:nb].unsqueeze(2).to_broadcast([rows, nb, 8])
                    S.op("dve", lambda e, bk=bk, L3=L3, rows=rows, nb=nb: e.tensor_copy(out=L3, in_=bk[0:rows, 0:nb * 8].rearrange("p (c e) -> p c e", c=nb)), reads=[Bbk], writes=[Blg])
                    S.op("dve", lambda e, L3=L3, o_=mx(0): e.tensor_reduce(out=o_, in_=L3, axis=AX.X, op=ALU.max), reads=[Blg], writes=[Bm1])
                    S.op("dve", lambda e, L3=L3, M1=M1, b_=mxb(0): e.tensor_tensor(out=M1, in0=L3, in1=b_, op=ALU.is_equal), reads=[Blg, Bm1], writes=[Bmk1])
                    S.op("dve", lambda e, L3=L3, M1=M1, L2=L2: e.scalar_tensor_tensor(out=L2, in0=M1, scalar=-1e30, in1=L3, op0=ALU.mult, op1=ALU.add), reads=[Bmk1, Blg], writes=[Bl2])
                    S.op("dve", lambda e, L2=L2, o_=mx(1): e.tensor_reduce(out=o_, in_=L2, axis=AX.X, op=ALU.max), reads=[Bl2], writes=[Bm1])
                    S.op("dve", lambda e, L2=L2, M2=M2, b_=mxb(1): e.tensor_tensor(out=M2, in0=L2, in1=b_, op=ALU.is_equal), reads=[Bl2, Bm1], writes=[Bmk2])
                    S.op("dve", lambda e, a_=mx(2), b_=mx(1), c_=mx(0): e.tensor_tensor(out=a_, in0=b_, in1=c_, op=ALU.subtract), reads=[Bm1], writes=[Bm1])
                    S.op("act", lambda e, a_=mx(3), b_=mx(2): e.activation(out=a_, in_=b_, func=AF.Sigmoid), reads=[Bm1], writes=[Bm1])
                    S.op("dve", lambda e, a_=mx(4), b_=mx(3): e.tensor_scalar(out=a_, in0=b_, scalar1=-1.0, scalar2=1.0, op0=ALU.mult, op1=ALU.add), reads=[Bm1], writes=[Bm1])
                    S.op("dve", lambda e, M1=M1, b_=mxb(4): e.tensor_tensor(out=M1, in0=M1, in1=b_, op=ALU.mult), reads=[Bmk1, Bm1], writes=[Bmk1])
                    S.op("dve", lambda e, M2=M2, b_=mxb(3): e.tensor_tensor(out=M2, in0=M2, in1=b_, op=ALU.mult), reads=[Bmk2, Bm1], writes=[Bmk2])
                    S.op("dve", lambda e, M1=M1, M2=M2, blk0=blk0, nb=nb, rows=rows: e.tensor_tensor(out=comb[0:rows, blk0:blk0 + nb, :], in0=M1, in1=M2, op=ALU.add),
                         reads=[Bmk1, Bmk2], writes=[Bcomb])
                    bkt, Bbkt = K.bank()

                    def trt(e, bkt=bkt, blk0=blk0, nb=nb, rows=rows):
                        ins = None
                        for c in range(nb):
                            ins = e.transpose(bkt[0:8, c * 128:c * 128 + rows], comb[0:rows, blk0 + c, :], identf[0:rows, 0:rows])
                        return ins
                    S.op("pe", trt, reads=[Bcomb, Bidf], writes=[Bbkt])
                    S.op("act", lambda e, bkt=bkt, t0=t0, W=W: e.activation(out=combT[:, t0:t0 + W], in_=bkt[0:8, 0:W], func=AF.Copy), reads=[Bbkt], writes=[BcombT])
            nexp = NEXP if moe else 1
            blocks = [(s0, min(G, 22 - s0)) for s0 in range(0, 22, G)]
            wi = 0
            hcnt = [0]
            pend = []
            for ex in range(nexp):
                if moe:
                    wgd, wud, wdd = w_exp_gate, w_exp_up, w_exp_down
                    rg0, rd0 = ex * 1024, ex * DFF
                    for (t0, W) in TILES512:
                        bk, Bbk = K.bank()
                        S.op("pe", lambda e, bk=bk, ex=ex, t0=t0, W=W: e.matmul(bk[:, 0:W], lhsT=selb[:, ex, :], rhs=combT[:, t0:t0 + W], start=True, stop=True),
                             reads=[Bselb, BcombT], writes=[Bbk])
                        S.op("act", lambda e, bk=bk, t0=t0, W=W: e.activation(out=cbc[:, t0:t0 + W], in_=bk[:, 0:W], func=AF.Copy), reads=[Bbk], writes=[Bcbc])
                else:
                    wgd, wud, wdd = w_ffn_gate, w_ffn_up, w_ffn_down
                    rg0, rd0 = 0, 0
                for (s0, ns) in blocks:
                    wgt, Bwg = wg[wi % 2]
                    wut, Bwu = wu[wi % 2]
                    wdt_, Bwd = wd[wi % 2]
                    wi += 1
                    wload(wgt[:, :, 0:ns * 128], Bwg, wview(wgd, rg0, 1024, s0 * 128, ns * 128))
                    wload(wut[:, :, 0:ns * 128], Bwu, wview(wud, rg0, 1024, s0 * 128, ns * 128))
                    wload(wdt_[:, 0:ns, :], Bwd, wview(wdd, rd0 + s0 * 128, ns * 128, 0, 1024))
                    for ti, (t0, W) in enumerate(TILES_E):
                        hht, Bhh = hh[hcnt[0] % 2]
                        hcnt[0] += 1
                        for sl in range(ns):
                            sgt, Bsg = sg[sl % 2]
                            bkg, Bbkg = K.bank()
                            mm_group(bkg[:, 0:W], [(wgt[:, kc, sl * 128:(sl + 1) * 128], hn[:, kc, t0:t0 + W]) for kc in range(8)], Bbkg, [Bwg, Bhn])
                            bku, Bbku = K.bank()
                            mm_group(bku[:, 0:W], [(wut[:, kc, sl * 128:(sl + 1) * 128], hn[:, kc, t0:t0 + W]) for kc in range(8)], Bbku, [Bwu, Bhn])
                            S.op("act", lambda e, bkg=bkg, sgt=sgt, W=W: e.activation(out=sgt[:, 0:W], in_=bkg[:, 0:W], func=AF.Silu), reads=[Bbkg], writes=[Bsg])
                            S.op("dve", lambda e, bku=bku, sgt=sgt, hht=hht, sl=sl, W=W: e.tensor_tensor(out=hht[:, sl, 0:W], in0=bku[:, 0:W], in1=sgt[:, 0:W], op=ALU.mult),
                                 reads=[Bbku, Bsg], writes=[Bhh])
                            if moe:
                                S.op("dve", lambda e, hht=hht, sl=sl, t0=t0, W=W: e.tensor_tensor(out=hht[:, sl, 0:W], in0=hht[:, sl, 0:W], in1=cbc[:, t0:t0 + W], op=ALU.mult),
                                     reads=[Bhh, Bcbc], writes=[Bhh])
                        def down(wdt_=wdt_, Bwd=Bwd, hht=hht, Bhh=Bhh, ns=ns, t0=t0, W=W):
                            for oc in range(8):
                                bk, Bbk = K.bank()
                                mm_group(bk[:, 0:W], [(wdt_[:, sl, oc * 128:(oc + 1) * 128], hht[:, sl, 0:W]) for sl in range(ns)], Bbk, [Bwd, Bhh])
                                add_to_x(oc, t0, W, bk, Bbk)
                        if pend:
                            pend.pop()()
                        pend.append(down)
            if pend:
                pend.pop()()

        def phase_mixc():
            S.barrier()
            K.off = PERSIST
            hn, Bhn = K.alloc("hn", [128, 8, 512], BF16)
            sq, Bsq = K.alloc("sq", [128, 8, 512], BF16)
            rstd, Brstd = K.alloc("rstd", [128, 512], F32)
            wic, Bwic = K.alloc("wic", [128, 8, 3072], BF16)
            woc, Bwoc = K.alloc("woc", [128, 8, 1024], BF16)
            diagc, Bdiagc = K.alloc("diagc", [128, 24, 128], BF16)
            chb, Bchb = K.alloc("chb", [128, 8, 2 + 512], BF16)
            chs, Bchs = K.alloc("chs", [128, 8, 16, 6], BF16)
            chl, Bchl = K.alloc("chl", [128, 8, 32], F32)
            cgs2 = [K.alloc("cgs%d" % i, [128, 512], F32) for i in range(2)]
            ysb2 = [K.alloc("ysb%d" % i, [128, 512], F32) for i in range(2)]
            yb, Byb = K.alloc("yb", [128, 8, 512], BF16)
            sci, Bsci = K.alloc("sci", [32, 1024], F32)
            clo, Bclo = K.alloc("clo", [32, 1024], F32)
            print("mixc SBUF bytes/partition:", K.off)
            for cb in range(6):
                wload(wic[:, :, cb * 512:(cb + 1) * 512], Bwic, wview(w_in_c, 0, 1024, cb * 512, 512))
            wload(woc, Bwoc, wview(w_out_c, 0, 1024, 0, 1024))
            for k in range(3):
                for j in range(8):
                    idx = k * 8 + j
                    S.op("dve", lambda e, idx=idx: e.tensor_scalar(out=diagc[:, idx, :], in0=identf, scalar1=pcol[:, C_CWC + idx:C_CWC + idx + 1], scalar2=None, op0=ALU.mult),
                         reads=[Bidf, Bpc], writes=[Bdiagc])
            S.op("dve", lambda e: e.memset(chb[:, :, 0:2], 0.0), writes=[Bchb])
            S.dma("sp", lambda e: [e.dma_start(out=sci, in_=ssc_d)], Bsci, writes=[Bsci])
            for j4 in range(2):
                bk, Bbk = K.bank()

                def trc(e, bk=bk, j4=j4):
                    ins = None
                    for jj in range(4):
                        j = j4 * 4 + jj
                        ins = e.transpose(bk[:, jj * 32:(jj + 1) * 32], sci[:, j * 128:(j + 1) * 128], identf[0:32, 0:32])
                    return ins
                S.op("pe", trc, reads=[Bsci, Bidf], writes=[Bbk])
                evac_copy(chs[:, j4 * 4:(j4 + 1) * 4, :, 0:2], bk[:, 0:128].rearrange("p (j b k) -> p j b k", j=4, b=16), Bbk, Bchs)
            for (t0, W) in TILES512:
                is_p = t0 < NP
                rmsnorm(t0, W, C_GMIX + 8, hn, Bhn, sq, Bsq, rstd, Brstd)
                ptail = []
                for j in range(8):
                    cgs, Bcgs = cgs2[j % 2]
                    ysb, Bysb = ysb2[j % 2]
                    bkb, Bbkb = K.bank()
                    mm_group(bkb[:, 0:W], [(wic[:, kc, j * 128:(j + 1) * 128], hn[:, kc, 0:W]) for kc in range(8)], Bbkb, [Bwic, Bhn])
                    S.op("act", lambda e, bkb=bkb, W=W: e.activation(out=ysb[:, 0:W], in_=bkb[:, 0:W], func=AF.Copy), reads=[Bbkb], writes=[Bysb])
                    bkc, Bbkc = K.bank()
                    mm_group(bkc[:, 0:W], [(wic[:, kc, 1024 + j * 128:1024 + (j + 1) * 128], hn[:, kc, 0:W]) for kc in range(8)], Bbkc, [Bwic, Bhn])
                    bkh, Bbkh = K.bank()
                    mm_group(bkh[:, 0:W], [(wic[:, kc, 2048 + j * 128:2048 + (j + 1) * 128], hn[:, kc, 0:W]) for kc in range(8)], Bbkh, [Bwic, Bhn])
                    S.op("act", lambda e, bkc=bkc, W=W: e.activation(out=cgs[:, 0:W], in_=bkc[:, 0:W], func=AF.Copy), reads=[Bbkc], writes=[Bcgs])
                    if is_p:
                        S.op("dve", lambda e, bkh=bkh, j=j, W=W: e.tensor_tensor(out=chb[:, j, 2:2 + W], in0=bkh[:, 0:W], in1=cgs[:, 0:W], op=ALU.mult), reads=[Bbkh, Bcgs], writes=[Bchb])
                        if t0 + W == NP:
                            S.op("dve", lambda e, bkh=bkh, j=j, W=W: e.tensor_tensor(out=chl[:, j, 0:2], in0=bkh[:, W - 2:W], in1=cgs[:, W - 2:W], op=ALU.mult), reads=[Bbkh, Bcgs], writes=[Bchl])
                        pairs = [(diagc[:, k * 8 + j, :], chb[:, j, k:k + W]) for k in range(3)]
                        rb = Bchb
                    else:
                        S.op("dve", lambda e, bkh=bkh, j=j: e.tensor_tensor(out=chs[:, j, :, 2:6], in0=bkh[:, 0:64].rearrange("p (b l) -> p b l", b=16),
                                                                           in1=cgs[:, 0:64].rearrange("p (b l) -> p b l", b=16), op=ALU.mult), reads=[Bbkh, Bcgs], writes=[Bchs])
                        S.op("dve", lambda e, bkh=bkh, j=j: e.tensor_tensor(out=chl[:, j, :].rearrange("p (b k) -> p b k", b=16), in0=bkh[:, 0:64].rearrange("p (b l) -> p b l", b=16)[:, :, 2:4],
                                                                           in1=cgs[:, 0:64].rearrange("p (b l) -> p b l", b=16)[:, :, 2:4], op=ALU.mult), reads=[Bbkh, Bcgs], writes=[Bchl])
                        pairs = [(diagc[:, k * 8 + j, :], chs[:, j, :, k:k + 4]) for k in range(3)]
                        rb = Bchs
                    def tail(pairs=pairs, rb=rb, ysb=ysb, Bysb=Bysb, bkb=bkb, Bbkb=Bbkb, j=j, W=W):
                        bky, Bbky = K.bank()
                        mm_group(bky[:, 0:W], pairs, Bbky, [Bdiagc, rb])
                        S.op("dve", lambda e, bky=bky, j=j, W=W: e.tensor_tensor(out=yb[:, j, 0:W], in0=bky[:, 0:W], in1=ysb[:, 0:W], op=ALU.mult), reads=[Bbky, Bysb], writes=[Byb])
                    if ptail:
                        ptail.pop()()
                    ptail.append(tail)
                if ptail:
                    ptail.pop()()
                if is_p:
                    S.op("dve", lambda e, W=W: e.tensor_copy(out=chb[:, :, 0:2], in_=chb[:, :, W:W + 2]), reads=[Bchb], writes=[Bchb])
                if (t0 + W == NP) or not is_p:
                    n = 2 if is_p else 32
                    dst = sccp_d if is_p else sccs_d
                    for j4 in range(2):
                        bk, Bbk = K.bank()

                        def trl(e, bk=bk, j4=j4, n=n):
                            ins = None
                            for jj in range(4):
                                ins = e.transpose(bk[0:n, jj * 128:(jj + 1) * 128], chl[:, j4 * 4 + jj, 0:n], identf)
                            return ins
                        S.op("pe", trl, reads=[Bchl, Bidf], writes=[Bbk])
                        S.op("dve", lambda e, bk=bk, j4=j4, n=n: e.tensor_copy(out=clo[0:n, j4 * 512:(j4 + 1) * 512], in_=bk[0:n, :]), reads=[Bbk], writes=[Bclo])
                    S.dma("sp", lambda e, n=n, dst=dst: [e.dma_start(out=dst, in_=clo[0:n, :])], Bclo, reads=[Bclo])
                    outbufs.append(Bclo)
                for oc in range(8):
                    bk, Bbk = K.bank()
                    mm_group(bk[:, 0:W], [(woc[:, kc, oc * 128:(oc + 1) * 128], yb[:, kc, 0:W]) for kc in range(8)], Bbk, [Bwoc, Byb])
                    add_to_x(oc, t0, W, bk, Bbk)

        def phase_final():
            S.barrier()
            K.off = PERSIST
            hnf, Bhnf = K.alloc("hnff", [128, 8, 512], F32)
            hnb, Bhnb = K.alloc("hnfb", [128, 8, 512], BF16)
            sq, Bsq = K.alloc("sq", [128, 8, 512], BF16)
            rstd, Brstd = K.alloc("rstd", [128, 512], F32)
            yo = [K.alloc("yo%d" % i, [128, 4, 1024], F32) for i in range(2)]
            for ti, (t0, W) in enumerate(TILES512):
                rmsnorm(t0, W, C_GFIN, None, None, sq, Bsq, rstd, Brstd, hnf, Bhnf)
                yt, Byt = yo[ti % 2]
                nc128 = (W + 127) // 128
                for c in range(nc128):
                    rows = min(128, W - c * 128)
                    for half in range(2):
                        bk, Bbk = K.bank()

                        def tro(e, bk=bk, c=c, half=half, rows=rows):
                            ins = None
                            for kk in range(4):
                                kc = half * 4 + kk
                                ins = e.transpose(bk[0:rows, kk * 128:(kk + 1) * 128], hnf[:, kc, c * 128:c * 128 + rows], identf)
                            return ins
                        S.op("pe", tro, reads=[Bhnf, Bidf], writes=[Bbk])
                        evac_copy(yt[0:rows, c, half * 512:(half + 1) * 512], bk[0:rows, :], Bbk, Byt)
                if t0 < NP:
                    S.dma("sp", lambda e, yt=yt, t0=t0: [e.dma_start(out=yp_d[t0:t0 + 512, :].rearrange("(c p) f -> p c f", p=128), in_=yt)], Byt, reads=[Byt])
                else:
                    S.dma("sp", lambda e, yt=yt: [e.dma_start(out=ys_d, in_=yt[0:64, 0, :])], Byt, reads=[Byt])
                outbufs.append(Byt)

        seq = [("attn0", lambda: phase_attn(0)), ("ffn0", lambda: phase_ffn(False)), ("mixc", phase_mixc),
               ("attn1", lambda: phase_attn(1)), ("moe", lambda: phase_ffn(True)), ("final", phase_final)]
        for i, (nm, f) in enumerate(seq):
            if stop_after >= i + 2:
                f()
                if dbg and stop_after == i + 2:
                    dump_x()
        S.barrier(engines=("sp",))
        print("ops:", S.nops, "sems:", len(S.sems))
        with nc.Block() as block:
            S.emit(block)
    return nc


OUT_NAMES = ["yp", "ys", "ssmp", "ssms", "scvp", "scvs", "sccp", "sccs", "mkp", "mvp", "vs"]


def make_in_maps(inp, ncores=NCORES):
    f = lambda a: np.ascontiguousarray(a, dtype=np.float32)
    ident, tri, Rm, selm = _consts()
    pcol = _lay_pcol(inp)
    bsp = f(inp["b_spatial"][0].reshape(1, 1024))
    wsT = f(np.transpose(inp["w_spatial"][0], (2, 0, 1)).reshape(128, 1024))
    w4 = inp["w_spatial"][0][:, 0:4, 0:4]
    wblk = np.zeros((16, 4, 8, 16, 4), np.float32)
    for b in range(16):
        wblk[b, :, :, b, :] = np.transpose(w4, (2, 0, 1))
    wblk = wblk.reshape(64, 512)
    tri4 = np.zeros((16, 4, 16, 4), np.float32)
    for b in range(16):
        tri4[b, :, b, :] = np.triu(np.ones((4, 4), np.float32))
    tri4 = tri4.reshape(64, 64)
    bsps = f(np.broadcast_to(inp["b_spatial"][0][:, None, 0:4], (8, 16, 4)).reshape(1, 512))
    shared = dict(
        pcol=pcol, bsp=bsp, wsT=wsT, ident=ident, tri=tri, Rm=Rm, selm=selm, wblk=wblk, tri4=tri4, bsps=bsps,
        w_in_ab=f(inp["w_in_ab"][0]), w_out_ab=f(inp["w_out_ab"][0]),
        w_ffn_gate=f(inp["w_ffn_gate"][0]), w_ffn_up=f(inp["w_ffn_up"][0]), w_ffn_down=f(inp["w_ffn_down"][0]),
        w_in_c=f(inp["w_in_c"][0]), w_out_c=f(inp["w_out_c"][0]), w_router=f(inp["w_router"][0]),
        w_exp_gate=f(inp["w_exp_gate"][0].reshape(8 * 1024, DFF)), w_exp_up=f(inp["w_exp_up"][0].reshape(8 * 1024, DFF)),
        w_exp_down=f(inp["w_exp_down"][0].reshape(8 * DFF, 1024)),
        w_mem_q=f(inp["w_mem_q"].reshape(2048, 1024)), w_mem_k=f(inp["w_mem_k"].reshape(2048, 1024)),
        w_mem_v=f(inp["w_mem_v"].reshape(2048, 1024)), w_mem_o=f(inp["w_mem_o"].reshape(2048, 1024)),
    )
    maps = []
    for c in range(ncores):
        b0, b1 = 16 * c, 16 * c + 16
        m = dict(shared)
        m["xp"] = f(inp["x_prompt"][c])
        m["xs"] = f(inp["x_sample"][b0:b1].reshape(64, 1024))
        m["memp"] = f(inp["mem_prompt"][c])
        m["sssm"] = f(inp["state_ssm"][0, b0:b1].reshape(16 * 1024, 128))
        m["scv"] = f(inp["state_ssm_conv"][0, b0:b1].reshape(48, 1536))
        m["ssc"] = f(inp["state_sconv"][0, b0:b1].reshape(32, 1024))
        m["ck"] = f(inp["cache_mem_k"][:, b0:b1].reshape(2 * 16 * 256, 1024))
        m["cv"] = f(inp["cache_mem_v"][:, b0:b1].reshape(2 * 16 * 256, 1024))
        maps.append(m)
    return maps


def assemble(results):
    n = len(results)
    g = lambda k: [np.asarray(r[k]) for r in results]
    yp = np.stack(g("yp"), 0)
    ys = np.concatenate(g("ys"), 0).reshape(16 * n, 4, 1024)
    ssmp = np.stack(g("ssmp"), 0).reshape(1, n, 16, 64, 128)
    ssms = np.concatenate(g("ssms"), 0).reshape(1, 16 * n, 16, 64, 128)
    scvp = np.stack(g("scvp"), 0).reshape(1, n, 3, 1536)
    scvs = np.concatenate(g("scvs"), 0).reshape(1, 16 * n, 3, 1536)
    sccp = np.stack(g("sccp"), 0).reshape(1, n, 2, 1024)
    sccs = np.concatenate(g("sccs"), 0).reshape(1, 16 * n, 2, 1024)
    mkp = np.stack([a.reshape(2, 256, 4, 256) for a in g("mkp")], 1)
    mvp = np.stack([a.reshape(2, 256, 4, 256) for a in g("mvp")], 1)
    vs = np.concatenate(g("vs"), 0).reshape(1, 16 * n, 4, 1024)
    return tuple(np.ascontiguousarray(a, dtype=np.float32) for a in (yp, ys, ssmp, ssms, scvp, scvs, sccp, sccs, mkp, mvp, vs))


def kernel(**inputs):
    inp = {k: np.asarray(v) for k, v in inputs.items()}
    nc = build_program()
    in_maps = make_in_maps(inp)
    res = run_bass_kernel_spmd(nc, in_maps, core_ids=list(range(NCORES)))
    return assemble(res.results)
```

```python
import os
import types
import numpy as np
from contextlib import ExitStack
import concourse.bass as bass
import concourse.mybir as mybir
from concourse.bass_utils import run_bass_kernel_spmd

F32 = mybir.dt.float32
BF16 = mybir.dt.bfloat16
AF = mybir.ActivationFunctionType
ALU = mybir.AluOpType
AX = mybir.AxisListType

NCORES = 8
D = 1024
NP = 2048
NS = 64
NT = NP + NS
EPS = 1e-6
DFF = 2816
NEXP = 8

C_GMIX, C_GMEM, C_GFFN, C_GFIN = 0, 16, 32, 48
C_CWS, C_CBS, C_GN, C_CWC = 56, 104, 116, 124
C_DTB, C_ALOG, C_DSK = 148, 149, 150
NPCOL = 166


class Buf:
    __slots__ = ("name", "last_w", "readers", "dsem", "dcount", "excl")

    def __init__(self, name, excl=False):
        self.name = name
        self.excl = excl
        self.last_w = None
        self.readers = {}
        self.dsem = None
        self.dcount = 0


def _freeze(fn):
    if fn.__closure__ is None:
        return fn
    cells = []
    for c in fn.__closure__:
        try:
            cells.append(types.CellType(c.cell_contents))
        except ValueError:
            cells.append(c)
    g = types.FunctionType(fn.__code__, fn.__globals__, fn.__name__, fn.__defaults__, tuple(cells))
    g.__kwdefaults__ = fn.__kwdefaults__
    return g


class Sched:
    ENG = ("pe", "act", "dve", "pool", "sp")

    def __init__(self, nc, stack):
        self.nc = nc
        self.prog = {e: [] for e in self.ENG}
        self.count = {e: 0 for e in self.ENG}
        self.seen = {e: {} for e in self.ENG}
        self.sems = {}
        self.dbufs = []
        self._stack = stack
        self.nops = 0

    def _sem(self, key):
        if key not in self.sems:
            self.sems[key] = self._stack.enter_context(self.nc.semaphore("s%d" % len(self.sems)))
        return self.sems[key]

    def _deps(self, eng, reads, writes):
        need = {}

        def want(k, v):
            if v > need.get(k, 0):
                need[k] = v
        me = ("e", eng)
        for b in reads:
            if b.last_w is not None:
                want(*b.last_w)
            if b.excl:
                for k, v in b.readers.items():
                    if k != me:
                        want(k, v)
        for b in writes:
            if b.last_w is not None:
                want(*b.last_w)
            for k, v in b.readers.items():
                want(k, v)
        out = []
        seen = self.seen[eng]
        for k, v in need.items():
            if seen.get(k, 0) < v:
                seen[k] = v
                out.append((self._sem(k), v))
        return out

    def op(self, eng, fn, reads=(), writes=()):
        fn = _freeze(fn)
        waits = self._deps(eng, reads, writes)
        self.count[eng] += 1
        tick = self.count[eng]
        key = ("e", eng)
        sem = self._sem(key)
        self.nops += 1

        def run(e, fn=fn, waits=waits, sem=sem):
            for s, v in waits:
                e.wait_ge(s, v)
            ins = fn(e)
            ins.then_inc(sem, 1)
        self.prog[eng].append(run)
        for b in writes:
            b.last_w = (key, tick)
            b.readers = {}
        for b in reads:
            if b not in writes:
                b.readers[key] = tick

    def dma(self, queue, fn, sbuf, reads=(), writes=(), n=1):
        fn = _freeze(fn)
        waits = self._deps(queue, reads, writes)
        if sbuf.dsem is None:
            sbuf.dsem = ("d", len(self.dbufs))
            self.dbufs.append(sbuf)
        key = sbuf.dsem
        sem = self._sem(key)
        sbuf.dcount += 16 * n
        val = sbuf.dcount

        def run(e, fn=fn, waits=waits, sem=sem, n=n):
            for s, v in waits:
                e.wait_ge(s, v)
            inss = fn(e)
            assert len(inss) == n
            for ins in inss:
                ins.then_inc(sem, 16)
        self.prog[queue].append(run)
        for b in writes:
            b.last_w = (key, val)
            b.readers = {}
        for b in reads:
            if b not in writes:
                b.readers[key] = val

    def barrier(self, engines=None):
        targets = [(("e", e), self.count[e]) for e in ("pe", "act", "dve") if self.count[e] > 0]
        targets += [(b.dsem, b.dcount) for b in self.dbufs]
        for eng in (engines or self.ENG):
            seen = self.seen[eng]
            waits = []
            for k, v in targets:
                if seen.get(k, 0) < v:
                    seen[k] = v
                    waits.append((self._sem(k), v))

            def run(e, waits=waits):
                for s, v in waits:
                    e.wait_ge(s, v)
            self.prog[eng].append(run)

    def emit(self, block):
        prog = self.prog

        @block.tensor
        def _(e):
            for f in prog["pe"]:
                f(e)

        @block.scalar
        def _(e):
            for f in prog["act"]:
                f(e)

        @block.vector
        def _(e):
            for f in prog["dve"]:
                f(e)

        @block.gpsimd
        def _(e):
            for f in prog["pool"]:
                f(e)

        @block.sync
        def _(e):
            for f in prog["sp"]:
                f(e)


class Ctx:
    def __init__(self, nc, stack):
        self.nc = nc
        self.st = stack
        self.S = Sched(nc, stack)
        self.arena_words = 52224
        self.arena = stack.enter_context(nc.sbuf_tensor("arena", [128, self.arena_words], F32))
        self.off = 0
        self.banks = []
        for i in range(8):
            t = stack.enter_context(nc.psum_tensor("bank%d" % i, [128, 512], F32))
            self.banks.append((t, Buf("bank%d" % i, excl=True)))
        self.bi = 0
        self.evi = 0

    def alloc(self, name, shape, dt):
        esz = 4 if dt == F32 else 2
        n = 1
        for s in shape[1:]:
            n *= s
        nbytes = (n * esz + 3) // 4 * 4
        w0 = self.off // 4
        nw = nbytes // 4
        assert w0 + nw <= self.arena_words, ("SBUF arena overflow", name, self.off, nbytes)
        ap = self.arena[:, w0:w0 + nw]
        if dt != F32:
            ap = ap.bitcast(dt)
        if shape[0] != 128:
            ap = ap[0:shape[0]]
        if len(shape) == 3:
            ap = ap.rearrange("p (a b) -> p a b", a=shape[1])
        elif len(shape) == 4:
            ap = ap.rearrange("p (a b c) -> p a b c", a=shape[1], b=shape[2])
        self.off += nbytes
        return ap, Buf(name)

    def bank(self):
        t, b = self.banks[self.bi]
        self.bi = (self.bi + 1) % 8
        return t, b


def _lay_pcol(inp):
    pc = np.zeros((128, NPCOL), np.float32)

    def cols(v):
        return np.ascontiguousarray(v.reshape(-1, 128).T)
    for l in range(2):
        pc[:, C_GMIX + 8 * l:C_GMIX + 8 * l + 8] = cols(inp["g_mix"][l])
        pc[:, C_GMEM + 8 * l:C_GMEM + 8 * l + 8] = cols(inp["g_mem"][l])
        pc[:, C_GFFN + 8 * l:C_GFFN + 8 * l + 8] = cols(inp["g_ffn"][l])
    pc[:, C_GFIN:C_GFIN + 8] = cols(inp["g_final"])
    for k in range(4):
        pc[:, C_CWS + 12 * k:C_CWS + 12 * k + 12] = cols(inp["conv_w_ssm"][0, k])
    pc[:, C_CBS:C_CBS + 12] = cols(inp["conv_b_ssm"][0])
    pc[:, C_GN:C_GN + 8] = cols(inp["g_ssm_norm"][0])
    for k in range(3):
        pc[:, C_CWC + 8 * k:C_CWC + 8 * k + 8] = cols(inp["conv_w_c"][0, k])
    pc[0:16, C_DTB] = inp["dt_bias"][0]
    pc[0:16, C_ALOG] = inp["a_log"][0]
    pc[:, C_DSK:C_DSK + 16] = np.broadcast_to(inp["d_skip"][0][None, :], (128, 16))
    return pc


def _consts():
    ident = np.eye(128, dtype=np.float32)
    tri = np.triu(np.ones((128, 128), np.float32))
    R = np.zeros((16, 8, 128), np.float32)
    for j in range(8):
        R[2 * j, j, 0:64] = 1.0
        R[2 * j + 1, j, 64:128] = 1.0
    sel = np.zeros((8, 8, 128), np.float32)
    for e in range(8):
        sel[e, e, :] = 1.0
    return ident, tri, R.reshape(16, 1024), sel.reshape(8, 1024)


def build_program(stop_after=99, dbg=False):
    nc = bass.Bass("TRN2", target_bir_lowering=False)

    def din(name, shape):
        return nc.dram_tensor(name, list(shape), F32, kind="ExternalInput").ap()

    def dout(name, shape):
        return nc.dram_tensor(name, list(shape), F32, kind="ExternalOutput").ap()

    xp_d = din("xp", [NP, D])
    xs_d = din("xs", [NS, D])
    memp_d = din("memp", [256, D])
    sssm_d = din("sssm", [16 * 1024, 128])
    scv_d = din("scv", [48, 1536])
    ssc_d = din("ssc", [32, 1024])
    ck_d = din("ck", [2 * 16 * 256, 1024])
    cv_d = din("cv", [2 * 16 * 256, 1024])
    pcol_d = din("pcol", [128, NPCOL])
    bsp_d = din("bsp", [1, 1024])
    wsT_d = din("wsT", [128, 1024])
    ident_d = din("ident", [128, 128])
    tri_d = din("tri", [128, 128])
    R_d = din("Rm", [16, 1024])
    sel_d = din("selm", [8, 1024])
    wblk_d = din("wblk", [64, 512])
    tri4_d = din("tri4", [64, 64])
    bsps_d = din("bsps", [1, 512])
    w_in_ab = din("w_in_ab", [1024, 4624])
    w_out_ab = din("w_out_ab", [2048, 1024])
    w_ffn_gate = din("w_ffn_gate", [1024, DFF])
    w_ffn_up = din("w_ffn_up", [1024, DFF])
    w_ffn_down = din("w_ffn_down", [DFF, 1024])
    w_in_c = din("w_in_c", [1024, 3072])
    w_out_c = din("w_out_c", [1024, 1024])
    w_router = din("w_router", [1024, 8])
    w_exp_gate = din("w_exp_gate", [8 * 1024, DFF])
    w_exp_up = din("w_exp_up", [8 * 1024, DFF])
    w_exp_down = din("w_exp_down", [8 * DFF, 1024])
    w_mem_q = din("w_mem_q", [2 * 1024, 1024])
    w_mem_k = din("w_mem_k", [2 * 1024, 1024])
    w_mem_v = din("w_mem_v", [2 * 1024, 1024])
    w_mem_o = din("w_mem_o", [2 * 1024, 1024])

    yp_d = dout("yp", [NP, D])
    ys_d = dout("ys", [NS, D])
    ssmp_d = dout("ssmp", [1024, 128])
    ssms_d = dout("ssms", [16 * 1024, 128])
    scvp_d = dout("scvp", [3, 1536])
    scvs_d = dout("scvs", [48, 1536])
    sccp_d = dout("sccp", [2, 1024])
    sccs_d = dout("sccs", [32, 1024])
    mkp_d = dout("mkp", [2 * 256, 1024])
    mvp_d = dout("mvp", [2 * 256, 1024])
    vs_d = dout("vs", [NS, 1024])
    if dbg:
        dbg_d = dout("dbgx", [128, 8 * NT])
    w_in_ab_b = nc.dram_tensor("w_in_ab_b", [9, 128, 8 * 512], BF16, kind="Internal").ap()
    w_dt_b = nc.dram_tensor("w_dt_b", [128, 8 * 16], BF16, kind="Internal").ap()
    w_out_ab_b = nc.dram_tensor("w_out_ab_b", [8, 128, 16 * 128], BF16, kind="Internal").ap()

    with ExitStack() as st:
        K = Ctx(nc, st)
        S = K.S
        outbufs = []

        x, Bx = K.alloc("x", [128, 8, NT], F32)
        pcol, Bpc = K.alloc("pcol", [128, NPCOL], F32)
        identf, Bidf = K.alloc("identf", [128, 128], F32)
        identb, Bidb = K.alloc("identb", [128, 128], BF16)
        trif, Btrf = K.alloc("trif", [128, 128], F32)
        onesf, Bonf = K.alloc("onesf", [128, 128], F32)
        onesb, Bonb = K.alloc("onesb", [128, 128], BF16)
        acol, Bacol = K.alloc("acol", [16, 2], F32)
        PERSIST = K.off

        Bwinb = [Buf("w_in_ab_b%d" % i) for i in range(10)]
        Bwoutb = [Buf("w_out_ab_b%d" % i) for i in range(8)]
        for cb in range(9):
            S.dma("pool", lambda e, cb=cb: [e.dma_start(out=w_in_ab_b[cb].rearrange("p (kc n) -> p kc n", kc=8),
                                                        in_=w_in_ab[:, cb * 512:(cb + 1) * 512].rearrange("(kc p) n -> p kc n", p=128))], Bwinb[cb], writes=[Bwinb[cb]])
        S.dma("pool", lambda e: [e.dma_start(out=w_dt_b.rearrange("p (kc n) -> p kc n", kc=8), in_=w_in_ab[:, 4608:4624].rearrange("(kc p) n -> p kc n", p=128))], Bwinb[9], writes=[Bwinb[9]])
        for cb in range(8):
            S.dma("pool", lambda e, cb=cb: [e.dma_start(out=w_out_ab_b[cb].rearrange("p (kc n) -> p kc n", kc=16),
                                                        in_=w_out_ab[:, cb * 128:(cb + 1) * 128].rearrange("(kc p) n -> p kc n", p=128))], Bwoutb[cb], writes=[Bwoutb[cb]])
        S.dma("sp", lambda e: [e.dma_start(out=pcol, in_=pcol_d)], Bpc, writes=[Bpc])
        S.dma("sp", lambda e: [e.dma_start(out=identf, in_=ident_d)], Bidf, writes=[Bidf])
        S.dma("pool", lambda e: [e.dma_start(out=identb, in_=ident_d)], Bidb, writes=[Bidb])
        S.dma("sp", lambda e: [e.dma_start(out=trif, in_=tri_d)], Btrf, writes=[Btrf])
        S.op("dve", lambda e: e.memset(onesf, 1.0), writes=[Bonf])
        S.op("dve", lambda e: e.memset(onesb, 1.0), writes=[Bonb])
        S.op("act", lambda e: e.activation(out=acol[:, 0:1], in_=pcol[0:16, C_ALOG:C_ALOG + 1], func=AF.Exp), reads=[Bpc], writes=[Bacol])
        S.op("dve", lambda e: e.tensor_scalar(out=acol[:, 1:2], in0=acol[:, 0:1], scalar1=-1.0, scalar2=None, op0=ALU.mult), reads=[Bacol], writes=[Bacol])

        def evac_copy(dst, src, rb, wb, extra_reads=()):
            K.evi += 1
            if K.evi % 2 == 0:
                S.op("act", lambda e: e.activation(out=dst, in_=src, func=AF.Copy), reads=[rb] + list(extra_reads), writes=[wb])
            else:
                S.op("dve", lambda e: e.tensor_copy(out=dst, in_=src), reads=[rb] + list(extra_reads), writes=[wb])

        def mm_group(out_ap, pairs, bankbuf, reads):
            n = len(pairs)

            def fn(e):
                ins = None
                for i, (l, r) in enumerate(pairs):
                    ins = e.matmul(out_ap, lhsT=l, rhs=r, start=(i == 0), stop=(i == n - 1))
                return ins
            S.op("pe", fn, reads=reads, writes=[bankbuf])

        def wload(dst, dbuf, src):
            S.dma("pool", lambda e: [e.dma_start(out=dst, in_=src)], dbuf, writes=[dbuf])

        def wview(w2d, r0, nrows, c0, ncols):
            return w2d[r0:r0 + nrows, c0:c0 + ncols].rearrange("(kc p) n -> p kc n", p=128)

        K.off = PERSIST
        xin = [K.alloc("xin%d" % i, [128, 4, D], F32) for i in range(2)]
        for ti in range(4):
            xi, Bxi = xin[ti % 2]
            S.dma("sp", lambda e, xi=xi, ti=ti: [e.dma_start(out=xi, in_=xp_d[ti * 512:(ti + 1) * 512, :].rearrange("(c p) f -> p c f", p=128))],
                  Bxi, writes=[Bxi])
            for kc in range(8):
                bk, Bbk = K.bank()

                def tr(e, xi=xi, bk=bk, kc=kc):
                    ins = None
                    for c in range(4):
                        ins = e.transpose(bk[:, c * 128:(c + 1) * 128], xi[:, c, kc * 128:(kc + 1) * 128], identf)
                    return ins
                S.op("pe", tr, reads=[Bxi, Bidf], writes=[Bbk])
                evac_copy(x[:, kc, ti * 512:(ti + 1) * 512], bk[:, :], Bbk, Bx)
        xi, Bxi = xin[0]
        S.dma("sp", lambda e: [e.dma_start(out=xi[0:64, 0, :], in_=xs_d)], Bxi, writes=[Bxi])
        bk, Bbk = K.bank()

        def trs(e, xi=xi, bk=bk):
            ins = None
            for kc in range(8):
                ins = e.transpose(bk[:, kc * 64:(kc + 1) * 64], xi[0:64, 0, kc * 128:(kc + 1) * 128], identf[0:64, 0:64])
            return ins
        S.op("pe", trs, reads=[Bxi, Bidf], writes=[Bbk])
        evac_copy(x[:, :, NP:NT], bk[:, :].rearrange("p (k t) -> p k t", k=8), Bbk, Bx)

        def rmsnorm(t0, W, gcol, hn, Bhn, sq, Bsq, rstd, Brstd, hnf=None, Bhnf=None):
            S.op("act", lambda e: e.activation(out=sq[:, :, 0:W], in_=x[:, :, t0:t0 + W], func=AF.Square), reads=[Bx], writes=[Bsq])
            bk, Bbk = K.bank()
            mm_group(bk[:, 0:W], [(onesb, sq[:, kc, 0:W]) for kc in range(8)], Bbk, [Bonb, Bsq])
            S.op("act", lambda e: e.activation(out=rstd[:, 0:W], in_=bk[:, 0:W], func=AF.Sqrt, bias=EPS, scale=1.0 / D), reads=[Bbk], writes=[Brstd])
            S.op("dve", lambda e: e.reciprocal(out=rstd[:, 0:W], in_=rstd[:, 0:W]), reads=[Brstd], writes=[Brstd])
            for kc in range(8):
                if hn is not None:
                    S.op("dve", lambda e, kc=kc: e.scalar_tensor_tensor(out=hn[:, kc, 0:W], in0=x[:, kc, t0:t0 + W], scalar=pcol[:, gcol + kc:gcol + kc + 1],
                                                                      in1=rstd[:, 0:W], op0=ALU.mult, op1=ALU.mult), reads=[Bx, Bpc, Brstd], writes=[Bhn])
                if hnf is not None:
                    S.op("dve", lambda e, kc=kc: e.scalar_tensor_tensor(out=hnf[:, kc, 0:W], in0=x[:, kc, t0:t0 + W], scalar=pcol[:, gcol + kc:gcol + kc + 1],
                                                                      in1=rstd[:, 0:W], op0=ALU.mult, op1=ALU.mult), reads=[Bx, Bpc, Brstd], writes=[Bhnf])

        def add_to_x(oc, t0, W, bk, Bbk):
            S.op("dve", lambda e: e.tensor_tensor(out=x[:, oc, t0:t0 + W], in0=bk[:, 0:W], in1=x[:, oc, t0:t0 + W], op=ALU.add), reads=[Bbk, Bx], writes=[Bx])

        def dump_x():
            S.barrier()
            S.dma("sp", lambda e: [e.dma_start(out=dbg_d, in_=x.rearrange("p k t -> p (k t)"))], Bx, reads=[Bx])
            outbufs.append(Bx)

        TILES512 = [(i * 512, 512) for i in range(4)] + [(NP, NS)]
        TILES_E = [(i * 448, 448) for i in range(4)] + [(1792, 320)]

        def phase1():
            S.barrier()
            K.off = PERSIST
            WT = 256
            hn, Bhn = K.alloc("hn", [128, 8, WT], BF16)
            sq, Bsq = K.alloc("sq", [128, 8, WT], BF16)
            rstd, Brstd = K.alloc("rstd", [128, WT], F32)
            wbufs = [K.alloc("w1_%d" % i, [128, 8, 512], BF16) for i in range(2)]
            wobufs = [K.alloc("wo1_%d" % i, [128, 16, 128], BF16) for i in range(2)]
            wdt, Bwdt = K.alloc("wdt", [128, 8, 16], BF16)
            u_fm, Bu = K.alloc("u_fm", [128, 8, WT], BF16)
            z_fm, Bz = K.alloc("z_fm", [128, 8, WT], BF16)
            v_tm, Bv = K.alloc("v_tm", [128, 2, 1024], BF16)
            v_f32, Bvf = K.alloc("v_f32", [64, 1024], F32)
            xlo, Bxlo = v_f32[0:48, 0:512], Bvf
            xbc, Bxbc = K.alloc("xbc", [128, 12, 3 + WT], BF16)
            xbcs, Bxbcs = K.alloc("xbcs", [128, 12, 16, 7], BF16)
            xlast, Bxl = K.alloc("xlast", [128, 12, 48], F32)
            xc, Bxc = K.alloc("xc", [128, 12, WT], BF16)
            dtf, Bdtf = K.alloc("dtf", [16, 2, WT], F32)
            _mk = K.off
            sci, Bsci = K.alloc("sci", [48, 1536], F32)
            K.off = _mk
            ycat, Bycat = K.alloc("ycat", [128, 16, WT], BF16)
            Bsci = Bycat
            diag, Bdiag = K.alloc("diag", [128, 48, 128], BF16)
            dI, BdI = K.alloc("dI", [128, 16, 128], BF16)
            wsm, Bwsm = K.alloc("wsm", [128, 8, 128], BF16)
            wsf, Bwsf = K.alloc("wsf", [128, 8, 128], BF16)
            bsp, Bbsp = K.alloc("bsp", [1, 1024], BF16)
            onesr, Bonesr = K.alloc("onesr", [1, 128], BF16)
            Sst = [K.alloc("Sst%d" % i, [128, 8, 128], F32) for i in range(2)]
            Rm, BRm = K.alloc("Rm", [16, 8, 128], F32)
            S.dma("sp", lambda e: [e.dma_start(out=Rm, in_=R_d.rearrange("h (j q) -> h j q", j=8))], BRm, writes=[BRm])
            STb, BSTb = K.alloc("STb", [128, 1024], BF16)
            xs_tm, Bxstm = K.alloc("xs_tm", [128, 1024], BF16)
            xdt_tm, Bxdt = K.alloc("xdt_tm", [128, 1024], BF16)
            xw_tm, Bxw = xdt_tm, Bxdt
            B_tm, BBtm = K.alloc("B_tm", [128, 2, 128], BF16)
            dtt, Bdtt = K.alloc("dtt", [128, 2, 16], F32)
            la_tm, Blat = K.alloc("la_tm", [128, 16], F32)
            wend, Bwend = K.alloc("wend", [128, 16], F32)
            sumd, Bsumd = K.alloc("sumd", [16, 1], F32)
            elc, Belc = K.alloc("elc", [128, 8], F32)
            rhsall, Brhs = K.alloc("rhsall", [128, 8, 128], F32)
            seg, Bseg = rhsall, Brhs
            Eb, BEb = K.alloc("Eb", [128, 8, 128], BF16)
            ELb, BELb = K.alloc("ELb", [128, 8, 128], BF16)
            CEb, BCEb = ELb, BELb
            wts, Bwts = Eb, BEb
            cbm, Bcbm = K.alloc("cbm", [128, 128], BF16)
            gated, Bgat = K.alloc("gated", [128, 4, 128], F32)
            gsq, Bgsq = K.alloc("gsq", [128, 4, 128], BF16)
            grs, Bgrs = K.alloc("grs", [128, 128], F32)
            wbf, Bwbf = K.alloc("wbf", [64, 8, 64], BF16)
            wbs, Bwbs = K.alloc("wbs", [64, 8, 64], BF16)
            tri4, Btri4 = K.alloc("tri4", [64, 64], F32)
            bsps, Bbsps = K.alloc("bsps", [1, 512], BF16)
            print("phase1 SBUF bytes/partition:", K.off)

            for k in range(4):
                for j in range(12):
                    idx = k * 12 + j
                    S.op("dve", lambda e, idx=idx: e.tensor_scalar(out=diag[:, idx, :], in0=identf, scalar1=pcol[:, C_CWS + idx:C_CWS + idx + 1], scalar2=None, op0=ALU.mult),
                         reads=[Bidf, Bpc], writes=[Bdiag])
            for h in range(16):
                S.op("dve", lambda e, h=h: e.tensor_scalar(out=dI[:, h, :], in0=identf, scalar1=pcol[:, C_DSK + h:C_DSK + h + 1], scalar2=None, op0=ALU.mult),
                     reads=[Bidf, Bpc], writes=[BdI])
            S.dma("pool", lambda e: [e.dma_start(out=wsf, in_=wsT_d.rearrange("s (g t) -> s g t", g=8))], Bwsf, writes=[Bwsf])
            S.op("dve", lambda e: e.tensor_tensor(out=wsm, in0=wsf, in1=trif.unsqueeze(1).to_broadcast([128, 8, 128]), op=ALU.mult), reads=[Bwsf, Btrf], writes=[Bwsm])
            S.dma("pool", lambda e: [e.dma_start(out=bsp, in_=bsp_d)], Bbsp, writes=[Bbsp])
            S.op("dve", lambda e: e.memset(onesr, 1.0), writes=[Bonesr])
            S.dma("pool", lambda e: [e.dma_start(out=wbf, in_=wblk_d.rearrange("r (g c) -> r g c", g=8))], Bwbf, writes=[Bwbf])
            S.dma("sp", lambda e: [e.dma_start(out=tri4, in_=tri4_d)], Btri4, writes=[Btri4])
            S.op("dve", lambda e: e.tensor_tensor(out=wbs, in0=wbf, in1=tri4.unsqueeze(1).to_broadcast([64, 8, 64]), op=ALU.mult), reads=[Bwbf, Btri4], writes=[Bwbs])
            S.dma("pool", lambda e: [e.dma_start(out=bsps, in_=bsps_d)], Bbsps, writes=[Bbsps])
            S.op("dve", lambda e: e.memset(xbc[:, :, 0:3], 0.0), writes=[Bxbc])
            S.dma("sp", lambda e: [e.dma_start(out=wdt, in_=w_dt_b.rearrange("p (kc n) -> p kc n", kc=8))], Bwdt, reads=[Bwinb[9]], writes=[Bwdt])
            S.dma("sp", lambda e: [e.dma_start(out=sci, in_=scv_d)], Bsci, writes=[Bsci])
            for j4 in range(3):
                bk, Bbk = K.bank()

                def trc(e, bk=bk, j4=j4):
                    ins = None
                    for jj in range(4):
                        j = j4 * 4 + jj
                        ins = e.transpose(bk[:, jj * 48:(jj + 1) * 48], sci[:, j * 128:(j + 1) * 128], identf[0:48, 0:48])
                    return ins
                S.op("pe", trc, reads=[Bsci, Bidf], writes=[Bbk])
                evac_copy(xbcs[:, j4 * 4:(j4 + 1) * 4, :, 0:3], bk[:, 0:192].rearrange("p (j b k) -> p j b k", j=4, b=16), Bbk, Bxbcs)

            sample_g1_bufs = tuple(Buf("sg1_%d" % i) for i in range(7))
            wi = [0]

            def next_w():
                r = wbufs[wi[0] % 2]
                wi[0] += 1
                return r
            woi = [0]

            tiles = [(i * WT, WT, True) for i in range(NP // WT)] + [(NP, NS, False)]
            sidx = [0]
            for (t0, W, is_p) in tiles:
                Q = 128 if is_p else 4
                nch = W // Q
                rmsnorm(t0, W, C_GMIX, hn, Bhn, sq, Bsq, rstd, Brstd)
                for blk in range(9):
                    wt, Bwt = next_w()
                    S.dma("sp", lambda e, wt=wt, blk=blk: [e.dma_start(out=wt, in_=w_in_ab_b[blk].rearrange("p (kc n) -> p kc n", kc=8))], Bwt, reads=[Bwinb[blk]], writes=[Bwt])
                    if blk in (2, 3):
                        vb = blk - 2
                        for c in range((W + 127) // 128):
                            rows = min(128, W - c * 128)
                            bk, Bbk = K.bank()
                            mm_group(bk[0:rows, :], [(hn[:, kc, c * 128:c * 128 + rows], wt[:, kc, :]) for kc in range(8)], Bbk, [Bhn, Bwt])
                            if is_p:
                                S.op("act", lambda e, bk=bk, c=c, vb=vb: e.activation(out=v_tm[:, c, vb * 512:(vb + 1) * 512], in_=bk[:, :], func=AF.Gelu_apprx_tanh),
                                     reads=[Bbk], writes=[Bv])
                            else:
                                S.op("act", lambda e, bk=bk, vb=vb: e.activation(out=v_f32[:, vb * 512:(vb + 1) * 512], in_=bk[0:64, :], func=AF.Gelu_apprx_tanh),
                                     reads=[Bbk], writes=[Bvf])
                                S.op("dve", lambda e, vb=vb: e.tensor_copy(out=v_tm[0:64, 0, vb * 512:(vb + 1) * 512], in_=v_f32[:, vb * 512:(vb + 1) * 512]),
                                     reads=[Bvf], writes=[Bv])
                        continue
                    for oc in range(4):
                        col = blk * 4 + oc
                        bk, Bbk = K.bank()
                        mm_group(bk[:, 0:W], [(wt[:, kc, oc * 128:(oc + 1) * 128], hn[:, kc, 0:W]) for kc in range(8)], Bbk, [Bhn, Bwt])
                        if col < 8:
                            S.op("act", lambda e, bk=bk, col=col: e.activation(out=u_fm[:, col, 0:W], in_=bk[:, 0:W], func=AF.Gelu_apprx_tanh), reads=[Bbk], writes=[Bu])
                        elif col < 24:
                            j = col - 16
                            S.op("act", lambda e, bk=bk, j=j: e.activation(out=z_fm[:, j, 0:W], in_=bk[:, 0:W], func=AF.Silu), reads=[Bbk], writes=[Bz])
                        else:
                            j = col - 24
                            if is_p:
                                S.op("act", lambda e, bk=bk, j=j: e.activation(out=xbc[:, j, 3:3 + W], in_=bk[:, 0:W], func=AF.Copy), reads=[Bbk], writes=[Bxbc])
                                if t0 + W == NP:
                                    S.op("dve", lambda e, bk=bk, j=j: e.tensor_copy(out=xlast[:, j, 0:3], in_=bk[:, W - 3:W]), reads=[Bbk], writes=[Bxl])
                            else:
                                S.op("act", lambda e, bk=bk, j=j: e.activation(out=xbcs[:, j, :, 3:7], in_=bk[:, 0:64].rearrange("p (b l) -> p b l", b=16), func=AF.Copy),
                                     reads=[Bbk], writes=[Bxbcs])
                                S.op("dve", lambda e, bk=bk, j=j: e.tensor_copy(out=xlast[:, j, :].rearrange("p (b k) -> p b k", b=16),
                                                                                 in_=bk[:, 0:64].rearrange("p (b l) -> p b l", b=16)[:, :, 1:4]), reads=[Bbk], writes=[Bxl])
                bk, Bbk = K.bank()
                mm_group(bk[0:16, 0:W], [(wdt[:, kc, :], hn[:, kc, 0:W]) for kc in range(8)], Bbk, [Bhn, Bwdt])
                S.op("act", lambda e, bk=bk: e.activation(out=dtf[:, 0, 0:W], in_=bk[0:16, 0:W], func=AF.Exp, bias=pcol[0:16, C_DTB:C_DTB + 1]), reads=[Bbk, Bpc], writes=[Bdtf])
                S.op("act", lambda e: e.activation(out=dtf[:, 0, 0:W], in_=dtf[:, 0, 0:W], func=AF.Ln, bias=1.0), reads=[Bdtf], writes=[Bdtf])
                S.op("dve", lambda e: e.tensor_scalar(out=dtf[:, 1, 0:W], in0=dtf[:, 0, 0:W], scalar1=acol[:, 1:2], scalar2=None, op0=ALU.mult), reads=[Bdtf, Bacol], writes=[Bdtf])
                for j in range(12):
                    bk, Bbk = K.bank()
                    if is_p:
                        pairs = [(diag[:, k * 12 + j, :], xbc[:, j, k:k + W]) for k in range(4)]
                        mm_group(bk[:, 0:W], pairs, Bbk, [Bdiag, Bxbc])
                    else:
                        pairs = [(diag[:, k * 12 + j, :], xbcs[:, j, :, k:k + 4]) for k in range(4)]
                        mm_group(bk[:, 0:W], pairs, Bbk, [Bdiag, Bxbcs])
                    S.op("act", lambda e, bk=bk, j=j: e.activation(out=xc[:, j, 0:W], in_=bk[:, 0:W], func=AF.Silu, bias=pcol[:, C_CBS + j:C_CBS + j + 1]), reads=[Bbk, Bpc], writes=[Bxc])
                if is_p:
                    S.op("dve", lambda e: e.tensor_copy(out=xbc[:, :, 0:3], in_=xbc[:, :, W:W + 3]), reads=[Bxbc], writes=[Bxbc])

                for c in range(nch):
                    o = c * Q
                    first = is_p and (t0 == 0 and c == 0)
                    if is_p:
                        Sc, BSc = Sst[0]
                    else:
                        b = c

                        def loadS(bb):
                            Sl, BSl = Sst[bb % 2]
                            S.dma("sp", lambda e, Sl=Sl, bb=bb: [e.dma_start(out=Sl, in_=sssm_d[bb * 1024:(bb + 1) * 1024, :].rearrange("(j q) n -> q j n", q=128))], BSl, writes=[BSl])
                        if b == 0:
                            loadS(0)
                        if b + 1 < 16:
                            loadS(b + 1)
                        Sc, BSc = Sst[b % 2]
                    for g in range(8):
                        bk, Bbk = K.bank()
                        if is_p:
                            vsl = v_tm[0:Q, c, g * 128:(g + 1) * 128]
                        else:
                            vsl = None
                        if is_p:
                            pairs = [(vsl, wsm[0:Q, g, 0:Q]), (onesr[0:1, 0:128], bsp[0:1, g * 128:g * 128 + Q])]
                            mm_group(bk[:, 0:Q], pairs, Bbk, [Bv, Bwsm, Bonesr, Bbsp])
                            S.op("dve", lambda e, bk=bk, g=g, o=o: e.tensor_tensor(out=ycat[:, g, o:o + Q], in0=bk[:, 0:Q], in1=u_fm[:, g, o:o + Q], op=ALU.mult),
                                 reads=[Bbk, Bu], writes=[Bycat])
                    bkx, Bbkx = K.bank()
                    bkxb = bkx[:, :].bitcast(BF16)

                    def trx(e, bkxb=bkxb, o=o, Q=Q):
                        ins = None
                        for j in range(8):
                            ins = e.transpose(bkxb[0:Q, j * 128:(j + 1) * 128], xc[:, j, o:o + Q], identb)
                        return ins
                    S.op("pe", trx, reads=[Bxc, Bidb], writes=[Bbkx])
                    S.op("act", lambda e, bkxb=bkxb, Q=Q: e.activation(out=xs_tm[0:Q, :], in_=bkxb[0:Q, :], func=AF.Copy), reads=[Bbkx], writes=[Bxstm])
                    bkb, Bbkb = K.bank()
                    bkbb = bkb[:, :].bitcast(BF16)

                    def trb(e, bkbb=bkbb, o=o, Q=Q):
                        ins = None
                        for g in range(2):
                            ins = e.transpose(bkbb[0:Q, g * 128:(g + 1) * 128], xc[:, 8 + g, o:o + Q], identb)
                        return ins
                    S.op("pe", trb, reads=[Bxc, Bidb], writes=[Bbkb])
                    S.op("dve", lambda e, bkbb=bkbb, Q=Q: e.tensor_copy(out=B_tm[0:Q, :, :], in_=bkbb[0:Q, 0:256].rearrange("p (g n) -> p g n", g=2)), reads=[Bbkb], writes=[BBtm])
                    bkd, Bbkd = K.bank()

                    def trd(e, bkd=bkd, o=o, Q=Q):
                        e.transpose(bkd[0:Q, 0:16], dtf[:, 0, o:o + Q], identf[0:16, 0:16])
                        return e.transpose(bkd[0:Q, 16:32], dtf[:, 1, o:o + Q], identf[0:16, 0:16])
                    S.op("pe", trd, reads=[Bdtf, Bidf], writes=[Bbkd])
                    S.op("dve", lambda e, bkd=bkd, Q=Q: e.tensor_copy(out=dtt[0:Q, :, :], in_=bkd[0:Q, 0:32].rearrange("p (a h) -> p a h", a=2)), reads=[Bbkd], writes=[Bdtt])
                    bkl, Bbkl = K.bank()

                    def mla(e, bkl=bkl, Q=Q):
                        e.matmul(bkl[0:Q, 0:16], lhsT=trif[0:Q, 0:Q], rhs=dtt[0:Q, 1, :], start=True, stop=True)
                        return e.matmul(bkl[0:Q, 16:32], lhsT=onesf[0:Q, 0:Q], rhs=dtt[0:Q, 1, :], start=True, stop=True)
                    S.op("pe", mla, reads=[Btrf, Bonf, Bdtt], writes=[Bbkl])
                    S.op("dve", lambda e, bkl=bkl, Q=Q: e.tensor_copy(out=la_tm[0:Q, :], in_=bkl[0:Q, 0:16]), reads=[Bbkl], writes=[Blat])
                    S.op("dve", lambda e, bkl=bkl, Q=Q: e.tensor_tensor(out=wend[0:Q, :], in0=bkl[0:Q, 16:32], in1=la_tm[0:Q, :], op=ALU.subtract), reads=[Bbkl, Blat], writes=[Bwend])
                    S.op("act", lambda e, Q=Q: e.activation(out=wend[0:Q, :], in_=wend[0:Q, :], func=AF.Exp), reads=[Bwend], writes=[Bwend])
                    S.op("dve", lambda e, o=o, Q=Q: e.tensor_reduce(out=sumd, in_=dtf[:, 1, o:o + Q], axis=AX.X, op=ALU.add), reads=[Bdtf], writes=[Bsumd])
                    bke, Bbke = K.bank()

                    def mel(e, bke=bke):
                        ins = None
                        for j in range(8):
                            ins = e.matmul(bke[:, j:j + 1], lhsT=Rm[:, j, :], rhs=sumd, start=True, stop=True)
                        return ins
                    S.op("pe", mel, reads=[BRm, Bsumd], writes=[Bbke])
                    S.op("act", lambda e, bke=bke: e.activation(out=elc, in_=bke[:, 0:8], func=AF.Exp), reads=[Bbke], writes=[Belc])
                    S.op("dve", lambda e, Q=Q: e.tensor_tensor(out=xdt_tm[0:Q, :].rearrange("p (h d) -> p h d", h=16), in0=xs_tm[0:Q, :].rearrange("p (h d) -> p h d", h=16),
                                                               in1=dtt[0:Q, 0, :].unsqueeze(2).to_broadcast([Q, 16, 64]), op=ALU.mult), reads=[Bxstm, Bdtt], writes=[Bxdt])
                    if not first:
                        for half in range(2):
                            bks, Bbks = K.bank()

                            def trS(e, bks=bks, half=half, Sc=Sc):
                                ins = None
                                for jj in range(4):
                                    ins = e.transpose(bks[:, jj * 128:(jj + 1) * 128], Sc[:, half * 4 + jj, :], identf)
                                return ins
                            S.op("pe", trS, reads=[BSc, Bidf], writes=[Bbks])
                            evac_copy(STb[:, half * 512:(half + 1) * 512], bks[:, :], Bbks, BSTb)
                    GB = []
                    for g in range(2):
                        c0 = 0 if is_p else g * 64
                        if is_p or g == 0:
                            bufs = (Brhs, BEb, BELb, Bcbm, Bgat, Bgsq, Bgrs)
                        else:
                            bufs = sample_g1_bufs
                        GB.append(dict(
                            rh=rhsall[0:Q, :, c0:c0 + Q], E=Eb[0:Q, :, c0:c0 + Q], EL=ELb[:, :, c0:c0 + Q], cb=cbm[0:Q, c0:c0 + Q],
                            ga=gated[:, :, c0:c0 + Q], gs=gsq[:, :, c0:c0 + Q], gr=grs[:, c0:c0 + Q],
                            Brh=bufs[0], BE=bufs[1], BEL=bufs[2], Bcb=bufs[3], Bga=bufs[4], Bgs=bufs[5], Bgr=bufs[6]))

                    def s1(g, G_):
                        rh, Brh = G_["rh"], G_["Brh"]
                        S.op("dve", lambda e, g=g, Q=Q, rh=rh: e.tensor_tensor(out=rh, in0=trif[0:Q, 0:Q].unsqueeze(1).to_broadcast([Q, 8, Q]),
                                                                              in1=dtt[0:Q, 1, g * 8:(g + 1) * 8].unsqueeze(2).to_broadcast([Q, 8, Q]), op=ALU.mult),
                             reads=[Btrf, Bdtt], writes=[Brh])
                        bkA, BbkA = K.bank()
                        if Q == 128:
                            bkB, BbkB = K.bank()

                            def mlab(e, bkA=bkA, bkB=bkB, rh=rh):
                                e.matmul(bkA[:, :], lhsT=onesf, rhs=rh[:, 0:4, :], start=True, stop=True)
                                return e.matmul(bkB[:, :], lhsT=onesf, rhs=rh[:, 4:8, :], start=True, stop=True)
                            S.op("pe", mlab, reads=[Bonf, Brh], writes=[BbkA, BbkB])
                            G_["lab"] = [(bkA[:, :].rearrange("p (h t) -> p h t", h=4), 0, 4, BbkA), (bkB[:, :].rearrange("p (h t) -> p h t", h=4), 4, 8, BbkB)]
                        else:
                            S.op("pe", lambda e, bkA=bkA, rh=rh: e.matmul(bkA[:, 0:32], lhsT=onesf[0:4, :], rhs=rh, start=True, stop=True), reads=[Bonf, Brh], writes=[BbkA])
                            G_["lab"] = [(bkA[:, 0:32].rearrange("p (h t) -> p h t", h=8), 0, 8, BbkA)]
                        bkc, Bbkc = K.bank()
                        S.op("pe", lambda e, bkc=bkc, g=g, o=o, Q=Q: e.matmul(bkc[0:Q, 0:Q], lhsT=xc[:, 8 + g, o:o + Q], rhs=xc[:, 10 + g, o:o + Q], start=True, stop=True),
                             reads=[Bxc], writes=[Bbkc])
                        G_["bkc"] = (bkc, Bbkc)

                    def s2(g, G_):
                        rh, Brh, EL, BEL, cb, Bcb = G_["rh"], G_["Brh"], G_["EL"], G_["BEL"], G_["cb"], G_["Bcb"]
                        for (lb, h0, h1, Bb_) in G_["lab"]:
                            nh = h1 - h0
                            S.op("act", lambda e, lb=lb, h0=h0, h1=h1, EL=EL: e.activation(out=EL[:, h0:h1, :], in_=lb, func=AF.Exp), reads=[Bb_], writes=[BEL])
                            S.op("dve", lambda e, lb=lb, h0=h0, h1=h1, g=g, Q=Q, nh=nh, rh=rh: e.tensor_tensor(
                                out=rh[:, h0:h1, :], in0=lb[0:Q], in1=la_tm[0:Q, g * 8 + h0:g * 8 + h1].unsqueeze(2).to_broadcast([Q, nh, Q]), op=ALU.subtract),
                                reads=[Bb_, Blat], writes=[Brh])
                        S.op("dve", lambda e, rh=rh: e.tensor_scalar(out=rh, in0=rh, scalar1=0.0, scalar2=None, op0=ALU.min), reads=[Brh], writes=[Brh])
                        bkc, Bbkc = G_["bkc"]
                        S.op("dve", lambda e, bkc=bkc, Q=Q, cb=cb: e.tensor_tensor(out=cb, in0=bkc[0:Q, 0:Q], in1=trif[0:Q, 0:Q], op=ALU.mult), reads=[Bbkc, Btrf], writes=[Bcb])

                    def s3(g, G_):
                        rh, Brh, E, BE, EL, BEL, cb, Bcb = G_["rh"], G_["Brh"], G_["E"], G_["BE"], G_["EL"], G_["BEL"], G_["cb"], G_["Bcb"]
                        S.op("act", lambda e, E=E, rh=rh: e.activation(out=E, in_=rh, func=AF.Exp), reads=[Brh], writes=[BE])
                        S.op("dve", lambda e, g=g, o=o, Q=Q, EL=EL: e.tensor_tensor(out=EL, in0=EL, in1=xc[:, 10 + g, o:o + Q].unsqueeze(1).to_broadcast([128, 8, Q]), op=ALU.mult),
                             reads=[BEL, Bxc], writes=[BEL])
                        S.op("dve", lambda e, Q=Q, E=E, cb=cb: e.tensor_tensor(out=E, in0=E, in1=cb.unsqueeze(1).to_broadcast([Q, 8, Q]), op=ALU.mult),
                             reads=[BE, Bcb], writes=[BE])

                    def s4(g, G_):
                        E, BE, EL, BEL, ga, Bga, gs, Bgs = G_["E"], G_["BE"], G_["EL"], G_["BEL"], G_["ga"], G_["Bga"], G_["gs"], G_["Bgs"]
                        bky, Bbky = K.bank()
                        for jj in range(4):
                            j = g * 4 + jj

                            def my(e, bky=bky, jj=jj, j=j, Q=Q, first=first, E=E, EL=EL):
                                ins = None
                                for hh in range(2):
                                    hl = jj * 2 + hh
                                    h = j * 2 + hh
                                    outp = bky[hh * 64:(hh + 1) * 64, jj * Q:(jj + 1) * Q]
                                    e.matmul(outp, lhsT=xdt_tm[0:Q, h * 64:(h + 1) * 64], rhs=E[:, hl, :], start=True, stop=False)
                                    ins = e.matmul(outp, lhsT=xs_tm[0:Q, h * 64:(h + 1) * 64], rhs=dI[0:Q, h, 0:Q], start=False, stop=first)
                                    if not first:
                                        ins = e.matmul(outp, lhsT=STb[:, h * 64:(h + 1) * 64], rhs=EL[:, hl, :], start=False, stop=True)
                                return ins
                            S.op("pe", my, reads=[Bxdt, BE, Bxstm, BdI, BSTb, BEL], writes=[Bbky])
                        S.op("dve", lambda e, bky=bky, g=g, o=o, Q=Q, ga=ga: e.tensor_tensor(out=ga, in0=bky[:, 0:4 * Q].rearrange("p (j t) -> p j t", j=4),
                                                                                          in1=z_fm[:, g * 4:(g + 1) * 4, o:o + Q], op=ALU.mult), reads=[Bbky, Bz], writes=[Bga])
                        S.op("act", lambda e, gs=gs, ga=ga: e.activation(out=gs, in_=ga, func=AF.Square), reads=[Bga], writes=[Bgs])

                    def s5(g, G_):
                        ga, Bga, gs, Bgs, gr, Bgr = G_["ga"], G_["Bga"], G_["gs"], G_["Bgs"], G_["gr"], G_["Bgr"]
                        bkr, Bbkr = K.bank()
                        mm_group(bkr[:, 0:Q], [(onesb, gs[:, jj, :]) for jj in range(4)], Bbkr, [Bonb, Bgs])
                        S.op("act", lambda e, bkr=bkr, Q=Q, gr=gr: e.activation(out=gr, in_=bkr[:, 0:Q], func=AF.Sqrt, bias=EPS, scale=1.0 / 512.0), reads=[Bbkr], writes=[Bgr])
                        S.op("dve", lambda e, gr=gr: e.reciprocal(out=gr, in_=gr), reads=[Bgr], writes=[Bgr])
                        for jj in range(4):
                            j = g * 4 + jj
                            S.op("dve", lambda e, jj=jj, j=j, o=o, Q=Q, ga=ga, gr=gr: e.scalar_tensor_tensor(out=ycat[:, 8 + j, o:o + Q], in0=ga[:, jj, :], scalar=pcol[:, C_GN + j:C_GN + j + 1],
                                                                                                            in1=gr, op0=ALU.mult, op1=ALU.mult), reads=[Bga, Bpc, Bgr], writes=[Bycat])
                    stages = (s1, s2, s3, s4, s5)
                    if is_p:
                        for g in range(2):
                            for st_ in stages:
                                st_(g, GB[g])
                    else:
                        for st_ in stages:
                            for g in range(2):
                                st_(g, GB[g])
                    S.op("dve", lambda e, Q=Q: e.tensor_tensor(out=xw_tm[0:Q, :].rearrange("p (h d) -> p h d", h=16), in0=xdt_tm[0:Q, :].rearrange("p (h d) -> p h d", h=16),
                                                               in1=wend[0:Q, :].unsqueeze(2).to_broadcast([Q, 16, 64]), op=ALU.mult), reads=[Bxdt, Bwend], writes=[Bxw])
                    for half in range(2):
                        bku, Bbku = K.bank()

                        def mu(e, bku=bku, half=half, Q=Q):
                            ins = None
                            for jj in range(4):
                                j = half * 4 + jj
                                ins = e.matmul(bku[:, jj * 128:(jj + 1) * 128], lhsT=xw_tm[0:Q, j * 128:(j + 1) * 128], rhs=B_tm[0:Q, j // 4, :], start=True, stop=True)
                            return ins
                        S.op("pe", mu, reads=[Bxw, BBtm], writes=[Bbku])
                        for jj in range(4):
                            j = half * 4 + jj
                            if first:
                                S.op("dve", lambda e, bku=bku, jj=jj, j=j, Sc=Sc: e.tensor_copy(out=Sc[:, j, :], in_=bku[:, jj * 128:(jj + 1) * 128]), reads=[Bbku], writes=[BSc])
                            else:
                                S.op("dve", lambda e, bku=bku, jj=jj, j=j, Sc=Sc: e.scalar_tensor_tensor(out=Sc[:, j, :], in0=Sc[:, j, :], scalar=elc[:, j:j + 1], in1=bku[:, jj * 128:(jj + 1) * 128],
                                                                                                        op0=ALU.mult, op1=ALU.add), reads=[BSc, Belc, Bbku], writes=[BSc])
                    if not is_p:
                        S.dma("sp", lambda e, Sc=Sc, b=b: [e.dma_start(out=ssms_d[b * 1024:(b + 1) * 1024, :].rearrange("(j q) n -> q j n", q=128), in_=Sc)], BSc, reads=[BSc])
                        outbufs.append(BSc)
                    elif t0 + o + Q == NP:
                        S.dma("sp", lambda e, Sc=Sc: [e.dma_start(out=ssmp_d.rearrange("(j q) n -> q j n", q=128), in_=Sc)], BSc, reads=[BSc])
                        outbufs.append(BSc)

                if not is_p:
                    for g in range(8):
                        bk, Bbk = K.bank()
                        pairs = [(v_tm[0:64, 0, g * 128:(g + 1) * 128], wbs[:, g, :]), (onesr[0:1, 0:128], bsps[0:1, g * 64:(g + 1) * 64])]
                        mm_group(bk[:, 0:64], pairs, Bbk, [Bv, Bwbs, Bonesr, Bbsps])
                        S.op("dve", lambda e, bk=bk, g=g: e.tensor_tensor(out=ycat[:, g, 0:64], in0=bk[:, 0:64], in1=u_fm[:, g, 0:64], op=ALU.mult),
                             reads=[Bbk, Bu], writes=[Bycat])
                    S.dma("sp", lambda e: [e.dma_start(out=vs_d, in_=v_f32)], Bvf, reads=[Bvf])
                    outbufs.append(Bvf)
                if (t0 + W == NP) or not is_p:
                    n = 3 if is_p else 48
                    dst = scvp_d if is_p else scvs_d
                    for j4 in range(3):
                        bk, Bbk = K.bank()

                        def trl(e, bk=bk, j4=j4, n=n):
                            ins = None
                            for jj in range(4):
                                ins = e.transpose(bk[0:n, jj * 128:(jj + 1) * 128], xlast[:, j4 * 4 + jj, 0:n], identf)
                            return ins
                        S.op("pe", trl, reads=[Bxl, Bidf], writes=[Bbk])
                        S.op("dve", lambda e, bk=bk, n=n: e.tensor_copy(out=xlo[0:n, :], in_=bk[0:n, :]), reads=[Bbk], writes=[Bxlo])
                        S.dma("sp", lambda e, n=n, dst=dst, j4=j4: [e.dma_start(out=dst[:, j4 * 512:(j4 + 1) * 512], in_=xlo[0:n, :])], Bxlo, reads=[Bxlo])
                    outbufs.append(Bxlo)

                for oc in range(8):
                    wo, Bwo = wobufs[woi[0] % 2]
                    woi[0] += 1
                    S.dma("sp", lambda e, wo=wo, oc=oc: [e.dma_start(out=wo, in_=w_out_ab_b[oc].rearrange("p (kc n) -> p kc n", kc=16))], Bwo, reads=[Bwoutb[oc]], writes=[Bwo])
                    bk, Bbk = K.bank()
                    mm_group(bk[:, 0:W], [(wo[:, kc, :], ycat[:, kc, 0:W]) for kc in range(16)], Bbk, [Bwo, Bycat])
                    add_to_x(oc, t0, W, bk, Bbk)
            return dict(v_f32=(v_f32, Bvf), xlast=(xlast, Bxl))

        if stop_after >= 1:
            p1 = phase1()
        elif dbg:
            dump_x()

        if dbg and stop_after == 1:
            dump_x()

        def phase_attn(l):
            S.barrier()
            K.off = PERSIST
            hn, Bhn = K.alloc("hn", [128, 8, 512], BF16)
            sq, Bsq = K.alloc("sq", [128, 8, 512], BF16)
            rstd, Brstd = K.alloc("rstd", [128, 512], F32)
            wq, Bwq = K.alloc("wq", [128, 8, 1024], BF16)
            wo, Bwo = K.alloc("wo", [128, 8, 1024], BF16)
            wkv = [K.alloc("wkv%d" % i, [128, 8, 512], BF16) for i in range(2)]
            Kfm, BKfm = K.alloc("Kfm", [128, 8, 256], BF16)
            Vb, BVb = K.alloc("Vb", [128, 2, 1024], BF16)
            rden2 = [K.alloc("rden%d" % i, [128, 512], F32) for i in range(2)]
            o_fm, Bo = K.alloc("o_fm", [128, 8, 512], BF16)
            _ov = K.off
            kvo = [K.alloc("kvo%d" % i, [128, 2, 1024], F32) for i in range(2)]
            mi, Bmi = K.alloc("mi", [128, 2, 1024], F32)
            _ov_end = K.off
            K.off = _ov
            q_fm, Bq = K.alloc("q_fm", [128, 8, 512], BF16)
            ET = [K.alloc("ET%d" % i, [128, 2, 512], BF16) for i in range(2)]
            Ks = [K.alloc("Ks%d" % i, [128, 2, 1024], BF16) for i in range(2)]
            Vs = [K.alloc("Vs%d" % i, [128, 2, 1024], BF16) for i in range(2)]
            K.off = max(K.off, _ov_end)
            Kfs2 = [K.alloc("Kfs%d" % i, [128, 8, 256], BF16) for i in range(2)]
            ETs2 = [K.alloc("ETs%d" % i, [128, 32], BF16) for i in range(2)]
            rds2 = [K.alloc("rds%d" % i, [128, 16], F32) for i in range(2)]
            memT, BmemT = K.alloc("memT", [128, 8, 256], BF16)
            print("attn SBUF bytes/partition:", K.off)
            S.dma("sp", lambda e: [e.dma_start(out=mi, in_=memp_d.rearrange("(c p) f -> p c f", p=128))], Bmi, writes=[Bmi])
            for kc2 in range(4):
                bk, Bbk = K.bank()

                def trm(e, bk=bk, kc2=kc2):
                    ins = None
                    for kk in range(2):
                        for mc in range(2):
                            kc = kc2 * 2 + kk
                            ins = e.transpose(bk[:, kk * 256 + mc * 128: kk * 256 + mc * 128 + 128], mi[:, mc, kc * 128:(kc + 1) * 128], identf)
                    return ins
                S.op("pe", trm, reads=[Bmi, Bidf], writes=[Bbk])
                evac_copy(memT[:, kc2 * 2:kc2 * 2 + 2, :], bk[:, :].rearrange("p (k m) -> p k m", k=2), Bbk, BmemT)

            SC = 1.0 / 16.0
            wload(wq, Bwq, wview(w_mem_q, l * 1024, 1024, 0, 1024))
            for which in range(2):
                wsrc = w_mem_k if which == 0 else w_mem_v
                dstd = mkp_d if which == 0 else mvp_d
                ko, Bko = kvo[which]
                for cb in range(2):
                    wt, Bwt = wkv[cb]
                    wload(wt, Bwt, wview(wsrc, l * 1024, 1024, cb * 512, 512))
                    for mc in range(2):
                        bk, Bbk = K.bank()
                        mm_group(bk[:, :], [(memT[:, kc, mc * 128:(mc + 1) * 128], wt[:, kc, :]) for kc in range(8)], Bbk, [BmemT, Bwt])
                        evac_copy(ko[:, mc, cb * 512:(cb + 1) * 512], bk[:, :], Bbk, Bko)
                        if which == 1:
                            S.op("act", lambda e, bk=bk, mc=mc, cb=cb: e.activation(out=Vb[:, mc, cb * 512:(cb + 1) * 512], in_=bk[:, :], func=AF.Copy), reads=[Bbk], writes=[BVb])
                    if which == 0:
                        for oc in range(4):
                            bk, Bbk = K.bank()
                            mm_group(bk[:, 0:256], [(wt[:, kc, oc * 128:(oc + 1) * 128], memT[:, kc, :]) for kc in range(8)], Bbk, [BmemT, Bwt])
                            evac_copy(Kfm[:, cb * 4 + oc, :], bk[:, 0:256], Bbk, BKfm)
                S.dma("sp", lambda e, ko=ko, dstd=dstd: [e.dma_start(out=dstd[l * 256:(l + 1) * 256, :].rearrange("(mc p) d -> p mc d", p=128), in_=ko)], Bko, reads=[Bko])
                outbufs.append(Bko)
            wload(wo, Bwo, wview(w_mem_o, l * 1024, 1024, 0, 1024))
            S.barrier()

            for (t0, W) in TILES512:
                is_p = t0 < NP
                rmsnorm(t0, W, C_GMEM + 8 * l, hn, Bhn, sq, Bsq, rstd, Brstd)
                for oc in range(8):
                    bk, Bbk = K.bank()
                    mm_group(bk[:, 0:W], [(wq[:, kc, oc * 128:(oc + 1) * 128], hn[:, kc, 0:W]) for kc in range(8)], Bbk, [Bwq, Bhn])
                    evac_copy(q_fm[:, oc, 0:W], bk[:, 0:W], Bbk, Bq)
                if is_p:
                    def scores(h):
                        Et, BEt = ET[h % 2]
                        for mc in range(2):
                            bk, Bbk = K.bank()
                            mm_group(bk[:, 0:W], [(Kfm[:, 2 * h + ec, mc * 128:(mc + 1) * 128], q_fm[:, 2 * h + ec, 0:W]) for ec in range(2)], Bbk, [BKfm, Bq])
                            S.op("act", lambda e, bk=bk, Et=Et, mc=mc: e.activation(out=Et[:, mc, 0:W], in_=bk[:, 0:W], func=AF.Exp, scale=SC), reads=[Bbk], writes=[BEt])
                    scores(0)
                    for h in range(4):
                        Et, BEt = ET[h % 2]
                        rden, Brden = rden2[h % 2]
                        if h + 1 < 4:
                            scores(h + 1)
                        bk, Bbk = K.bank()
                        mm_group(bk[:, 0:W], [(onesb, Et[:, mc, 0:W]) for mc in range(2)], Bbk, [Bonb, BEt])
                        S.op("dve", lambda e, bk=bk: e.reciprocal(out=rden[:, 0:W], in_=bk[:, 0:W]), reads=[Bbk], writes=[Brden])
                        for ec in range(2):
                            bk, Bbk = K.bank()
                            mm_group(bk[:, 0:W], [(Vb[:, mc, (2 * h + ec) * 128:(2 * h + ec + 1) * 128], Et[:, mc, 0:W]) for mc in range(2)], Bbk, [BVb, BEt])
                            S.op("dve", lambda e, bk=bk, h=h, ec=ec: e.tensor_tensor(out=o_fm[:, 2 * h + ec, 0:W], in0=bk[:, 0:W], in1=rden[:, 0:W], op=ALU.mult),
                                 reads=[Bbk, Brden], writes=[Bo])
                else:
                    def prep(b):
                        Kb, BKb = Ks[b % 2]
                        Vs_, BVs = Vs[b % 2]
                        Kfs, BKfs = Kfs2[b % 2]
                        r0 = (l * 16 + b) * 256
                        wload(Kb, BKb, ck_d[r0:r0 + 256, :].rearrange("(mc p) d -> p mc d", p=128))
                        wload(Vs_, BVs, cv_d[r0:r0 + 256, :].rearrange("(mc p) d -> p mc d", p=128))
                        for kc4 in range(2):
                            bk, Bbk = K.bank()
                            bkb = bk[:, :].bitcast(BF16)

                            def trk(e, bkb=bkb, Kb=Kb, kc4=kc4):
                                ins = None
                                for kk in range(4):
                                    for mc in range(2):
                                        kc = kc4 * 4 + kk
                                        ins = e.transpose(bkb[:, kk * 256 + mc * 128:kk * 256 + mc * 128 + 128], Kb[:, mc, kc * 128:(kc + 1) * 128], identb)
                                return ins
                            S.op("pe", trk, reads=[BKb, Bidb], writes=[Bbk])
                            evac_copy(Kfs[:, kc4 * 4:(kc4 + 1) * 4, :], bkb.rearrange("p (k m) -> p k m", k=4), Bbk, BKfs)

                    def score(b):
                        Kfs, BKfs = Kfs2[b % 2]
                        ETs, BETs = ETs2[b % 2]
                        bk, Bbk = K.bank()

                        def msc(e, bk=bk, b=b, Kfs=Kfs):
                            ins = None
                            for h in range(4):
                                for mc in range(2):
                                    c0 = (h * 2 + mc) * 4
                                    for ec in range(2):
                                        ins = e.matmul(bk[:, c0:c0 + 4], lhsT=Kfs[:, 2 * h + ec, mc * 128:(mc + 1) * 128], rhs=q_fm[:, 2 * h + ec, b * 4:b * 4 + 4],
                                                       start=(ec == 0), stop=(ec == 1))
                            return ins
                        S.op("pe", msc, reads=[BKfs, Bq], writes=[Bbk])
                        S.op("act", lambda e, bk=bk, ETs=ETs: e.activation(out=ETs, in_=bk[:, 0:32], func=AF.Exp, scale=SC), reads=[Bbk], writes=[BETs])

                    def pv(b):
                        Vs_, BVs = Vs[b % 2]
                        ETs, BETs = ETs2[b % 2]
                        rds, Brds = rds2[b % 2]
                        bk2, Bbk2 = K.bank()

                        def mpv(e, bk2=bk2, Vs_=Vs_, ETs=ETs):
                            ins = None
                            for h in range(4):
                                for mc in range(2):
                                    c0 = (h * 2 + mc) * 4
                                    ins = e.matmul(bk2[:, h * 4:h * 4 + 4], lhsT=onesb, rhs=ETs[:, c0:c0 + 4], start=(mc == 0), stop=(mc == 1))
                                for ec in range(2):
                                    for mc in range(2):
                                        c0 = (h * 2 + mc) * 4
                                        o0 = 16 + (2 * h + ec) * 4
                                        ins = e.matmul(bk2[:, o0:o0 + 4], lhsT=Vs_[:, mc, (2 * h + ec) * 128:(2 * h + ec + 1) * 128], rhs=ETs[:, c0:c0 + 4],
                                                       start=(mc == 0), stop=(mc == 1))
                            return ins
                        S.op("pe", mpv, reads=[Bonb, BETs, BVs], writes=[Bbk2])
                        S.op("dve", lambda e, bk2=bk2, rds=rds: e.reciprocal(out=rds, in_=bk2[:, 0:16]), reads=[Bbk2], writes=[Brds])
                        S.op("dve", lambda e, bk2=bk2, b=b, rds=rds: e.tensor_tensor(out=o_fm[:, :, b * 4:b * 4 + 4].rearrange("p (h c) l -> p h c l", h=4),
                                                                                     in0=bk2[:, 16:48].rearrange("p (h c l) -> p h c l", h=4, c=2),
                                                                                     in1=rds.rearrange("p (h l) -> p h l", h=4).unsqueeze(2).to_broadcast([128, 4, 2, 4]), op=ALU.mult),
                             reads=[Bbk2, Brds], writes=[Bo])

                    prep(0)
                    for b in range(16):
                        if b >= 1:
                            pv(b - 1)
                        if b + 1 < 16:
                            prep(b + 1)
                        score(b)
                    pv(15)
                for oc in range(8):
                    bk, Bbk = K.bank()
                    mm_group(bk[:, 0:W], [(wo[:, kc, oc * 128:(oc + 1) * 128], o_fm[:, kc, 0:W]) for kc in range(8)], Bbk, [Bwo, Bo])
                    add_to_x(oc, t0, W, bk, Bbk)

        def phase_ffn(moe):
            S.barrier()
            K.off = PERSIST
            hn, Bhn = K.alloc("hnall", [128, 8, NT], BF16)
            G = 4
            wg = [K.alloc("wg%d" % i, [128, 8, G * 128], BF16) for i in range(2)]
            wu = [K.alloc("wu%d" % i, [128, 8, G * 128], BF16) for i in range(2)]
            wd = [K.alloc("wd%d" % i, [128, G, 1024], BF16) for i in range(2)]
            sg = [K.alloc("sg%d" % i, [128, 512], BF16) for i in range(2)]
            hh = [K.alloc("hh%d" % i, [128, G, 512], BF16) for i in range(2)]
            sq, Bsq = K.alloc("sq", [128, 8, 512], BF16)
            rstd, Brstd = K.alloc("rstd", [128, 512], F32)
            hnf = Bhnf = None
            if moe:
                wr, Bwr = K.alloc("wr", [128, 8, 8], F32)
                comb, Bcomb = K.alloc("comb", [128, 17, 8], F32)
                lg3, Blg = K.alloc("lg3", [128, 4, 8], F32)
                m13, Bm1 = K.alloc("m13", [128, 5, 4], F32)
                mk13, Bmk1 = K.alloc("mk13", [128, 4, 8], F32)
                mk23, Bmk2 = K.alloc("mk23", [128, 4, 8], F32)
                l23, Bl2 = K.alloc("l23", [128, 4, 8], F32)
                combT, BcombT = K.alloc("combT", [8, NT], BF16)
                selb, Bselb = K.alloc("selb", [8, 8, 128], BF16)
                cbc, Bcbc = K.alloc("cbc", [128, NT], BF16)
                hnf, Bhnf = K.alloc("hnf", [128, 8, 512], F32)
                S.dma("sp", lambda e: [e.dma_start(out=wr, in_=w_router.rearrange("(kc p) n -> p kc n", p=128))], Bwr, writes=[Bwr])
                S.dma("pool", lambda e: [e.dma_start(out=selb, in_=sel_d.rearrange("a (b p) -> a b p", b=8))], Bselb, writes=[Bselb])
            print("ffn SBUF bytes/partition:", K.off)
            gcol = C_GFFN + (8 if moe else 0)
            for (t0, W) in TILES512:
                rmsnorm(t0, W, gcol, hn[:, :, t0:t0 + W], Bhn, sq, Bsq, rstd, Brstd, hnf, Bhnf)
                if moe:
                    nb = (W + 127) // 128
                    rows = min(128, W)
                    blk0 = t0 // 128
                    bk, Bbk = K.bank()

                    def mrt(e, bk=bk, nb=nb, rows=rows):
                        ins = None
                        for c in range(nb):
                            for kc in range(8):
                                ins = e.matmul(bk[0:rows, c * 8:(c + 1) * 8], lhsT=hnf[:, kc, c * 128:c * 128 + rows], rhs=wr[:, kc, :], start=(kc == 0), stop=(kc == 7))
                        return ins
                    S.op("pe", mrt, reads=[Bhnf, Bwr], writes=[Bbk])
                    L3 = lg3[0:rows, 0:nb, :]
                    M1 = mk13[0:rows, 0:nb, :]
                    M2 = mk23[0:rows, 0:nb, :]
                    L2 = l23[0:rows, 0:nb, :]
                    mx = lambda i: m13[0:rows, i, 0:nb]
                    mxb = lambda i: m13[0:rows, i, 0:nb].unsqueeze(2).to_broadcast([rows, nb, 8])
                    S.op("dve", lambda e, bk=bk, L3=L3, rows=rows, nb=nb: e.tensor_copy(out=L3, in_=bk[0:rows, 0:nb * 8].rearrange("p (c e) -> p c e", c=nb)), reads=[Bbk], writes=[Blg])
                    S.op("dve", lambda e, L3=L3, o_=mx(0): e.tensor_reduce(out=o_, in_=L3, axis=AX.X, op=ALU.max), reads=[Blg], writes=[Bm1])
                    S.op("dve", lambda e, L3=L3, M1=M1, b_=mxb(0): e.tensor_tensor(out=M1, in0=L3, in1=b_, op=ALU.is_equal), reads=[Blg, Bm1], writes=[Bmk1])
                    S.op("dve", lambda e, L3=L3, M1=M1, L2=L2: e.scalar_tensor_tensor(out=L2, in0=M1, scalar=-1e30, in1=L3, op0=ALU.mult, op1=ALU.add), reads=[Bmk1, Blg], writes=[Bl2])
                    S.op("dve", lambda e, L2=L2, o_=mx(1): e.tensor_reduce(out=o_, in_=L2, axis=AX.X, op=ALU.max), reads=[Bl2], writes=[Bm1])
                    S.op("dve", lambda e, L2=L2, M2=M2, b_=mxb(1): e.tensor_tensor(out=M2, in0=L2, in1=b_, op=ALU.is_equal), reads=[Bl2, Bm1], writes=[Bmk2])
                    S.op("dve", lambda e, a_=mx(2), b_=mx(1), c_=mx(0): e.tensor_tensor(out=a_, in0=b_, in1=c_, op=ALU.subtract), reads=[Bm1], writes=[Bm1])
                    S.op("act", lambda e, a_=mx(3), b_=mx(2): e.activation(out=a_, in_=b_, func=AF.Sigmoid), reads=[Bm1], writes=[Bm1])
                    S.op("dve", lambda e, a_=mx(4), b_=mx(3): e.tensor_scalar(out=a_, in0=b_, scalar1=-1.0, scalar2=1.0, op0=ALU.mult, op1=ALU.add), reads=[Bm1], writes=[Bm1])
                    S.op("dve", lambda e, M1=M1, b_=mxb(4): e.tensor_tensor(out=M1, in0=M1, in1=b_, op=ALU.mult), reads=[Bmk1, Bm1], writes=[Bmk1])
                    S.op("dve", lambda e, M2=M2, b_=mxb(3): e.tensor_tensor(out=M2, in0=M2, in1=b_, op=ALU.mult), reads=[Bmk2, Bm1], writes=[Bmk2])
                    S.op("dve", lambda e, M1=M1, M2=M2, blk0=blk0, nb=nb, rows=rows: e.tensor_tensor(out=comb[0:rows, blk0:blk0 + nb, :], in0=M1, in1=M2, op=ALU.add),
                         reads=[Bmk1, Bmk2], writes=[Bcomb])
                    bkt, Bbkt = K.bank()

                    def trt(e, bkt=bkt, blk0=blk0, nb=nb, rows=rows):
                        ins = None
                        for c in range(nb):
                            ins = e.transpose(bkt[0:8, c * 128:c * 128 + rows], comb[0:rows, blk0 + c, :], identf[0:rows, 0:rows])
                        return ins
                    S.op("pe", trt, reads=[Bcomb, Bidf], writes=[Bbkt])
                    S.op("act", lambda e, bkt=bkt, t0=t0, W=W: e.activation(out=combT[:, t0:t0 + W], in_=bkt[0:8, 0:W], func=AF.Copy), reads=[Bbkt], writes=[BcombT])
            nexp = NEXP if moe else 1
            blocks = [(s0, min(G, 22 - s0)) for s0 in range(0, 22, G)]
            wi = 0
            hcnt = [0]
            pend = []
            for ex in range(nexp):
                if moe:
                    wgd, wud, wdd = w_exp_gate, w_exp_up, w_exp_down
                    rg0, rd0 = ex * 1024, ex * DFF
                    for (t0, W) in TILES512:
                        bk, Bbk = K.bank()
                        S.op("pe", lambda e, bk=bk, ex=ex, t0=t0, W=W: e.matmul(bk[:, 0:W], lhsT=selb[:, ex, :], rhs=combT[:, t0:t0 + W], start=True, stop=True),
                             reads=[Bselb, BcombT], writes=[Bbk])
                        S.op("act", lambda e, bk=bk, t0=t0, W=W: e.activation(out=cbc[:, t0:t0 + W], in_=bk[:, 0:W], func=AF.Copy), reads=[Bbk], writes=[Bcbc])
                else:
                    wgd, wud, wdd = w_ffn_gate, w_ffn_up, w_ffn_down
                    rg0, rd0 = 0, 0
                for (s0, ns) in blocks:
                    wgt, Bwg = wg[wi % 2]
                    wut, Bwu = wu[wi % 2]
                    wdt_, Bwd = wd[wi % 2]
                    wi += 1
                    wload(wgt[:, :, 0:ns * 128], Bwg, wview(wgd, rg0, 1024, s0 * 128, ns * 128))
                    wload(wut[:, :, 0:ns * 128], Bwu, wview(wud, rg0, 1024, s0 * 128, ns * 128))
                    wload(wdt_[:, 0:ns, :], Bwd, wview(wdd, rd0 + s0 * 128, ns * 128, 0, 1024))
                    for ti, (t0, W) in enumerate(TILES_E):
                        hht, Bhh = hh[hcnt[0] % 2]
                        hcnt[0] += 1
                        for sl in range(ns):
                            sgt, Bsg = sg[sl % 2]
                            bkg, Bbkg = K.bank()
                            mm_group(bkg[:, 0:W], [(wgt[:, kc, sl * 128:(sl + 1) * 128], hn[:, kc, t0:t0 + W]) for kc in range(8)], Bbkg, [Bwg, Bhn])
                            bku, Bbku = K.bank()
                            mm_group(bku[:, 0:W], [(wut[:, kc, sl * 128:(sl + 1) * 128], hn[:, kc, t0:t0 + W]) for kc in range(8)], Bbku, [Bwu, Bhn])
                            S.op("act", lambda e, bkg=bkg, sgt=sgt, W=W: e.activation(out=sgt[:, 0:W], in_=bkg[:, 0:W], func=AF.Silu), reads=[Bbkg], writes=[Bsg])
                            S.op("dve", lambda e, bku=bku, sgt=sgt, hht=hht, sl=sl, W=W: e.tensor_tensor(out=hht[:, sl, 0:W], in0=bku[:, 0:W], in1=sgt[:, 0:W], op=ALU.mult),
                                 reads=[Bbku, Bsg], writes=[Bhh])
                            if moe:
                                S.op("dve", lambda e, hht=hht, sl=sl, t0=t0, W=W: e.tensor_tensor(out=hht[:, sl, 0:W], in0=hht[:, sl, 0:W], in1=cbc[:, t0:t0 + W], op=ALU.mult),
                                     reads=[Bhh, Bcbc], writes=[Bhh])
                        def down(wdt_=wdt_, Bwd=Bwd, hht=hht, Bhh=Bhh, ns=ns, t0=t0, W=W):
                            for oc in range(8):
                                bk, Bbk = K.bank()
                                mm_group(bk[:, 0:W], [(wdt_[:, sl, oc * 128:(oc + 1) * 128], hht[:, sl, 0:W]) for sl in range(ns)], Bbk, [Bwd, Bhh])
                                add_to_x(oc, t0, W, bk, Bbk)
                        if pend:
                            pend.pop()()
                        pend.append(down)
            if pend:
                pend.pop()()

        def phase_mixc():
            S.barrier()
            K.off = PERSIST
            hn, Bhn = K.alloc("hn", [128, 8, 512], BF16)
            sq, Bsq = K.alloc("sq", [128, 8, 512], BF16)
            rstd, Brstd = K.alloc("rstd", [128, 512], F32)
            wic, Bwic = K.alloc("wic", [128, 8, 3072], BF16)
            woc, Bwoc = K.alloc("woc", [128, 8, 1024], BF16)
            diagc, Bdiagc = K.alloc("diagc", [128, 24, 128], BF16)
            chb, Bchb = K.alloc("chb", [128, 8, 2 + 512], BF16)
            chs, Bchs = K.alloc("chs", [128, 8, 16, 6], BF16)
            chl, Bchl = K.alloc("chl", [128, 8, 32], F32)
            cgs2 = [K.alloc("cgs%d" % i, [128, 512], F32) for i in range(2)]
            ysb2 = [K.alloc("ysb%d" % i, [128, 512], F32) for i in range(2)]
            yb, Byb = K.alloc("yb", [128, 8, 512], BF16)
            sci, Bsci = K.alloc("sci", [32, 1024], F32)
            clo, Bclo = K.alloc("clo", [32, 1024], F32)
            print("mixc SBUF bytes/partition:", K.off)
            for cb in range(6):
                wload(wic[:, :, cb * 512:(cb + 1) * 512], Bwic, wview(w_in_c, 0, 1024, cb * 512, 512))
            wload(woc, Bwoc, wview(w_out_c, 0, 1024, 0, 1024))
            for k in range(3):
                for j in range(8):
                    idx = k * 8 + j
                    S.op("dve", lambda e, idx=idx: e.tensor_scalar(out=diagc[:, idx, :], in0=identf, scalar1=pcol[:, C_CWC + idx:C_CWC + idx + 1], scalar2=None, op0=ALU.mult),
                         reads=[Bidf, Bpc], writes=[Bdiagc])
            S.op("dve", lambda e: e.memset(chb[:, :, 0:2], 0.0), writes=[Bchb])
            S.dma("sp", lambda e: [e.dma_start(out=sci, in_=ssc_d)], Bsci, writes=[Bsci])
            for j4 in range(2):
                bk, Bbk = K.bank()

                def trc(e, bk=bk, j4=j4):
                    ins = None
                    for jj in range(4):
                        j = j4 * 4 + jj
                        ins = e.transpose(bk[:, jj * 32:(jj + 1) * 32], sci[:, j * 128:(j + 1) * 128], identf[0:32, 0:32])
                    return ins
                S.op("pe", trc, reads=[Bsci, Bidf], writes=[Bbk])
                evac_copy(chs[:, j4 * 4:(j4 + 1) * 4, :, 0:2], bk[:, 0:128].rearrange("p (j b k) -> p j b k", j=4, b=16), Bbk, Bchs)
            for (t0, W) in TILES512:
                is_p = t0 < NP
                rmsnorm(t0, W, C_GMIX + 8, hn, Bhn, sq, Bsq, rstd, Brstd)
                ptail = []
                for j in range(8):
                    cgs, Bcgs = cgs2[j % 2]
                    ysb, Bysb = ysb2[j % 2]
                    bkb, Bbkb = K.bank()
                    mm_group(bkb[:, 0:W], [(wic[:, kc, j * 128:(j + 1) * 128], hn[:, kc, 0:W]) for kc in range(8)], Bbkb, [Bwic, Bhn])
                    S.op("act", lambda e, bkb=bkb, W=W: e.activation(out=ysb[:, 0:W], in_=bkb[:, 0:W], func=AF.Copy), reads=[Bbkb], writes=[Bysb])
                    bkc, Bbkc = K.bank()
                    mm_group(bkc[:, 0:W], [(wic[:, kc, 1024 + j * 128:1024 + (j + 1) * 128], hn[:, kc, 0:W]) for kc in range(8)], Bbkc, [Bwic, Bhn])
                    bkh, Bbkh = K.bank()
                    mm_group(bkh[:, 0:W], [(wic[:, kc, 2048 + j * 128:2048 + (j + 1) * 128], hn[:, kc, 0:W]) for kc in range(8)], Bbkh, [Bwic, Bhn])
                    S.op("act", lambda e, bkc=bkc, W=W: e.activation(out=cgs[:, 0:W], in_=bkc[:, 0:W], func=AF.Copy), reads=[Bbkc], writes=[Bcgs])
                    if is_p:
                        S.op("dve", lambda e, bkh=bkh, j=j, W=W: e.tensor_tensor(out=chb[:, j, 2:2 + W], in0=bkh[:, 0:W], in1=cgs[:, 0:W], op=ALU.mult), reads=[Bbkh, Bcgs], writes=[Bchb])
                        if t0 + W == NP:
                            S.op("dve", lambda e, bkh=bkh, j=j, W=W: e.tensor_tensor(out=chl[:, j, 0:2], in0=bkh[:, W - 2:W], in1=cgs[:, W - 2:W], op=ALU.mult), reads=[Bbkh, Bcgs], writes=[Bchl])
                        pairs = [(diagc[:, k * 8 + j, :], chb[:, j, k:k + W]) for k in range(3)]
                        rb = Bchb
                    else:
                        S.op("dve", lambda e, bkh=bkh, j=j: e.tensor_tensor(out=chs[:, j, :, 2:6], in0=bkh[:, 0:64].rearrange("p (b l) -> p b l", b=16),
                                                                           in1=cgs[:, 0:64].rearrange("p (b l) -> p b l", b=16), op=ALU.mult), reads=[Bbkh, Bcgs], writes=[Bchs])
                        S.op("dve", lambda e, bkh=bkh, j=j: e.tensor_tensor(out=chl[:, j, :].rearrange("p (b k) -> p b k", b=16), in0=bkh[:, 0:64].rearrange("p (b l) -> p b l", b=16)[:, :, 2:4],
                                                                           in1=cgs[:, 0:64].rearrange("p (b l) -> p b l", b=16)[:, :, 2:4], op=ALU.mult), reads=[Bbkh, Bcgs], writes=[Bchl])
                        pairs = [(diagc[:, k * 8 + j, :], chs[:, j, :, k:k + 4]) for k in range(3)]
                        rb = Bchs
                    def tail(pairs=pairs, rb=rb, ysb=ysb, Bysb=Bysb, bkb=bkb, Bbkb=Bbkb, j=j, W=W):
                        bky, Bbky = K.bank()
                        mm_group(bky[:, 0:W], pairs, Bbky, [Bdiagc, rb])
                        S.op("dve", lambda e, bky=bky, j=j, W=W: e.tensor_tensor(out=yb[:, j, 0:W], in0=bky[:, 0:W], in1=ysb[:, 0:W], op=ALU.mult), reads=[Bbky, Bysb], writes=[Byb])
                    if ptail:
                        ptail.pop()()
                    ptail.append(tail)
                if ptail:
                    ptail.pop()()
                if is_p:
                    S.op("dve", lambda e, W=W: e.tensor_copy(out=chb[:, :, 0:2], in_=chb[:, :, W:W + 2]), reads=[Bchb], writes=[Bchb])
                if (t0 + W == NP) or not is_p:
                    n = 2 if is_p else 32
                    dst = sccp_d if is_p else sccs_d
                    for j4 in range(2):
                        bk, Bbk = K.bank()

                        def trl(e, bk=bk, j4=j4, n=n):
                            ins = None
                            for jj in range(4):
                                ins = e.transpose(bk[0:n, jj * 128:(jj + 1) * 128], chl[:, j4 * 4 + jj, 0:n], identf)
                            return ins
                        S.op("pe", trl, reads=[Bchl, Bidf], writes=[Bbk])
                        S.op("dve", lambda e, bk=bk, j4=j4, n=n: e.tensor_copy(out=clo[0:n, j4 * 512:(j4 + 1) * 512], in_=bk[0:n, :]), reads=[Bbk], writes=[Bclo])
                    S.dma("sp", lambda e, n=n, dst=dst: [e.dma_start(out=dst, in_=clo[0:n, :])], Bclo, reads=[Bclo])
                    outbufs.append(Bclo)
                for oc in range(8):
                    bk, Bbk = K.bank()
                    mm_group(bk[:, 0:W], [(woc[:, kc, oc * 128:(oc + 1) * 128], yb[:, kc, 0:W]) for kc in range(8)], Bbk, [Bwoc, Byb])
                    add_to_x(oc, t0, W, bk, Bbk)

        def phase_final():
            S.barrier()
            K.off = PERSIST
            hnf, Bhnf = K.alloc("hnff", [128, 8, 512], F32)
            hnb, Bhnb = K.alloc("hnfb", [128, 8, 512], BF16)
            sq, Bsq = K.alloc("sq", [128, 8, 512], BF16)
            rstd, Brstd = K.alloc("rstd", [128, 512], F32)
            yo = [K.alloc("yo%d" % i, [128, 4, 1024], F32) for i in range(2)]
            for ti, (t0, W) in enumerate(TILES512):
                rmsnorm(t0, W, C_GFIN, None, None, sq, Bsq, rstd, Brstd, hnf, Bhnf)
                yt, Byt = yo[ti % 2]
                nc128 = (W + 127) // 128
                for c in range(nc128):
                    rows = min(128, W - c * 128)
                    for half in range(2):
                        bk, Bbk = K.bank()

                        def tro(e, bk=bk, c=c, half=half, rows=rows):
                            ins = None
                            for kk in range(4):
                                kc = half * 4 + kk
                                ins = e.transpose(bk[0:rows, kk * 128:(kk + 1) * 128], hnf[:, kc, c * 128:c * 128 + rows], identf)
                            return ins
                        S.op("pe", tro, reads=[Bhnf, Bidf], writes=[Bbk])
                        evac_copy(yt[0:rows, c, half * 512:(half + 1) * 512], bk[0:rows, :], Bbk, Byt)
                if t0 < NP:
                    S.dma("sp", lambda e, yt=yt, t0=t0: [e.dma_start(out=yp_d[t0:t0 + 512, :].rearrange("(c p) f -> p c f", p=128), in_=yt)], Byt, reads=[Byt])
                else:
                    S.dma("sp", lambda e, yt=yt: [e.dma_start(out=ys_d, in_=yt[0:64, 0, :])], Byt, reads=[Byt])
                outbufs.append(Byt)

        seq = [("attn0", lambda: phase_attn(0)), ("ffn0", lambda: phase_ffn(False)), ("mixc", phase_mixc),
               ("attn1", lambda: phase_attn(1)), ("moe", lambda: phase_ffn(True)), ("final", phase_final)]
        for i, (nm, f) in enumerate(seq):
            if stop_after >= i + 2:
                f()
                if dbg and stop_after == i + 2:
                    dump_x()
        S.barrier(engines=("sp",))
        print("ops:", S.nops, "sems:", len(S.sems))
        with nc.Block() as block:
            S.emit(block)
    return nc


OUT_NAMES = ["yp", "ys", "ssmp", "ssms", "scvp", "scvs", "sccp", "sccs", "mkp", "mvp", "vs"]


def make_in_maps(inp, ncores=NCORES):
    f = lambda a: np.ascontiguousarray(a, dtype=np.float32)
    ident, tri, Rm, selm = _consts()
    pcol = _lay_pcol(inp)
    bsp = f(inp["b_spatial"][0].reshape(1, 1024))
    wsT = f(np.transpose(inp["w_spatial"][0], (2, 0, 1)).reshape(128, 1024))
    w4 = inp["w_spatial"][0][:, 0:4, 0:4]
    wblk = np.zeros((16, 4, 8, 16, 4), np.float32)
    for b in range(16):
        wblk[b, :, :, b, :] = np.transpose(w4, (2, 0, 1))
    wblk = wblk.reshape(64, 512)
    tri4 = np.zeros((16, 4, 16, 4), np.float32)
    for b in range(16):
        tri4[b, :, b, :] = np.triu(np.ones((4, 4), np.float32))
    tri4 = tri4.reshape(64, 64)
    bsps = f(np.broadcast_to(inp["b_spatial"][0][:, None, 0:4], (8, 16, 4)).reshape(1, 512))
    shared = dict(
        pcol=pcol, bsp=bsp, wsT=wsT, ident=ident, tri=tri, Rm=Rm, selm=selm, wblk=wblk, tri4=tri4, bsps=bsps,
        w_in_ab=f(inp["w_in_ab"][0]), w_out_ab=f(inp["w_out_ab"][0]),
        w_ffn_gate=f(inp["w_ffn_gate"][0]), w_ffn_up=f(inp["w_ffn_up"][0]), w_ffn_down=f(inp["w_ffn_down"][0]),
        w_in_c=f(inp["w_in_c"][0]), w_out_c=f(inp["w_out_c"][0]), w_router=f(inp["w_router"][0]),
        w_exp_gate=f(inp["w_exp_gate"][0].reshape(8 * 1024, DFF)), w_exp_up=f(inp["w_exp_up"][0].reshape(8 * 1024, DFF)),
        w_exp_down=f(inp["w_exp_down"][0].reshape(8 * DFF, 1024)),
        w_mem_q=f(inp["w_mem_q"].reshape(2048, 1024)), w_mem_k=f(inp["w_mem_k"].reshape(2048, 1024)),
        w_mem_v=f(inp["w_mem_v"].reshape(2048, 1024)), w_mem_o=f(inp["w_mem_o"].reshape(2048, 1024)),
    )
    maps = []
    for c in range(ncores):
        b0, b1 = 16 * c, 16 * c + 16
        m = dict(shared)
        m["xp"] = f(inp["x_prompt"][c])
        m["xs"] = f(inp["x_sample"][b0:b1].reshape(64, 1024))
        m["memp"] = f(inp["mem_prompt"][c])
        m["sssm"] = f(inp["state_ssm"][0, b0:b1].reshape(16 * 1024, 128))
        m["scv"] = f(inp["state_ssm_conv"][0, b0:b1].reshape(48, 1536))
        m["ssc"] = f(inp["state_sconv"][0, b0:b1].reshape(32, 1024))
        m["ck"] = f(inp["cache_mem_k"][:, b0:b1].reshape(2 * 16 * 256, 1024))
        m["cv"] = f(inp["cache_mem_v"][:, b0:b1].reshape(2 * 16 * 256, 1024))
        maps.append(m)
    return maps


def assemble(results):
    n = len(results)
    g = lambda k: [np.asarray(r[k]) for r in results]
    yp = np.stack(g("yp"), 0)
    ys = np.concatenate(g("ys"), 0).reshape(16 * n, 4, 1024)
    ssmp = np.stack(g("ssmp"), 0).reshape(1, n, 16, 64, 128)
    ssms = np.concatenate(g("ssms"), 0).reshape(1, 16 * n, 16, 64, 128)
    scvp = np.stack(g("scvp"), 0).reshape(1, n, 3, 1536)
    scvs = np.concatenate(g("scvs"), 0).reshape(1, 16 * n, 3, 1536)
    sccp = np.stack(g("sccp"), 0).reshape(1, n, 2, 1024)
    sccs = np.concatenate(g("sccs"), 0).reshape(1, 16 * n, 2, 1024)
    mkp = np.stack([a.reshape(2, 256, 4, 256) for a in g("mkp")], 1)
    mvp = np.stack([a.reshape(2, 256, 4, 256) for a in g("mvp")], 1)
    vs = np.concatenate(g("vs"), 0).reshape(1, 16 * n, 4, 1024)
    return tuple(np.ascontiguousarray(a, dtype=np.float32) for a in (yp, ys, ssmp, ssms, scvp, scvs, sccp, sccs, mkp, mvp, vs))


def kernel(**inputs):
    inp = {k: np.asarray(v) for k, v in inputs.items()}
    nc = build_program()
    in_maps = make_in_maps(inp)
    res = run_bass_kernel_spmd(nc, in_maps, core_ids=list(range(NCORES)))
    return assemble(res.results)
```

```python
import os
import types
import numpy as np
from contextlib import ExitStack
import concourse.bass as bass
import concourse.mybir as mybir
from concourse.bass_utils import run_bass_kernel_spmd

F32 = mybir.dt.float32
BF16 = mybir.dt.bfloat16
AF = mybir.ActivationFunctionType
ALU = mybir.AluOpType
AX = mybir.AxisListType

NCORES = 8
D = 1024
NP = 2048
NS = 64
NT = NP + NS
EPS = 1e-6
DFF = 2816
NEXP = 8

C_GMIX, C_GMEM, C_GFFN, C_GFIN = 0, 16, 32, 48
C_CWS, C_CBS, C_GN, C_CWC = 56, 104, 116, 124
C_DTB, C_ALOG, C_DSK = 148, 149, 150
NPCOL = 166


class Buf:
    __slots__ = ("name", "last_w", "readers", "dsem", "dcount", "excl")

    def __init__(self, name, excl=False):
        self.name = name
        self.excl = excl
        self.last_w = None
        self.readers = {}
        self.dsem = None
        self.dcount = 0


def _freeze(fn):
    if fn.__closure__ is None:
        return fn
    cells = []
    for c in fn.__closure__:
        try:
            cells.append(types.CellType(c.cell_contents))
        except ValueError:
            cells.append(c)
    g = types.FunctionType(fn.__code__, fn.__globals__, fn.__name__, fn.__defaults__, tuple(cells))
    g.__kwdefaults__ = fn.__kwdefaults__
    return g


class Sched:
    ENG = ("pe", "act", "dve", "pool", "sp")

    def __init__(self, nc, stack):
        self.nc = nc
        self.prog = {e: [] for e in self.ENG}
        self.count = {e: 0 for e in self.ENG}
        self.seen = {e: {} for e in self.ENG}
        self.sems = {}
        self.dbufs = []
        self._stack = stack
        self.nops = 0

    def _sem(self, key):
        if key not in self.sems:
            self.sems[key] = self._stack.enter_context(self.nc.semaphore("s%d" % len(self.sems)))
        return self.sems[key]

    def _deps(self, eng, reads, writes):
        need = {}

        def want(k, v):
            if v > need.get(k, 0):
                need[k] = v
        me = ("e", eng)
        for b in reads:
            if b.last_w is not None:
                want(*b.last_w)
            if b.excl:
                for k, v in b.readers.items():
                    if k != me:
                        want(k, v)
        for b in writes:
            if b.last_w is not None:
                want(*b.last_w)
            for k, v in b.readers.items():
                want(k, v)
        out = []
        seen = self.seen[eng]
        for k, v in need.items():
            if seen.get(k, 0) < v:
                seen[k] = v
                out.append((self._sem(k), v))
        return out

    def op(self, eng, fn, reads=(), writes=()):
        fn = _freeze(fn)
        waits = self._deps(eng, reads, writes)
        self.count[eng] += 1
        tick = self.count[eng]
        key = ("e", eng)
        sem = self._sem(key)
        self.nops += 1

        def run(e, fn=fn, waits=waits, sem=sem):
            for s, v in waits:
                e.wait_ge(s, v)
            ins = fn(e)
            ins.then_inc(sem, 1)
        self.prog[eng].append(run)
        for b in writes:
            b.last_w = (key, tick)
            b.readers = {}
        for b in reads:
            if b not in writes:
                b.readers[key] = tick

    def dma(self, queue, fn, sbuf, reads=(), writes=(), n=1):
        fn = _freeze(fn)
        waits = self._deps(queue, reads, writes)
        if sbuf.dsem is None:
            sbuf.dsem = ("d", len(self.dbufs))
            self.dbufs.append(sbuf)
        key = sbuf.dsem
        sem = self._sem(key)
        sbuf.dcount += 16 * n
        val = sbuf.dcount

        def run(e, fn=fn, waits=waits, sem=sem, n=n):
            for s, v in waits:
                e.wait_ge(s, v)
            inss = fn(e)
            assert len(inss) == n
            for ins in inss:
                ins.then_inc(sem, 16)
        self.prog[queue].append(run)
        for b in writes:
            b.last_w = (key, val)
            b.readers = {}
        for b in reads:
            if b not in writes:
                b.readers[key] = val

    def barrier(self, engines=None):
        targets = [(("e", e), self.count[e]) for e in ("pe", "act", "dve") if self.count[e] > 0]
        targets += [(b.dsem, b.dcount) for b in self.dbufs]
        for eng in (engines or self.ENG):
            seen = self.seen[eng]
            waits = []
            for k, v in targets:
                if seen.get(k, 0) < v:
                    seen[k] = v
                    waits.append((self._sem(k), v))

            def run(e, waits=waits):
                for s, v in waits:
                    e.wait_ge(s, v)
            self.prog[eng].append(run)

    def emit(self, block):
        prog = self.prog

        @block.tensor
        def _(e):
            for f in prog["pe"]:
                f(e)

        @block.scalar
        def _(e):
            for f in prog["act"]:
                f(e)

        @block.vector
        def _(e):
            for f in prog["dve"]:
                f(e)

        @block.gpsimd
        def _(e):
            for f in prog["pool"]:
                f(e)

        @block.sync
        def _(e):
            for f in prog["sp"]:
                f(e)


class Ctx:
    def __init__(self, nc, stack):
        self.nc = nc
        self.st = stack
        self.S = Sched(nc, stack)
        self.arena_words = 52224
        self.arena = stack.enter_context(nc.sbuf_tensor("arena", [128, self.arena_words], F32))
        self.off = 0
        self.banks = []
        for i in range(8):
            t = stack.enter_context(nc.psum_tensor("bank%d" % i, [128, 512], F32))
            self.banks.append((t, Buf("bank%d" % i, excl=True)))
        self.bi = 0
        self.evi = 0

    def alloc(self, name, shape, dt):
        esz = 4 if dt == F32 else 2
        n = 1
        for s in shape[1:]:
            n *= s
        nbytes = (n * esz + 3) // 4 * 4
        w0 = self.off // 4
        nw = nbytes // 4
        assert w0 + nw <= self.arena_words, ("SBUF arena overflow", name, self.off, nbytes)
        ap = self.arena[:, w0:w0 + nw]
        if dt != F32:
            ap = ap.bitcast(dt)
        if shape[0] != 128:
            ap = ap[0:shape[0]]
        if len(shape) == 3:
            ap = ap.rearrange("p (a b) -> p a b", a=shape[1])
        elif len(shape) == 4:
            ap = ap.rearrange("p (a b c) -> p a b c", a=shape[1], b=shape[2])
        self.off += nbytes
        return ap, Buf(name)

    def bank(self):
        t, b = self.banks[self.bi]
        self.bi = (self.bi + 1) % 8
        return t, b


def _lay_pcol(inp):
    pc = np.zeros((128, NPCOL), np.float32)

    def cols(v):
        return np.ascontiguousarray(v.reshape(-1, 128).T)
    for l in range(2):
        pc[:, C_GMIX + 8 * l:C_GMIX + 8 * l + 8] = cols(inp["g_mix"][l])
        pc[:, C_GMEM + 8 * l:C_GMEM + 8 * l + 8] = cols(inp["g_mem"][l])
        pc[:, C_GFFN + 8 * l:C_GFFN + 8 * l + 8] = cols(inp["g_ffn"][l])
    pc[:, C_GFIN:C_GFIN + 8] = cols(inp["g_final"])
    for k in range(4):
        pc[:, C_CWS + 12 * k:C_CWS + 12 * k + 12] = cols(inp["conv_w_ssm"][0, k])
    pc[:, C_CBS:C_CBS + 12] = cols(inp["conv_b_ssm"][0])
    pc[:, C_GN:C_GN + 8] = cols(inp["g_ssm_norm"][0])
    for k in range(3):
        pc[:, C_CWC + 8 * k:C_CWC + 8 * k + 8] = cols(inp["conv_w_c"][0, k])
    pc[0:16, C_DTB] = inp["dt_bias"][0]
    pc[0:16, C_ALOG] = inp["a_log"][0]
    pc[:, C_DSK:C_DSK + 16] = np.broadcast_to(inp["d_skip"][0][None, :], (128, 16))
    return pc


def _consts():
    ident = np.eye(128, dtype=np.float32)
    tri = np.triu(np.ones((128, 128), np.float32))
    R = np.zeros((16, 8, 128), np.float32)
    for j in range(8):
        R[2 * j, j, 0:64] = 1.0
        R[2 * j + 1, j, 64:128] = 1.0
    sel = np.zeros((8, 8, 128), np.float32)
    for e in range(8):
        sel[e, e, :] = 1.0
    return ident, tri, R.reshape(16, 1024), sel.reshape(8, 1024)


def build_program(stop_after=99, dbg=False):
    nc = bass.Bass("TRN2", target_bir_lowering=False)

    def din(name, shape):
        return nc.dram_tensor(name, list(shape), F32, kind="ExternalInput").ap()

    def dout(name, shape):
        return nc.dram_tensor(name, list(shape), F32, kind="ExternalOutput").ap()

    xp_d = din("xp", [NP, D])
    xs_d = din("xs", [NS, D])
    memp_d = din("memp", [256, D])
    sssm_d = din("sssm", [16 * 1024, 128])
    scv_d = din("scv", [48, 1536])
    ssc_d = din("ssc", [32, 1024])
    ck_d = din("ck", [2 * 16 * 256, 1024])
    cv_d = din("cv", [2 * 16 * 256, 1024])
    pcol_d = din("pcol", [128, NPCOL])
    bsp_d = din("bsp", [1, 1024])
    wsT_d = din("wsT", [128, 1024])
    ident_d = din("ident", [128, 128])
    tri_d = din("tri", [128, 128])
    R_d = din("Rm", [16, 1024])
    sel_d = din("selm", [8, 1024])
    wblk_d = din("wblk", [64, 512])
    tri4_d = din("tri4", [64, 64])
    bsps_d = din("bsps", [1, 512])
    w_in_ab = din("w_in_ab", [1024, 4624])
    w_out_ab = din("w_out_ab", [2048, 1024])
    w_ffn_gate = din("w_ffn_gate", [1024, DFF])
    w_ffn_up = din("w_ffn_up", [1024, DFF])
    w_ffn_down = din("w_ffn_down", [DFF, 1024])
    w_in_c = din("w_in_c", [1024, 3072])
    w_out_c = din("w_out_c", [1024, 1024])
    w_router = din("w_router", [1024, 8])
    w_exp_gate = din("w_exp_gate", [8 * 1024, DFF])
    w_exp_up = din("w_exp_up", [8 * 1024, DFF])
    w_exp_down = din("w_exp_down", [8 * DFF, 1024])
    w_mem_q = din("w_mem_q", [2 * 1024, 1024])
    w_mem_k = din("w_mem_k", [2 * 1024, 1024])
    w_mem_v = din("w_mem_v", [2 * 1024, 1024])
    w_mem_o = din("w_mem_o", [2 * 1024, 1024])

    yp_d = dout("yp", [NP, D])
    ys_d = dout("ys", [NS, D])
    ssmp_d = dout("ssmp", [1024, 128])
    ssms_d = dout("ssms", [16 * 1024, 128])
    scvp_d = dout("scvp", [3, 1536])
    scvs_d = dout("scvs", [48, 1536])
    sccp_d = dout("sccp", [2, 1024])
    sccs_d = dout("sccs", [32, 1024])
    mkp_d = dout("mkp", [2 * 256, 1024])
    mvp_d = dout("mvp", [2 * 256, 1024])
    vs_d = dout("vs", [NS, 1024])
    if dbg:
        dbg_d = dout("dbgx", [128, 8 * NT])
    w_in_ab_b = nc.dram_tensor("w_in_ab_b", [9, 128, 8 * 512], BF16, kind="Internal").ap()
    w_dt_b = nc.dram_tensor("w_dt_b", [128, 8 * 16], BF16, kind="Internal").ap()
    w_out_ab_b = nc.dram_tensor("w_out_ab_b", [8, 128, 16 * 128], BF16, kind="Internal").ap()

    with ExitStack() as st:
        K = Ctx(nc, st)
        S = K.S
        outbufs = []

        x, Bx = K.alloc("x", [128, 8, NT], F32)
        pcol, Bpc = K.alloc("pcol", [128, NPCOL], F32)
        identf, Bidf = K.alloc("identf", [128, 128], F32)
        identb, Bidb = K.alloc("identb", [128, 128], BF16)
        trif, Btrf = K.alloc("trif", [128, 128], F32)
        onesf, Bonf = K.alloc("onesf", [128, 128], F32)
        onesb, Bonb = K.alloc("onesb", [128, 128], BF16)
        acol, Bacol = K.alloc("acol", [16, 2], F32)
        PERSIST = K.off

        Bwinb = [Buf("w_in_ab_b%d" % i) for i in range(10)]
        Bwoutb = [Buf("w_out_ab_b%d" % i) for i in range(8)]
        for cb in range(9):
            S.dma("pool", lambda e, cb=cb: [e.dma_start(out=w_in_ab_b[cb].rearrange("p (kc n) -> p kc n", kc=8),
                                                        in_=w_in_ab[:, cb * 512:(cb + 1) * 512].rearrange("(kc p) n -> p kc n", p=128))], Bwinb[cb], writes=[Bwinb[cb]])
        S.dma("pool", lambda e: [e.dma_start(out=w_dt_b.rearrange("p (kc n) -> p kc n", kc=8), in_=w_in_ab[:, 4608:4624].rearrange("(kc p) n -> p kc n", p=128))], Bwinb[9], writes=[Bwinb[9]])
        for cb in range(8):
            S.dma("pool", lambda e, cb=cb: [e.dma_start(out=w_out_ab_b[cb].rearrange("p (kc n) -> p kc n", kc=16),
                                                        in_=w_out_ab[:, cb * 128:(cb + 1) * 128].rearrange("(kc p) n -> p kc n", p=128))], Bwoutb[cb], writes=[Bwoutb[cb]])
        S.dma("sp", lambda e: [e.dma_start(out=pcol, in_=pcol_d)], Bpc, writes=[Bpc])
        S.dma("sp", lambda e: [e.dma_start(out=identf, in_=ident_d)], Bidf, writes=[Bidf])
        S.dma("pool", lambda e: [e.dma_start(out=identb, in_=ident_d)], Bidb, writes=[Bidb])
        S.dma("sp", lambda e: [e.dma_start(out=trif, in_=tri_d)], Btrf, writes=[Btrf])
        S.op("dve", lambda e: e.memset(onesf, 1.0), writes=[Bonf])
        S.op("dve", lambda e: e.memset(onesb, 1.0), writes=[Bonb])
        S.op("act", lambda e: e.activation(out=acol[:, 0:1], in_=pcol[0:16, C_ALOG:C_ALOG + 1], func=AF.Exp), reads=[Bpc], writes=[Bacol])
        S.op("dve", lambda e: e.tensor_scalar(out=acol[:, 1:2], in0=acol[:, 0:1], scalar1=-1.0, scalar2=None, op0=ALU.mult), reads=[Bacol], writes=[Bacol])

        def evac_copy(dst, src, rb, wb, extra_reads=()):
            K.evi += 1
            if K.evi % 2 == 0:
                S.op("act", lambda e: e.activation(out=dst, in_=src, func=AF.Copy), reads=[rb] + list(extra_reads), writes=[wb])
            else:
                S.op("dve", lambda e: e.tensor_copy(out=dst, in_=src), reads=[rb] + list(extra_reads), writes=[wb])

        def mm_group(out_ap, pairs, bankbuf, reads):
            n = len(pairs)

            def fn(e):
                ins = None
                for i, (l, r) in enumerate(pairs):
                    ins = e.matmul(out_ap, lhsT=l, rhs=r, start=(i == 0), stop=(i == n - 1))
                return ins
            S.op("pe", fn, reads=reads, writes=[bankbuf])

        def wload(dst, dbuf, src):
            S.dma("pool", lambda e: [e.dma_start(out=dst, in_=src)], dbuf, writes=[dbuf])

        def wview(w2d, r0, nrows, c0, ncols):
            return w2d[r0:r0 + nrows, c0:c0 + ncols].rearrange("(kc p) n -> p kc n", p=128)

        K.off = PERSIST
        xin = [K.alloc("xin%d" % i, [128, 4, D], F32) for i in range(2)]
        for ti in range(4):
            xi, Bxi = xin[ti % 2]
            S.dma("sp", lambda e, xi=xi, ti=ti: [e.dma_start(out=xi, in_=xp_d[ti * 512:(ti + 1) * 512, :].rearrange("(c p) f -> p c f", p=128))],
                  Bxi, writes=[Bxi])
            for kc in range(8):
                bk, Bbk = K.bank()

                def tr(e, xi=xi, bk=bk, kc=kc):
                    ins = None
                    for c in range(4):
                        ins = e.transpose(bk[:, c * 128:(c + 1) * 128], xi[:, c, kc * 128:(kc + 1) * 128], identf)
                    return ins
                S.op("pe", tr, reads=[Bxi, Bidf], writes=[Bbk])
                evac_copy(x[:, kc, ti * 512:(ti + 1) * 512], bk[:, :], Bbk, Bx)
        xi, Bxi = xin[0]
        S.dma("sp", lambda e: [e.dma_start(out=xi[0:64, 0, :], in_=xs_d)], Bxi, writes=[Bxi])
        bk, Bbk = K.bank()

        def trs(e, xi=xi, bk=bk):
            ins = None
            for kc in range(8):
                ins = e.transpose(bk[:, kc * 64:(kc + 1) * 64], xi[0:64, 0, kc * 128:(kc + 1) * 128], identf[0:64, 0:64])
            return ins
        S.op("pe", trs, reads=[Bxi, Bidf], writes=[Bbk])
        evac_copy(x[:, :, NP:NT], bk[:, :].rearrange("p (k t) -> p k t", k=8), Bbk, Bx)

        def rmsnorm(t0, W, gcol, hn, Bhn, sq, Bsq, rstd, Brstd, hnf=None, Bhnf=None):
            S.op("act", lambda e: e.activation(out=sq[:, :, 0:W], in_=x[:, :, t0:t0 + W], func=AF.Square), reads=[Bx], writes=[Bsq])
            bk, Bbk = K.bank()
            mm_group(bk[:, 0:W], [(onesb, sq[:, kc, 0:W]) for kc in range(8)], Bbk, [Bonb, Bsq])
            S.op("act", lambda e: e.activation(out=rstd[:, 0:W], in_=bk[:, 0:W], func=AF.Sqrt, bias=EPS, scale=1.0 / D), reads=[Bbk], writes=[Brstd])
            S.op("dve", lambda e: e.reciprocal(out=rstd[:, 0:W], in_=rstd[:, 0:W]), reads=[Brstd], writes=[Brstd])
            for kc in range(8):
                if hn is not None:
                    S.op("dve", lambda e, kc=kc: e.scalar_tensor_tensor(out=hn[:, kc, 0:W], in0=x[:, kc, t0:t0 + W], scalar=pcol[:, gcol + kc:gcol + kc + 1],
                                                                      in1=rstd[:, 0:W], op0=ALU.mult, op1=ALU.mult), reads=[Bx, Bpc, Brstd], writes=[Bhn])
                if hnf is not None:
                    S.op("dve", lambda e, kc=kc: e.scalar_tensor_tensor(out=hnf[:, kc, 0:W], in0=x[:, kc, t0:t0 + W], scalar=pcol[:, gcol + kc:gcol + kc + 1],
                                                                      in1=rstd[:, 0:W], op0=ALU.mult, op1=ALU.mult), reads=[Bx, Bpc, Brstd], writes=[Bhnf])

        def add_to_x(oc, t0, W, bk, Bbk):
            S.op("dve", lambda e: e.tensor_tensor(out=x[:, oc, t0:t0 + W], in0=bk[:, 0:W], in1=x[:, oc, t0:t0 + W], op=ALU.add), reads=[Bbk, Bx], writes=[Bx])

        def dump_x():
            S.barrier()
            S.dma("sp", lambda e: [e.dma_start(out=dbg_d, in_=x.rearrange("p k t -> p (k t)"))], Bx, reads=[Bx])
            outbufs.append(Bx)

        TILES512 = [(i * 512, 512) for i in range(4)] + [(NP, NS)]
        TILES_E = [(i * 448, 448) for i in range(4)] + [(1792, 320)]

        def phase1():
            S.barrier()
            K.off = PERSIST
            WT = 256
            hn, Bhn = K.alloc("hn", [128, 8, WT], BF16)
            sq, Bsq = K.alloc("sq", [128, 8, WT], BF16)
            rstd, Brstd = K.alloc("rstd", [128, WT], F32)
            wbufs = [K.alloc("w1_%d" % i, [128, 8, 512], BF16) for i in range(2)]
            wobufs = [K.alloc("wo1_%d" % i, [128, 16, 128], BF16) for i in range(2)]
            wdt, Bwdt = K.alloc("wdt", [128, 8, 16], BF16)
            u_fm, Bu = K.alloc("u_fm", [128, 8, WT], BF16)
            z_fm, Bz = K.alloc("z_fm", [128, 8, WT], BF16)
            v_tm, Bv = K.alloc("v_tm", [128, 2, 1024], BF16)
            v_f32, Bvf = K.alloc("v_f32", [64, 1024], F32)
            xlo, Bxlo = v_f32[0:48, 0:512], Bvf
            xbc, Bxbc = K.alloc("xbc", [128, 12, 3 + WT], BF16)
            xbcs, Bxbcs = K.alloc("xbcs", [128, 12, 16, 7], BF16)
            xlast, Bxl = K.alloc("xlast", [128, 12, 48], F32)
            xc, Bxc = K.alloc("xc", [128, 12, WT], BF16)
            dtf, Bdtf = K.alloc("dtf", [16, 2, WT], F32)
            _mk = K.off
            sci, Bsci = K.alloc("sci", [48, 1536], F32)
            K.off = _mk
            ycat, Bycat = K.alloc("ycat", [128, 16, WT], BF16)
            Bsci = Bycat
            diag, Bdiag = K.alloc("diag", [128, 48, 128], BF16)
            dI, BdI = K.alloc("dI", [128, 16, 128], BF16)
            wsm, Bwsm = K.alloc("wsm", [128, 8, 128], BF16)
            wsf, Bwsf = K.alloc("wsf", [128, 8, 128], BF16)
            bsp, Bbsp = K.alloc("bsp", [1, 1024], BF16)
            onesr, Bonesr = K.alloc("onesr", [1, 128], BF16)
            Sst = [K.alloc("Sst%d" % i, [128, 8, 128], F32) for i in range(2)]
            Rm, BRm = K.alloc("Rm", [16, 8, 128], F32)
            S.dma("sp", lambda e: [e.dma_start(out=Rm, in_=R_d.rearrange("h (j q) -> h j q", j=8))], BRm, writes=[BRm])
            STb, BSTb = K.alloc("STb", [128, 1024], BF16)
            xs_tm, Bxstm = K.alloc("xs_tm", [128, 1024], BF16)
            xdt_tm, Bxdt = K.alloc("xdt_tm", [128, 1024], BF16)
            xw_tm, Bxw = xdt_tm, Bxdt
            B_tm, BBtm = K.alloc("B_tm", [128, 2, 128], BF16)
            dtt, Bdtt = K.alloc("dtt", [128, 2, 16], F32)
            la_tm, Blat = K.alloc("la_tm", [128, 16], F32)
            wend, Bwend = K.alloc("wend", [128, 16], F32)
            sumd, Bsumd = K.alloc("sumd", [16, 1], F32)
            elc, Belc = K.alloc("elc", [128, 8], F32)
            rhsall, Brhs = K.alloc("rhsall", [128, 8, 128], F32)
            seg, Bseg = rhsall, Brhs
            Eb, BEb = K.alloc("Eb", [128, 8, 128], BF16)
            ELb, BELb = K.alloc("ELb", [128, 8, 128], BF16)
            CEb, BCEb = ELb, BELb
            wts, Bwts = Eb, BEb
            cbm, Bcbm = K.alloc("cbm", [128, 128], BF16)
            gated, Bgat = K.alloc("gated", [128, 4, 128], F32)
            gsq, Bgsq = K.alloc("gsq", [128, 4, 128], BF16)
            grs, Bgrs = K.alloc("grs", [128, 128], F32)
            wbf, Bwbf = K.alloc("wbf", [64, 8, 64], BF16)
            wbs, Bwbs = K.alloc("wbs", [64, 8, 64], BF16)
            tri4, Btri4 = K.alloc("tri4", [64, 64], F32)
            bsps, Bbsps = K.alloc("bsps", [1, 512], BF16)
            print("phase1 SBUF bytes/partition:", K.off)

            for k in range(4):
                for j in range(12):
                    idx = k * 12 + j
                    S.op("dve", lambda e, idx=idx: e.tensor_scalar(out=diag[:, idx, :], in0=identf, scalar1=pcol[:, C_CWS + idx:C_CWS + idx + 1], scalar2=None, op0=ALU.mult),
                         reads=[Bidf, Bpc], writes=[Bdiag])
            for h in range(16):
                S.op("dve", lambda e, h=h: e.tensor_scalar(out=dI[:, h, :], in0=identf, scalar1=pcol[:, C_DSK + h:C_DSK + h + 1], scalar2=None, op0=ALU.mult),
                     reads=[Bidf, Bpc], writes=[BdI])
            S.dma("pool", lambda e: [e.dma_start(out=wsf, in_=wsT_d.rearrange("s (g t) -> s g t", g=8))], Bwsf, writes=[Bwsf])
            S.op("dve", lambda e: e.tensor_tensor(out=wsm, in0=wsf, in1=trif.unsqueeze(1).to_broadcast([128, 8, 128]), op=ALU.mult), reads=[Bwsf, Btrf], writes=[Bwsm])
            S.dma("pool", lambda e: [e.dma_start(out=bsp, in_=bsp_d)], Bbsp, writes=[Bbsp])
            S.op("dve", lambda e: e.memset(onesr, 1.0), writes=[Bonesr])
            S.dma("pool", lambda e: [e.dma_start(out=wbf, in_=wblk_d.rearrange("r (g c) -> r g c", g=8))], Bwbf, writes=[Bwbf])
            S.dma("sp", lambda e: [e.dma_start(out=tri4, in_=tri4_d)], Btri4, writes=[Btri4])
            S.op("dve", lambda e: e.tensor_tensor(out=wbs, in0=wbf, in1=tri4.unsqueeze(1).to_broadcast([64, 8, 64]), op=ALU.mult), reads=[Bwbf, Btri4], writes=[Bwbs])
            S.dma("pool", lambda e: [e.dma_start(out=bsps, in_=bsps_d)], Bbsps, writes=[Bbsps])
            S.op("dve", lambda e: e.memset(xbc[:, :, 0:3], 0.0), writes=[Bxbc])
            S.dma("sp", lambda e: [e.dma_start(out=wdt, in_=w_dt_b.rearrange("p (kc n) -> p kc n", kc=8))], Bwdt, reads=[Bwinb[9]], writes=[Bwdt])
            S.dma("sp", lambda e: [e.dma_start(out=sci, in_=scv_d)], Bsci, writes=[Bsci])
            for j4 in range(3):
                bk, Bbk = K.bank()

                def trc(e, bk=bk, j4=j4):
                    ins = None
                    for jj in range(4):
                        j = j4 * 4 + jj
                        ins = e.transpose(bk[:, jj * 48:(jj + 1) * 48], sci[:, j * 128:(j + 1) * 128], identf[0:48, 0:48])
                    return ins
                S.op("pe", trc, reads=[Bsci, Bidf], writes=[Bbk])
                evac_copy(xbcs[:, j4 * 4:(j4 + 1) * 4, :, 0:3], bk[:, 0:192].rearrange("p (j b k) -> p j b k", j=4, b=16), Bbk, Bxbcs)

            sample_g1_bufs = tuple(Buf("sg1_%d" % i) for i in range(7))
            wi = [0]

            def next_w():
                r = wbufs[wi[0] % 2]
                wi[0] += 1
                return r
            woi = [0]

            tiles = [(i * WT, WT, True) for i in range(NP // WT)] + [(NP, NS, False)]
            sidx = [0]
            for (t0, W, is_p) in tiles:
                Q = 128 if is_p else 4
                nch = W // Q
                rmsnorm(t0, W, C_GMIX, hn, Bhn, sq, Bsq, rstd, Brstd)
                for blk in range(9):
                    wt, Bwt = next_w()
                    S.dma("sp", lambda e, wt=wt, blk=blk: [e.dma_start(out=wt, in_=w_in_ab_b[blk].rearrange("p (kc n) -> p kc n", kc=8))], Bwt, reads=[Bwinb[blk]], writes=[Bwt])
                    if blk in (2, 3):
                        vb = blk - 2
                        for c in range((W + 127) // 128):
                            rows = min(128, W - c * 128)
                            bk, Bbk = K.bank()
                            mm_group(bk[0:rows, :], [(hn[:, kc, c * 128:c * 128 + rows], wt[:, kc, :]) for kc in range(8)], Bbk, [Bhn, Bwt])
                            if is_p:
                                S.op("act", lambda e, bk=bk, c=c, vb=vb: e.activation(out=v_tm[:, c, vb * 512:(vb + 1) * 512], in_=bk[:, :], func=AF.Gelu_apprx_tanh),
                                     reads=[Bbk], writes=[Bv])
                            else:
                                S.op("act", lambda e, bk=bk, vb=vb: e.activation(out=v_f32[:, vb * 512:(vb + 1) * 512], in_=bk[0:64, :], func=AF.Gelu_apprx_tanh),
                                     reads=[Bbk], writes=[Bvf])
                                S.op("dve", lambda e, vb=vb: e.tensor_copy(out=v_tm[0:64, 0, vb * 512:(vb + 1) * 512], in_=v_f32[:, vb * 512:(vb + 1) * 512]),
                                     reads=[Bvf], writes=[Bv])
                        continue
                    for oc in range(4):
                        col = blk * 4 + oc
                        bk, Bbk = K.bank()
                        mm_group(bk[:, 0:W], [(wt[:, kc, oc * 128:(oc + 1) * 128], hn[:, kc, 0:W]) for kc in range(8)], Bbk, [Bhn, Bwt])
                        if col < 8:
                            S.op("act", lambda e, bk=bk, col=col: e.activation(out=u_fm[:, col, 0:W], in_=bk[:, 0:W], func=AF.Gelu_apprx_tanh), reads=[Bbk], writes=[Bu])
                        elif col < 24:
                            j = col - 16
                            S.op("act", lambda e, bk=bk, j=j: e.activation(out=z_fm[:, j, 0:W], in_=bk[:, 0:W], func=AF.Silu), reads=[Bbk], writes=[Bz])
                        else:
                            j = col - 24
                            if is_p:
                                S.op("act", lambda e, bk=bk, j=j: e.activation(out=xbc[:, j, 3:3 + W], in_=bk[:, 0:W], func=AF.Copy), reads=[Bbk], writes=[Bxbc])
                                if t0 + W == NP:
                                    S.op("dve", lambda e, bk=bk, j=j: e.tensor_copy(out=xlast[:, j, 0:3], in_=bk[:, W - 3:W]), reads=[Bbk], writes=[Bxl])
                            else:
                                S.op("act", lambda e, bk=bk, j=j: e.activation(out=xbcs[:, j, :, 3:7], in_=bk[:, 0:64].rearrange("p (b l) -> p b l", b=16), func=AF.Copy),
                                     reads=[Bbk], writes=[Bxbcs])
                                S.op("dve", lambda e, bk=bk, j=j: e.tensor_copy(out=xlast[:, j, :].rearrange("p (b k) -> p b k", b=16),
                                                                                 in_=bk[:, 0:64].rearrange("p (b l) -> p b l", b=16)[:, :, 1:4]), reads=[Bbk], writes=[Bxl])
                bk, Bbk = K.bank()
                mm_group(bk[0:16, 0:W], [(wdt[:, kc, :], hn[:, kc, 0:W]) for kc in range(8)], Bbk, [Bhn, Bwdt])
                S.op("act", lambda e, bk=bk: e.activation(out=dtf[:, 0, 0:W], in_=bk[0:16, 0:W], func=AF.Exp, bias=pcol[0:16, C_DTB:C_DTB + 1]), reads=[Bbk, Bpc], writes=[Bdtf])
                S.op("act", lambda e: e.activation(out=dtf[:, 0, 0:W], in_=dtf[:, 0, 0:W], func=AF.Ln, bias=1.0), reads=[Bdtf], writes=[Bdtf])
                S.op("dve", lambda e: e.tensor_scalar(out=dtf[:, 1, 0:W], in0=dtf[:, 0, 0:W], scalar1=acol[:, 1:2], scalar2=None, op0=ALU.mult), reads=[Bdtf, Bacol], writes=[Bdtf])
                for j in range(12):
                    bk, Bbk = K.bank()
                    if is_p:
                        pairs = [(diag[:, k * 12 + j, :], xbc[:, j, k:k + W]) for k in range(4)]
                        mm_group(bk[:, 0:W], pairs, Bbk, [Bdiag, Bxbc])
                    else:
                        pairs = [(diag[:, k * 12 + j, :], xbcs[:, j, :, k:k + 4]) for k in range(4)]
                        mm_group(bk[:, 0:W], pairs, Bbk, [Bdiag, Bxbcs])
                    S.op("act", lambda e, bk=bk, j=j: e.activation(out=xc[:, j, 0:W], in_=bk[:, 0:W], func=AF.Silu, bias=pcol[:, C_CBS + j:C_CBS + j + 1]), reads=[Bbk, Bpc], writes=[Bxc])
                if is_p:
                    S.op("dve", lambda e: e.tensor_copy(out=xbc[:, :, 0:3], in_=xbc[:, :, W:W + 3]), reads=[Bxbc], writes=[Bxbc])

                for c in range(nch):
                    o = c * Q
                    first = is_p and (t0 == 0 and c == 0)
                    if is_p:
                        Sc, BSc = Sst[0]
                    else:
                        b = c

                        def loadS(bb):
                            Sl, BSl = Sst[bb % 2]
                            S.dma("sp", lambda e, Sl=Sl, bb=bb: [e.dma_start(out=Sl, in_=sssm_d[bb * 1024:(bb + 1) * 1024, :].rearrange("(j q) n -> q j n", q=128))], BSl, writes=[BSl])
                        if b == 0:
                            loadS(0)
                        if b + 1 < 16:
                            loadS(b + 1)
                        Sc, BSc = Sst[b % 2]
                    for g in range(8):
                        bk, Bbk = K.bank()
                        if is_p:
                            vsl = v_tm[0:Q, c, g * 128:(g + 1) * 128]
                        else:
                            vsl = None
                        if is_p:
                            pairs = [(vsl, wsm[0:Q, g, 0:Q]), (onesr[0:1, 0:128], bsp[0:1, g * 128:g * 128 + Q])]
                            mm_group(bk[:, 0:Q], pairs, Bbk, [Bv, Bwsm, Bonesr, Bbsp])
                            S.op("dve", lambda e, bk=bk, g=g, o=o: e.tensor_tensor(out=ycat[:, g, o:o + Q], in0=bk[:, 0:Q], in1=u_fm[:, g, o:o + Q], op=ALU.mult),
                                 reads=[Bbk, Bu], writes=[Bycat])
                    bkx, Bbkx = K.bank()
                    bkxb = bkx[:, :].bitcast(BF16)

                    def trx(e, bkxb=bkxb, o=o, Q=Q):
                        ins = None
                        for j in range(8):
                            ins = e.transpose(bkxb[0:Q, j * 128:(j + 1) * 128], xc[:, j, o:o + Q], identb)
                        return ins
                    S.op("pe", trx, reads=[Bxc, Bidb], writes=[Bbkx])
                    S.op("act", lambda e, bkxb=bkxb, Q=Q: e.activation(out=xs_tm[0:Q, :], in_=bkxb[0:Q, :], func=AF.Copy), reads=[Bbkx], writes=[Bxstm])
                    bkb, Bbkb = K.bank()
                    bkbb = bkb[:, :].bitcast(BF16)

                    def trb(e, bkbb=bkbb, o=o, Q=Q):
                        ins = None
                        for g in range(2):
                            ins = e.transpose(bkbb[0:Q, g * 128:(g + 1) * 128], xc[:, 8 + g, o:o + Q], identb)
                        return ins
                    S.op("pe", trb, reads=[Bxc, Bidb], writes=[Bbkb])
                    S.op("dve", lambda e, bkbb=bkbb, Q=Q: e.tensor_copy(out=B_tm[0:Q, :, :], in_=bkbb[0:Q, 0:256].rearrange("p (g n) -> p g n", g=2)), reads=[Bbkb], writes=[BBtm])
                    bkd, Bbkd = K.bank()

                    def trd(e, bkd=bkd, o=o, Q=Q):
                        e.transpose(bkd[0:Q, 0:16], dtf[:, 0, o:o + Q], identf[0:16, 0:16])
                        return e.transpose(bkd[0:Q, 16:32], dtf[:, 1, o:o + Q], identf[0:16, 0:16])
                    S.op("pe", trd, reads=[Bdtf, Bidf], writes=[Bbkd])
                    S.op("dve", lambda e, bkd=bkd, Q=Q: e.tensor_copy(out=dtt[0:Q, :, :], in_=bkd[0:Q, 0:32].rearrange("p (a h) -> p a h", a=2)), reads=[Bbkd], writes=[Bdtt])
                    bkl, Bbkl = K.bank()

                    def mla(e, bkl=bkl, Q=Q):
                        e.matmul(bkl[0:Q, 0:16], lhsT=trif[0:Q, 0:Q], rhs=dtt[0:Q, 1, :], start=True, stop=True)
                        return e.matmul(bkl[0:Q, 16:32], lhsT=onesf[0:Q, 0:Q], rhs=dtt[0:Q, 1, :], start=True, stop=True)
                    S.op("pe", mla, reads=[Btrf, Bonf, Bdtt], writes=[Bbkl])
                    S.op("dve", lambda e, bkl=bkl, Q=Q: e.tensor_copy(out=la_tm[0:Q, :], in_=bkl[0:Q, 0:16]), reads=[Bbkl], writes=[Blat])
                    S.op("dve", lambda e, bkl=bkl, Q=Q: e.tensor_tensor(out=wend[0:Q, :], in0=bkl[0:Q, 16:32], in1=la_tm[0:Q, :], op=ALU.subtract), reads=[Bbkl, Blat], writes=[Bwend])
                    S.op("act", lambda e, Q=Q: e.activation(out=wend[0:Q, :], in_=wend[0:Q, :], func=AF.Exp), reads=[Bwend], writes=[Bwend])
                    S.op("dve", lambda e, o=o, Q=Q: e.tensor_reduce(out=sumd, in_=dtf[:, 1, o:o + Q], axis=AX.X, op=ALU.add), reads=[Bdtf], writes=[Bsumd])
                    bke, Bbke = K.bank()

                    def mel(e, bke=bke):
                        ins = None
                        for j in range(8):
                            ins = e.matmul(bke[:, j:j + 1], lhsT=Rm[:, j, :], rhs=sumd, start=True, stop=True)
                        return ins
                    S.op("pe", mel, reads=[BRm, Bsumd], writes=[Bbke])
                    S.op("act", lambda e, bke=bke: e.activation(out=elc, in_=bke[:, 0:8], func=AF.Exp), reads=[Bbke], writes=[Belc])
                    S.op("dve", lambda e, Q=Q: e.tensor_tensor(out=xdt_tm[0:Q, :].rearrange("p (h d) -> p h d", h=16), in0=xs_tm[0:Q, :].rearrange("p (h d) -> p h d", h=16),
                                                               in1=dtt[0:Q, 0, :].unsqueeze(2).to_broadcast([Q, 16, 64]), op=ALU.mult), reads=[Bxstm, Bdtt], writes=[Bxdt])
                    if not first:
                        for half in range(2):
                            bks, Bbks = K.bank()

                            def trS(e, bks=bks, half=half, Sc=Sc):
                                ins = None
                                for jj in range(4):
                                    ins = e.transpose(bks[:, jj * 128:(jj + 1) * 128], Sc[:, half * 4 + jj, :], identf)
                                return ins
                            S.op("pe", trS, reads=[BSc, Bidf], writes=[Bbks])
                            evac_copy(STb[:, half * 512:(half + 1) * 512], bks[:, :], Bbks, BSTb)
                    GB = []
                    for g in range(2):
                        c0 = 0 if is_p else g * 64
                        if is_p or g == 0:
                            bufs = (Brhs, BEb, BELb, Bcbm, Bgat, Bgsq, Bgrs)
                        else:
                            bufs = sample_g1_bufs
                        GB.append(dict(
                            rh=rhsall[0:Q, :, c0:c0 + Q], E=Eb[0:Q, :, c0:c0 + Q], EL=ELb[:, :, c0:c0 + Q], cb=cbm[0:Q, c0:c0 + Q],
                            ga=gated[:, :, c0:c0 + Q], gs=gsq[:, :, c0:c0 + Q], gr=grs[:, c0:c0 + Q],
                            Brh=bufs[0], BE=bufs[1], BEL=bufs[2], Bcb=bufs[3], Bga=bufs[4], Bgs=bufs[5], Bgr=bufs[6]))

                    def s1(g, G_):
                        rh, Brh = G_["rh"], G_["Brh"]
                        S.op("dve", lambda e, g=g, Q=Q, rh=rh: e.tensor_tensor(out=rh, in0=trif[0:Q, 0:Q].unsqueeze(1).to_broadcast([Q, 8, Q]),
                                                                              in1=dtt[0:Q, 1, g * 8:(g + 1) * 8].unsqueeze(2).to_broadcast([Q, 8, Q]), op=ALU.mult),
                             reads=[Btrf, Bdtt], writes=[Brh])
                        bkA, BbkA = K.bank()
                        if Q == 128:
                            bkB, BbkB = K.bank()

                            def mlab(e, bkA=bkA, bkB=bkB, rh=rh):
                                e.matmul(bkA[:, :], lhsT=onesf, rhs=rh[:, 0:4, :], start=True, stop=True)
                                return e.matmul(bkB[:, :], lhsT=onesf, rhs=rh[:, 4:8, :], start=True, stop=True)
                            S.op("pe", mlab, reads=[Bonf, Brh], writes=[BbkA, BbkB])
                            G_["lab"] = [(bkA[:, :].rearrange("p (h t) -> p h t", h=4), 0, 4, BbkA), (bkB[:, :].rearrange("p (h t) -> p h t", h=4), 4, 8, BbkB)]
                        else:
                            S.op("pe", lambda e, bkA=bkA, rh=rh: e.matmul(bkA[:, 0:32], lhsT=onesf[0:4, :], rhs=rh, start=True, stop=True), reads=[Bonf, Brh], writes=[BbkA])
                            G_["lab"] = [(bkA[:, 0:32].rearrange("p (h t) -> p h t", h=8), 0, 8, BbkA)]
                        bkc, Bbkc = K.bank()
                        S.op("pe", lambda e, bkc=bkc, g=g, o=o, Q=Q: e.matmul(bkc[0:Q, 0:Q], lhsT=xc[:, 8 + g, o:o + Q], rhs=xc[:, 10 + g, o:o + Q], start=True, stop=True),
                             reads=[Bxc], writes=[Bbkc])
                        G_["bkc"] = (bkc, Bbkc)

                    def s2(g, G_):
                        rh, Brh, EL, BEL, cb, Bcb = G_["rh"], G_["Brh"], G_["EL"], G_["BEL"], G_["cb"], G_["Bcb"]
                        for (lb, h0, h1, Bb_) in G_["lab"]:
                            nh = h1 - h0
                            S.op("act", lambda e, lb=lb, h0=h0, h1=h1, EL=EL: e.activation(out=EL[:, h0:h1, :], in_=lb, func=AF.Exp), reads=[Bb_], writes=[BEL])
                            S.op("dve", lambda e, lb=lb, h0=h0, h1=h1, g=g, Q=Q, nh=nh, rh=rh: e.tensor_tensor(
                                out=rh[:, h0:h1, :], in0=lb[0:Q], in1=la_tm[0:Q, g * 8 + h0:g * 8 + h1].unsqueeze(2).to_broadcast([Q, nh, Q]), op=ALU.subtract),
                                reads=[Bb_, Blat], writes=[Brh])
                        S.op("dve", lambda e, rh=rh: e.tensor_scalar(out=rh, in0=rh, scalar1=0.0, scalar2=None, op0=ALU.min), reads=[Brh], writes=[Brh])
                        bkc, Bbkc = G_["bkc"]
                        S.op("dve", lambda e, bkc=bkc, Q=Q, cb=cb: e.tensor_tensor(out=cb, in0=bkc[0:Q, 0:Q], in1=trif[0:Q, 0:Q], op=ALU.mult), reads=[Bbkc, Btrf], writes=[Bcb])

                    def s3(g, G_):
                        rh, Brh, E, BE, EL, BEL, cb, Bcb = G_["rh"], G_["Brh"], G_["E"], G_["BE"], G_["EL"], G_["BEL"], G_["cb"], G_["Bcb"]
                        S.op("act", lambda e, E=E, rh=rh: e.activation(out=E, in_=rh, func=AF.Exp), reads=[Brh], writes=[BE])
                        S.op("dve", lambda e, g=g, o=o, Q=Q, EL=EL: e.tensor_tensor(out=EL, in0=EL, in1=xc[:, 10 + g, o:o + Q].unsqueeze(1).to_broadcast([128, 8, Q]), op=ALU.mult),
                             reads=[BEL, Bxc], writes=[BEL])
                        S.op("dve", lambda e, Q=Q, E=E, cb=cb: e.tensor_tensor(out=E, in0=E, in1=cb.unsqueeze(1).to_broadcast([Q, 8, Q]), op=ALU.mult),
                             reads=[BE, Bcb], writes=[BE])

                    def s4(g, G_):
                        E, BE, EL, BEL, ga, Bga, gs, Bgs = G_["E"], G_["BE"], G_["EL"], G_["BEL"], G_["ga"], G_["Bga"], G_["gs"], G_["Bgs"]
                        bky, Bbky = K.bank()
                        for jj in range(4):
                            j = g * 4 + jj

                            def my(e, bky=bky, jj=jj, j=j, Q=Q, first=first, E=E, EL=EL):
                                ins = None
                                for hh in range(2):
                                    hl = jj * 2 + hh
                                    h = j * 2 + hh
                                    outp = bky[hh * 64:(hh + 1) * 64, jj * Q:(jj + 1) * Q]
                                    e.matmul(outp, lhsT=xdt_tm[0:Q, h * 64:(h + 1) * 64], rhs=E[:, hl, :], start=True, stop=False)
                                    ins = e.matmul(outp, lhsT=xs_tm[0:Q, h * 64:(h + 1) * 64], rhs=dI[0:Q, h, 0:Q], start=False, stop=first)
                                    if not first:
                                        ins = e.matmul(outp, lhsT=STb[:, h * 64:(h + 1) * 64], rhs=EL[:, hl, :], start=False, stop=True)
                                return ins
                            S.op("pe", my, reads=[Bxdt, BE, Bxstm, BdI, BSTb, BEL], writes=[Bbky])
                        S.op("dve", lambda e, bky=bky, g=g, o=o, Q=Q, ga=ga: e.tensor_tensor(out=ga, in0=bky[:, 0:4 * Q].rearrange("p (j t) -> p j t", j=4),
                                                                                          in1=z_fm[:, g * 4:(g + 1) * 4, o:o + Q], op=ALU.mult), reads=[Bbky, Bz], writes=[Bga])
                        S.op("act", lambda e, gs=gs, ga=ga: e.activation(out=gs, in_=ga, func=AF.Square), reads=[Bga], writes=[Bgs])

                    def s5(g, G_):
                        ga, Bga, gs, Bgs, gr, Bgr = G_["ga"], G_["Bga"], G_["gs"], G_["Bgs"], G_["gr"], G_["Bgr"]
                        bkr, Bbkr = K.bank()
                        mm_group(bkr[:, 0:Q], [(onesb, gs[:, jj, :]) for jj in range(4)], Bbkr, [Bonb, Bgs])
                        S.op("act", lambda e, bkr=bkr, Q=Q, gr=gr: e.activation(out=gr, in_=bkr[:, 0:Q], func=AF.Sqrt, bias=EPS, scale=1.0 / 512.0), reads=[Bbkr], writes=[Bgr])
                        S.op("dve", lambda e, gr=gr: e.reciprocal(out=gr, in_=gr), reads=[Bgr], writes=[Bgr])
                        for jj in range(4):
                            j = g * 4 + jj
                            S.op("dve", lambda e, jj=jj, j=j, o=o, Q=Q, ga=ga, gr=gr: e.scalar_tensor_tensor(out=ycat[:, 8 + j, o:o + Q], in0=ga[:, jj, :], scalar=pcol[:, C_GN + j:C_GN + j + 1],
                                                                                                            in1=gr, op0=ALU.mult, op1=ALU.mult), reads=[Bga, Bpc, Bgr], writes=[Bycat])
                    stages = (s1, s2, s3, s4, s5)
                    if is_p:
                        s1(0, GB[0])
                        s1(1, GB[1])
                        for g in range(2):
                            for st_ in stages[1:]:
                                st_(g, GB[g])
                    else:
                        for st_ in stages:
                            for g in range(2):
                                st_(g, GB[g])
                    S.op("dve", lambda e, Q=Q: e.tensor_tensor(out=xw_tm[0:Q, :].rearrange("p (h d) -> p h d", h=16), in0=xdt_tm[0:Q, :].rearrange("p (h d) -> p h d", h=16),
                                                               in1=wend[0:Q, :].unsqueeze(2).to_broadcast([Q, 16, 64]), op=ALU.mult), reads=[Bxdt, Bwend], writes=[Bxw])
                    for half in range(2):
                        bku, Bbku = K.bank()

                        def mu(e, bku=bku, half=half, Q=Q):
                            ins = None
                            for jj in range(4):
                                j = half * 4 + jj
                                ins = e.matmul(bku[:, jj * 128:(jj + 1) * 128], lhsT=xw_tm[0:Q, j * 128:(j + 1) * 128], rhs=B_tm[0:Q, j // 4, :], start=True, stop=True)
                            return ins
                        S.op("pe", mu, reads=[Bxw, BBtm], writes=[Bbku])
                        for jj in range(4):
                            j = half * 4 + jj
                            if first:
                                S.op("dve", lambda e, bku=bku, jj=jj, j=j, Sc=Sc: e.tensor_copy(out=Sc[:, j, :], in_=bku[:, jj * 128:(jj + 1) * 128]), reads=[Bbku], writes=[BSc])
                            else:
                                S.op("dve", lambda e, bku=bku, jj=jj, j=j, Sc=Sc: e.scalar_tensor_tensor(out=Sc[:, j, :], in0=Sc[:, j, :], scalar=elc[:, j:j + 1], in1=bku[:, jj * 128:(jj + 1) * 128],
                                                                                                        op0=ALU.mult, op1=ALU.add), reads=[BSc, Belc, Bbku], writes=[BSc])
                    if not is_p:
                        S.dma("sp", lambda e, Sc=Sc, b=b: [e.dma_start(out=ssms_d[b * 1024:(b + 1) * 1024, :].rearrange("(j q) n -> q j n", q=128), in_=Sc)], BSc, reads=[BSc])
                        outbufs.append(BSc)
                    elif t0 + o + Q == NP:
                        S.dma("sp", lambda e, Sc=Sc: [e.dma_start(out=ssmp_d.rearrange("(j q) n -> q j n", q=128), in_=Sc)], BSc, reads=[BSc])
                        outbufs.append(BSc)

                if not is_p:
                    for g in range(8):
                        bk, Bbk = K.bank()
                        pairs = [(v_tm[0:64, 0, g * 128:(g + 1) * 128], wbs[:, g, :]), (onesr[0:1, 0:128], bsps[0:1, g * 64:(g + 1) * 64])]
                        mm_group(bk[:, 0:64], pairs, Bbk, [Bv, Bwbs, Bonesr, Bbsps])
                        S.op("dve", lambda e, bk=bk, g=g: e.tensor_tensor(out=ycat[:, g, 0:64], in0=bk[:, 0:64], in1=u_fm[:, g, 0:64], op=ALU.mult),
                             reads=[Bbk, Bu], writes=[Bycat])
                    S.dma("sp", lambda e: [e.dma_start(out=vs_d, in_=v_f32)], Bvf, reads=[Bvf])
                    outbufs.append(Bvf)
                if (t0 + W == NP) or not is_p:
                    n = 3 if is_p else 48
                    dst = scvp_d if is_p else scvs_d
                    for j4 in range(3):
                        bk, Bbk = K.bank()

                        def trl(e, bk=bk, j4=j4, n=n):
                            ins = None
                            for jj in range(4):
                                ins = e.transpose(bk[0:n, jj * 128:(jj + 1) * 128], xlast[:, j4 * 4 + jj, 0:n], identf)
                            return ins
                        S.op("pe", trl, reads=[Bxl, Bidf], writes=[Bbk])
                        S.op("dve", lambda e, bk=bk, n=n: e.tensor_copy(out=xlo[0:n, :], in_=bk[0:n, :]), reads=[Bbk], writes=[Bxlo])
                        S.dma("sp", lambda e, n=n, dst=dst, j4=j4: [e.dma_start(out=dst[:, j4 * 512:(j4 + 1) * 512], in_=xlo[0:n, :])], Bxlo, reads=[Bxlo])
                    outbufs.append(Bxlo)

                for oc in range(8):
                    wo, Bwo = wobufs[woi[0] % 2]
                    woi[0] += 1
                    S.dma("sp", lambda e, wo=wo, oc=oc: [e.dma_start(out=wo, in_=w_out_ab_b[oc].rearrange("p (kc n) -> p kc n", kc=16))], Bwo, reads=[Bwoutb[oc]], writes=[Bwo])
                    bk, Bbk = K.bank()
                    mm_group(bk[:, 0:W], [(wo[:, kc, :], ycat[:, kc, 0:W]) for kc in range(16)], Bbk, [Bwo, Bycat])
                    add_to_x(oc, t0, W, bk, Bbk)
            return dict(v_f32=(v_f32, Bvf), xlast=(xlast, Bxl))

        if stop_after >= 1:
            p1 = phase1()
        elif dbg:
            dump_x()

        if dbg and stop_after == 1:
            dump_x()

        def phase_attn(l):
            S.barrier()
            K.off = PERSIST
            hn, Bhn = K.alloc("hn", [128, 8, 512], BF16)
            sq, Bsq = K.alloc("sq", [128, 8, 512], BF16)
            rstd, Brstd = K.alloc("rstd", [128, 512], F32)
            wq, Bwq = K.alloc("wq", [128, 8, 1024], BF16)
            wo, Bwo = K.alloc("wo", [128, 8, 1024], BF16)
            wkv = [K.alloc("wkv%d" % i, [128, 8, 512], BF16) for i in range(2)]
            Kfm, BKfm = K.alloc("Kfm", [128, 8, 256], BF16)
            Vb, BVb = K.alloc("Vb", [128, 2, 1024], BF16)
            rden2 = [K.alloc("rden%d" % i, [128, 512], F32) for i in range(2)]
            o_fm, Bo = K.alloc("o_fm", [128, 8, 512], BF16)
            _ov = K.off
            kvo = [K.alloc("kvo%d" % i, [128, 2, 1024], F32) for i in range(2)]
            mi, Bmi = K.alloc("mi", [128, 2, 1024], F32)
            _ov_end = K.off
            K.off = _ov
            q_fm, Bq = K.alloc("q_fm", [128, 8, 512], BF16)
            ET = [K.alloc("ET%d" % i, [128, 2, 512], BF16) for i in range(2)]
            Ks = [K.alloc("Ks%d" % i, [128, 2, 1024], BF16) for i in range(2)]
            Vs = [K.alloc("Vs%d" % i, [128, 2, 1024], BF16) for i in range(2)]
            K.off = max(K.off, _ov_end)
            Kfs2 = [K.alloc("Kfs%d" % i, [128, 8, 256], BF16) for i in range(2)]
            ETs2 = [K.alloc("ETs%d" % i, [128, 32], BF16) for i in range(2)]
            rds2 = [K.alloc("rds%d" % i, [128, 16], F32) for i in range(2)]
            memT, BmemT = K.alloc("memT", [128, 8, 256], BF16)
            print("attn SBUF bytes/partition:", K.off)
            S.dma("sp", lambda e: [e.dma_start(out=mi, in_=memp_d.rearrange("(c p) f -> p c f", p=128))], Bmi, writes=[Bmi])
            for kc2 in range(4):
                bk, Bbk = K.bank()

                def trm(e, bk=bk, kc2=kc2):
                    ins = None
                    for kk in range(2):
                        for mc in range(2):
                            kc = kc2 * 2 + kk
                            ins = e.transpose(bk[:, kk * 256 + mc * 128: kk * 256 + mc * 128 + 128], mi[:, mc, kc * 128:(kc + 1) * 128], identf)
                    return ins
                S.op("pe", trm, reads=[Bmi, Bidf], writes=[Bbk])
                evac_copy(memT[:, kc2 * 2:kc2 * 2 + 2, :], bk[:, :].rearrange("p (k m) -> p k m", k=2), Bbk, BmemT)

            SC = 1.0 / 16.0
            wload(wq, Bwq, wview(w_mem_q, l * 1024, 1024, 0, 1024))
            for which in range(2):
                wsrc = w_mem_k if which == 0 else w_mem_v
                dstd = mkp_d if which == 0 else mvp_d
                ko, Bko = kvo[which]
                for cb in range(2):
                    wt, Bwt = wkv[cb]
                    wload(wt, Bwt, wview(wsrc, l * 1024, 1024, cb * 512, 512))
                    for mc in range(2):
                        bk, Bbk = K.bank()
                        mm_group(bk[:, :], [(memT[:, kc, mc * 128:(mc + 1) * 128], wt[:, kc, :]) for kc in range(8)], Bbk, [BmemT, Bwt])
                        evac_copy(ko[:, mc, cb * 512:(cb + 1) * 512], bk[:, :], Bbk, Bko)
                        if which == 1:
                            S.op("act", lambda e, bk=bk, mc=mc, cb=cb: e.activation(out=Vb[:, mc, cb * 512:(cb + 1) * 512], in_=bk[:, :], func=AF.Copy), reads=[Bbk], writes=[BVb])
                    if which == 0:
                        for oc in range(4):
                            bk, Bbk = K.bank()
                            mm_group(bk[:, 0:256], [(wt[:, kc, oc * 128:(oc + 1) * 128], memT[:, kc, :]) for kc in range(8)], Bbk, [BmemT, Bwt])
                            evac_copy(Kfm[:, cb * 4 + oc, :], bk[:, 0:256], Bbk, BKfm)
                S.dma("sp", lambda e, ko=ko, dstd=dstd: [e.dma_start(out=dstd[l * 256:(l + 1) * 256, :].rearrange("(mc p) d -> p mc d", p=128), in_=ko)], Bko, reads=[Bko])
                outbufs.append(Bko)
            wload(wo, Bwo, wview(w_mem_o, l * 1024, 1024, 0, 1024))
            S.barrier()

            for (t0, W) in TILES512:
                is_p = t0 < NP
                rmsnorm(t0, W, C_GMEM + 8 * l, hn, Bhn, sq, Bsq, rstd, Brstd)
                for oc in range(8):
                    bk, Bbk = K.bank()
                    mm_group(bk[:, 0:W], [(wq[:, kc, oc * 128:(oc + 1) * 128], hn[:, kc, 0:W]) for kc in range(8)], Bbk, [Bwq, Bhn])
                    evac_copy(q_fm[:, oc, 0:W], bk[:, 0:W], Bbk, Bq)
                if is_p:
                    def scores(h):
                        Et, BEt = ET[h % 2]
                        for mc in range(2):
                            bk, Bbk = K.bank()
                            mm_group(bk[:, 0:W], [(Kfm[:, 2 * h + ec, mc * 128:(mc + 1) * 128], q_fm[:, 2 * h + ec, 0:W]) for ec in range(2)], Bbk, [BKfm, Bq])
                            S.op("act", lambda e, bk=bk, Et=Et, mc=mc: e.activation(out=Et[:, mc, 0:W], in_=bk[:, 0:W], func=AF.Exp, scale=SC), reads=[Bbk], writes=[BEt])
                    scores(0)
                    for h in range(4):
                        Et, BEt = ET[h % 2]
                        rden, Brden = rden2[h % 2]
                        if h + 1 < 4:
                            scores(h + 1)
                        bk, Bbk = K.bank()
                        mm_group(bk[:, 0:W], [(onesb, Et[:, mc, 0:W]) for mc in range(2)], Bbk, [Bonb, BEt])
                        S.op("dve", lambda e, bk=bk: e.reciprocal(out=rden[:, 0:W], in_=bk[:, 0:W]), reads=[Bbk], writes=[Brden])
                        for ec in range(2):
                            bk, Bbk = K.bank()
                            mm_group(bk[:, 0:W], [(Vb[:, mc, (2 * h + ec) * 128:(2 * h + ec + 1) * 128], Et[:, mc, 0:W]) for mc in range(2)], Bbk, [BVb, BEt])
                            S.op("dve", lambda e, bk=bk, h=h, ec=ec: e.tensor_tensor(out=o_fm[:, 2 * h + ec, 0:W], in0=bk[:, 0:W], in1=rden[:, 0:W], op=ALU.mult),
                                 reads=[Bbk, Brden], writes=[Bo])
                else:
                    def prep(b):
                        Kb, BKb = Ks[b % 2]
                        Vs_, BVs = Vs[b % 2]
                        Kfs, BKfs = Kfs2[b % 2]
                        r0 = (l * 16 + b) * 256
                        wload(Kb, BKb, ck_d[r0:r0 + 256, :].rearrange("(mc p) d -> p mc d", p=128))
                        wload(Vs_, BVs, cv_d[r0:r0 + 256, :].rearrange("(mc p) d -> p mc d", p=128))
                        for kc4 in range(2):
                            bk, Bbk = K.bank()
                            bkb = bk[:, :].bitcast(BF16)

                            def trk(e, bkb=bkb, Kb=Kb, kc4=kc4):
                                ins = None
                                for kk in range(4):
                                    for mc in range(2):
                                        kc = kc4 * 4 + kk
                                        ins = e.transpose(bkb[:, kk * 256 + mc * 128:kk * 256 + mc * 128 + 128], Kb[:, mc, kc * 128:(kc + 1) * 128], identb)
                                return ins
                            S.op("pe", trk, reads=[BKb, Bidb], writes=[Bbk])
                            evac_copy(Kfs[:, kc4 * 4:(kc4 + 1) * 4, :], bkb.rearrange("p (k m) -> p k m", k=4), Bbk, BKfs)

                    def score(b):
                        Kfs, BKfs = Kfs2[b % 2]
                        ETs, BETs = ETs2[b % 2]
                        bk, Bbk = K.bank()

                        def msc(e, bk=bk, b=b, Kfs=Kfs):
                            ins = None
                            for h in range(4):
                                for mc in range(2):
                                    c0 = (h * 2 + mc) * 4
                                    for ec in range(2):
                                        ins = e.matmul(bk[:, c0:c0 + 4], lhsT=Kfs[:, 2 * h + ec, mc * 128:(mc + 1) * 128], rhs=q_fm[:, 2 * h + ec, b * 4:b * 4 + 4],
                                                       start=(ec == 0), stop=(ec == 1))
                            return ins
                        S.op("pe", msc, reads=[BKfs, Bq], writes=[Bbk])
                        S.op("act", lambda e, bk=bk, ETs=ETs: e.activation(out=ETs, in_=bk[:, 0:32], func=AF.Exp, scale=SC), reads=[Bbk], writes=[BETs])

                    def pv(b):
                        Vs_, BVs = Vs[b % 2]
                        ETs, BETs = ETs2[b % 2]
                        rds, Brds = rds2[b % 2]
                        bk2, Bbk2 = K.bank()

                        def mpv(e, bk2=bk2, Vs_=Vs_, ETs=ETs):
                            ins = None
                            for h in range(4):
                                for mc in range(2):
                                    c0 = (h * 2 + mc) * 4
                                    ins = e.matmul(bk2[:, h * 4:h * 4 + 4], lhsT=onesb, rhs=ETs[:, c0:c0 + 4], start=(mc == 0), stop=(mc == 1))
                                for ec in range(2):
                                    for mc in range(2):
                                        c0 = (h * 2 + mc) * 4
                                        o0 = 16 + (2 * h + ec) * 4
                                        ins = e.matmul(bk2[:, o0:o0 + 4], lhsT=Vs_[:, mc, (2 * h + ec) * 128:(2 * h + ec + 1) * 128], rhs=ETs[:, c0:c0 + 4],
                                                       start=(mc == 0), stop=(mc == 1))
                            return ins
                        S.op("pe", mpv, reads=[Bonb, BETs, BVs], writes=[Bbk2])
                        S.op("dve", lambda e, bk2=bk2, rds=rds: e.reciprocal(out=rds, in_=bk2[:, 0:16]), reads=[Bbk2], writes=[Brds])
                        S.op("dve", lambda e, bk2=bk2, b=b, rds=rds: e.tensor_tensor(out=o_fm[:, :, b * 4:b * 4 + 4].rearrange("p (h c) l -> p h c l", h=4),
                                                                                     in0=bk2[:, 16:48].rearrange("p (h c l) -> p h c l", h=4, c=2),
                                                                                     in1=rds.rearrange("p (h l) -> p h l", h=4).unsqueeze(2).to_broadcast([128, 4, 2, 4]), op=ALU.mult),
                             reads=[Bbk2, Brds], writes=[Bo])

                    prep(0)
                    for b in range(16):
                        if b >= 1:
                            pv(b - 1)
                        if b + 1 < 16:
                            prep(b + 1)
                        score(b)
                    pv(15)
                for oc in range(8):
                    bk, Bbk = K.bank()
                    mm_group(bk[:, 0:W], [(wo[:, kc, oc * 128:(oc + 1) * 128], o_fm[:, kc, 0:W]) for kc in range(8)], Bbk, [Bwo, Bo])
                    add_to_x(oc, t0, W, bk, Bbk)

        def phase_ffn(moe):
            S.barrier()
            K.off = PERSIST
            hn, Bhn = K.alloc("hnall", [128, 8, NT], BF16)
            G = 4
            wg = [K.alloc("wg%d" % i, [128, 8, G * 128], BF16) for i in range(2)]
            wu = [K.alloc("wu%d" % i, [128, 8, G * 128], BF16) for i in range(2)]
            wd = [K.alloc("wd%d" % i, [128, G, 1024], BF16) for i in range(2)]
            sg = [K.alloc("sg%d" % i, [128, 512], BF16) for i in range(2)]
            hh = [K.alloc("hh%d" % i, [128, G, 512], BF16) for i in range(2)]
            sq, Bsq = K.alloc("sq", [128, 8, 512], BF16)
            rstd, Brstd = K.alloc("rstd", [128, 512], F32)
            hnf = Bhnf = None
            if moe:
                wr, Bwr = K.alloc("wr", [128, 8, 8], F32)
                comb, Bcomb = K.alloc("comb", [128, 17, 8], F32)
                lg3, Blg = K.alloc("lg3", [128, 4, 8], F32)
                m13, Bm1 = K.alloc("m13", [128, 5, 4], F32)
                mk13, Bmk1 = K.alloc("mk13", [128, 4, 8], F32)
                mk23, Bmk2 = K.alloc("mk23", [128, 4, 8], F32)
                l23, Bl2 = K.alloc("l23", [128, 4, 8], F32)
                combT, BcombT = K.alloc("combT", [8, NT], BF16)
                selb, Bselb = K.alloc("selb", [8, 8, 128], BF16)
                cbc, Bcbc = K.alloc("cbc", [128, NT], BF16)
                hnf, Bhnf = K.alloc("hnf", [128, 8, 512], F32)
                S.dma("sp", lambda e: [e.dma_start(out=wr, in_=w_router.rearrange("(kc p) n -> p kc n", p=128))], Bwr, writes=[Bwr])
                S.dma("pool", lambda e: [e.dma_start(out=selb, in_=sel_d.rearrange("a (b p) -> a b p", b=8))], Bselb, writes=[Bselb])
            print("ffn SBUF bytes/partition:", K.off)
            gcol = C_GFFN + (8 if moe else 0)
            for (t0, W) in TILES512:
                rmsnorm(t0, W, gcol, hn[:, :, t0:t0 + W], Bhn, sq, Bsq, rstd, Brstd, hnf, Bhnf)
                if moe:
                    nb = (W + 127) // 128
                    rows = min(128, W)
                    blk0 = t0 // 128
                    bk, Bbk = K.bank()

                    def mrt(e, bk=bk, nb=nb, rows=rows):
                        ins = None
                        for c in range(nb):
                            for kc in range(8):
                                ins = e.matmul(bk[0:rows, c * 8:(c + 1) * 8], lhsT=hnf[:, kc, c * 128:c * 128 + rows], rhs=wr[:, kc, :], start=(kc == 0), stop=(kc == 7))
                        return ins
                    S.op("pe", mrt, reads=[Bhnf, Bwr], writes=[Bbk])
                    L3 = lg3[0:rows, 0:nb, :]
                    M1 = mk13[0:rows, 0:nb, :]
                    M2 = mk23[0:rows, 0:nb, :]
                    L2 = l23[0:rows, 0:nb, :]
                    mx = lambda i: m13[0:rows, i, 0:nb]
                    mxb = lambda i: m13[0:rows, i, 0:nb].unsqueeze(2).to_broadcast([rows, nb, 8])
                    S.op("dve", lambda e, bk=bk, L3=L3, rows=rows, nb=nb: e.tensor_copy(out=L3, in_=bk[0:rows, 0:nb * 8].rearrange("p (c e) -> p c e", c=nb)), reads=[Bbk], writes=[Blg])
                    S.op("dve", lambda e, L3=L3, o_=mx(0): e.tensor_reduce(out=o_, in_=L3, axis=AX.X, op=ALU.max), reads=[Blg], writes=[Bm1])
                    S.op("dve", lambda e, L3=L3, M1=M1, b_=mxb(0): e.tensor_tensor(out=M1, in0=L3, in1=b_, op=ALU.is_equal), reads=[Blg, Bm1], writes=[Bmk1])
                    S.op("dve", lambda e, L3=L3, M1=M1, L2=L2: e.scalar_tensor_tensor(out=L2, in0=M1, scalar=-1e30, in1=L3, op0=ALU.mult, op1=ALU.add), reads=[Bmk1, Blg], writes=[Bl2])
                    S.op("dve", lambda e, L2=L2, o_=mx(1): e.tensor_reduce(out=o_, in_=L2, axis=AX.X, op=ALU.max), reads=[Bl2], writes=[Bm1])
                    S.op("dve", lambda e, L2=L2, M2=M2, b_=mxb(1): e.tensor_tensor(out=M2, in0=L2, in1=b_, op=ALU.is_equal), reads=[Bl2, Bm1], writes=[Bmk2])
                    S.op("dve", lambda e, a_=mx(2), b_=mx(1), c_=mx(0): e.tensor_tensor(out=a_, in0=b_, in1=c_, op=ALU.subtract), reads=[Bm1], writes=[Bm1])
                    S.op("act", lambda e, a_=mx(3), b_=mx(2): e.activation(out=a_, in_=b_, func=AF.Sigmoid), reads=[Bm1], writes=[Bm1])
                    S.op("dve", lambda e, a_=mx(4), b_=mx(3): e.tensor_scalar(out=a_, in0=b_, scalar1=-1.0, scalar2=1.0, op0=ALU.mult, op1=ALU.add), reads=[Bm1], writes=[Bm1])
                    S.op("dve", lambda e, M1=M1, b_=mxb(4): e.tensor_tensor(out=M1, in0=M1, in1=b_, op=ALU.mult), reads=[Bmk1, Bm1], writes=[Bmk1])
                    S.op("dve", lambda e, M2=M2, b_=mxb(3): e.tensor_tensor(out=M2, in0=M2, in1=b_, op=ALU.mult), reads=[Bmk2, Bm1], writes=[Bmk2])
                    S.op("dve", lambda e, M1=M1, M2=M2, blk0=blk0, nb=nb, rows=rows: e.tensor_tensor(out=comb[0:rows, blk0:blk0 + nb, :], in0=M1, in1=M2, op=ALU.add),
                         reads=[Bmk1, Bmk2], writes=[Bcomb])
                    bkt, Bbkt = K.bank()

                    def trt(e, bkt=bkt, blk0=blk0, nb=nb, rows=rows):
                        ins = None
                        for c in range(nb):
                            ins = e.transpose(bkt[0:8, c * 128:c * 128 + rows], comb[0:rows, blk0 + c, :], identf[0:rows, 0:rows])
                        return ins
                    S.op("pe", trt, reads=[Bcomb, Bidf], writes=[Bbkt])
                    S.op("act", lambda e, bkt=bkt, t0=t0, W=W: e.activation(out=combT[:, t0:t0 + W], in_=bkt[0:8, 0:W], func=AF.Copy), reads=[Bbkt], writes=[BcombT])
            nexp = NEXP if moe else 1
            blocks = [(s0, min(G, 22 - s0)) for s0 in range(0, 22, G)]
            wi = 0
            hcnt = [0]
            pend = []
            for ex in range(nexp):
                if moe:
                    wgd, wud, wdd = w_exp_gate, w_exp_up, w_exp_down
                    rg0, rd0 = ex * 1024, ex * DFF
                    for (t0, W) in TILES512:
                        bk, Bbk = K.bank()
                        S.op("pe", lambda e, bk=bk, ex=ex, t0=t0, W=W: e.matmul(bk[:, 0:W], lhsT=selb[:, ex, :], rhs=combT[:, t0:t0 + W], start=True, stop=True),
                             reads=[Bselb, BcombT], writes=[Bbk])
                        S.op("act", lambda e, bk=bk, t0=t0, W=W: e.activation(out=cbc[:, t0:t0 + W], in_=bk[:, 0:W], func=AF.Copy), reads=[Bbk], writes=[Bcbc])
                else:
                    wgd, wud, wdd = w_ffn_gate, w_ffn_up, w_ffn_down
                    rg0, rd0 = 0, 0
                for (s0, ns) in blocks:
                    wgt, Bwg = wg[wi % 2]
                    wut, Bwu = wu[wi % 2]
                    wdt_, Bwd = wd[wi % 2]
                    wi += 1
                    wload(wgt[:, :, 0:ns * 128], Bwg, wview(wgd, rg0, 1024, s0 * 128, ns * 128))
                    wload(wut[:, :, 0:ns * 128], Bwu, wview(wud, rg0, 1024, s0 * 128, ns * 128))
                    wload(wdt_[:, 0:ns, :], Bwd, wview(wdd, rd0 + s0 * 128, ns * 128, 0, 1024))
                    for ti, (t0, W) in enumerate(TILES_E):
                        hht, Bhh = hh[hcnt[0] % 2]
                        hcnt[0] += 1
                        for sl in range(ns):
                            sgt, Bsg = sg[sl % 2]
                            bkg, Bbkg = K.bank()
                            mm_group(bkg[:, 0:W], [(wgt[:, kc, sl * 128:(sl + 1) * 128], hn[:, kc, t0:t0 + W]) for kc in range(8)], Bbkg, [Bwg, Bhn])
                            bku, Bbku = K.bank()
                            mm_group(bku[:, 0:W], [(wut[:, kc, sl * 128:(sl + 1) * 128], hn[:, kc, t0:t0 + W]) for kc in range(8)], Bbku, [Bwu, Bhn])
                            S.op("act", lambda e, bkg=bkg, sgt=sgt, W=W: e.activation(out=sgt[:, 0:W], in_=bkg[:, 0:W], func=AF.Silu), reads=[Bbkg], writes=[Bsg])
                            S.op("dve", lambda e, bku=bku, sgt=sgt, hht=hht, sl=sl, W=W: e.tensor_tensor(out=hht[:, sl, 0:W], in0=bku[:, 0:W], in1=sgt[:, 0:W], op=ALU.mult),
                                 reads=[Bbku, Bsg], writes=[Bhh])
                            if moe:
                                S.op("dve", lambda e, hht=hht, sl=sl, t0=t0, W=W: e.tensor_tensor(out=hht[:, sl, 0:W], in0=hht[:, sl, 0:W], in1=cbc[:, t0:t0 + W], op=ALU.mult),
                                     reads=[Bhh, Bcbc], writes=[Bhh])
                        def down(wdt_=wdt_, Bwd=Bwd, hht=hht, Bhh=Bhh, ns=ns, t0=t0, W=W):
                            for oc in range(8):
                                bk, Bbk = K.bank()
                                mm_group(bk[:, 0:W], [(wdt_[:, sl, oc * 128:(oc + 1) * 128], hht[:, sl, 0:W]) for sl in range(ns)], Bbk, [Bwd, Bhh])
                                add_to_x(oc, t0, W, bk, Bbk)
                        if pend:
                            pend.pop()()
                        pend.append(down)
            if pend:
                pend.pop()()

        def phase_mixc():
            S.barrier()
            K.off = PERSIST
            hn, Bhn = K.alloc("hn", [128, 8, 512], BF16)
            sq, Bsq = K.alloc("sq", [128, 8, 512], BF16)
            rstd, Brstd = K.alloc("rstd", [128, 512], F32)
            wic, Bwic = K.alloc("wic", [128, 8, 3072], BF16)
            woc, Bwoc = K.alloc("woc", [128, 8, 1024], BF16)
            diagc, Bdiagc = K.alloc("diagc", [128, 24, 128], BF16)
            chb, Bchb = K.alloc("chb", [128, 8, 2 + 512], BF16)
            chs, Bchs = K.alloc("chs", [128, 8, 16, 6], BF16)
            chl, Bchl = K.alloc("chl", [128, 8, 32], F32)
            cgs2 = [K.alloc("cgs%d" % i, [128, 512], F32) for i in range(2)]
            ysb2 = [K.alloc("ysb%d" % i, [128, 512], F32) for i in range(2)]
            yb, Byb = K.alloc("yb", [128, 8, 512], BF16)
            sci, Bsci = K.alloc("sci", [32, 1024], F32)
            clo, Bclo = K.alloc("clo", [32, 1024], F32)
            print("mixc SBUF bytes/partition:", K.off)
            for cb in range(6):
                wload(wic[:, :, cb * 512:(cb + 1) * 512], Bwic, wview(w_in_c, 0, 1024, cb * 512, 512))
            wload(woc, Bwoc, wview(w_out_c, 0, 1024, 0, 1024))
            for k in range(3):
                for j in range(8):
                    idx = k * 8 + j
                    S.op("dve", lambda e, idx=idx: e.tensor_scalar(out=diagc[:, idx, :], in0=identf, scalar1=pcol[:, C_CWC + idx:C_CWC + idx + 1], scalar2=None, op0=ALU.mult),
                         reads=[Bidf, Bpc], writes=[Bdiagc])
            S.op("dve", lambda e: e.memset(chb[:, :, 0:2], 0.0), writes=[Bchb])
            S.dma("sp", lambda e: [e.dma_start(out=sci, in_=ssc_d)], Bsci, writes=[Bsci])
            for j4 in range(2):
                bk, Bbk = K.bank()

                def trc(e, bk=bk, j4=j4):
                    ins = None
                    for jj in range(4):
                        j = j4 * 4 + jj
                        ins = e.transpose(bk[:, jj * 32:(jj + 1) * 32], sci[:, j * 128:(j + 1) * 128], identf[0:32, 0:32])
                    return ins
                S.op("pe", trc, reads=[Bsci, Bidf], writes=[Bbk])
                evac_copy(chs[:, j4 * 4:(j4 + 1) * 4, :, 0:2], bk[:, 0:128].rearrange("p (j b k) -> p j b k", j=4, b=16), Bbk, Bchs)
            for (t0, W) in TILES512:
                is_p = t0 < NP
                rmsnorm(t0, W, C_GMIX + 8, hn, Bhn, sq, Bsq, rstd, Brstd)
                ptail = []
                for j in range(8):
                    cgs, Bcgs = cgs2[j % 2]
                    ysb, Bysb = ysb2[j % 2]
                    bkb, Bbkb = K.bank()
                    mm_group(bkb[:, 0:W], [(wic[:, kc, j * 128:(j + 1) * 128], hn[:, kc, 0:W]) for kc in range(8)], Bbkb, [Bwic, Bhn])
                    S.op("act", lambda e, bkb=bkb, W=W: e.activation(out=ysb[:, 0:W], in_=bkb[:, 0:W], func=AF.Copy), reads=[Bbkb], writes=[Bysb])
                    bkc, Bbkc = K.bank()
                    mm_group(bkc[:, 0:W], [(wic[:, kc, 1024 + j * 128:1024 + (j + 1) * 128], hn[:, kc, 0:W]) for kc in range(8)], Bbkc, [Bwic, Bhn])
                    bkh, Bbkh = K.bank()
                    mm_group(bkh[:, 0:W], [(wic[:, kc, 2048 + j * 128:2048 + (j + 1) * 128], hn[:, kc, 0:W]) for kc in range(8)], Bbkh, [Bwic, Bhn])
                    S.op("act", lambda e, bkc=bkc, W=W: e.activation(out=cgs[:, 0:W], in_=bkc[:, 0:W], func=AF.Copy), reads=[Bbkc], writes=[Bcgs])
                    if is_p:
                        S.op("dve", lambda e, bkh=bkh, j=j, W=W: e.tensor_tensor(out=chb[:, j, 2:2 + W], in0=bkh[:, 0:W], in1=cgs[:, 0:W], op=ALU.mult), reads=[Bbkh, Bcgs], writes=[Bchb])
                        if t0 + W == NP:
                            S.op("dve", lambda e, bkh=bkh, j=j, W=W: e.tensor_tensor(out=chl[:, j, 0:2], in0=bkh[:, W - 2:W], in1=cgs[:, W - 2:W], op=ALU.mult), reads=[Bbkh, Bcgs], writes=[Bchl])
                        pairs = [(diagc[:, k * 8 + j, :], chb[:, j, k:k + W]) for k in range(3)]
                        rb = Bchb
                    else:
                        S.op("dve", lambda e, bkh=bkh, j=j: e.tensor_tensor(out=chs[:, j, :, 2:6], in0=bkh[:, 0:64].rearrange("p (b l) -> p b l", b=16),
                                                                           in1=cgs[:, 0:64].rearrange("p (b l) -> p b l", b=16), op=ALU.mult), reads=[Bbkh, Bcgs], writes=[Bchs])
                        S.op("dve", lambda e, bkh=bkh, j=j: e.tensor_tensor(out=chl[:, j, :].rearrange("p (b k) -> p b k", b=16), in0=bkh[:, 0:64].rearrange("p (b l) -> p b l", b=16)[:, :, 2:4],
                                                                           in1=cgs[:, 0:64].rearrange("p (b l) -> p b l", b=16)[:, :, 2:4], op=ALU.mult), reads=[Bbkh, Bcgs], writes=[Bchl])
                        pairs = [(diagc[:, k * 8 + j, :], chs[:, j, :, k:k + 4]) for k in range(3)]
                        rb = Bchs
                    def tail(pairs=pairs, rb=rb, ysb=ysb, Bysb=Bysb, bkb=bkb, Bbkb=Bbkb, j=j, W=W):
                        bky, Bbky = K.bank()
                        mm_group(bky[:, 0:W], pairs, Bbky, [Bdiagc, rb])
                        S.op("dve", lambda e, bky=bky, j=j, W=W: e.tensor_tensor(out=yb[:, j, 0:W], in0=bky[:, 0:W], in1=ysb[:, 0:W], op=ALU.mult), reads=[Bbky, Bysb], writes=[Byb])
                    if ptail:
                        ptail.pop()()
                    ptail.append(tail)
                if ptail:
                    ptail.pop()()
                if is_p:
                    S.op("dve", lambda e, W=W: e.tensor_copy(out=chb[:, :, 0:2], in_=chb[:, :, W:W + 2]), reads=[Bchb], writes=[Bchb])
                if (t0 + W == NP) or not is_p:
                    n = 2 if is_p else 32
                    dst = sccp_d if is_p else sccs_d
                    for j4 in range(2):
                        bk, Bbk = K.bank()

                        def trl(e, bk=bk, j4=j4, n=n):
                            ins = None
                            for jj in range(4):
                                ins = e.transpose(bk[0:n, jj * 128:(jj + 1) * 128], chl[:, j4 * 4 + jj, 0:n], identf)
                            return ins
                        S.op("pe", trl, reads=[Bchl, Bidf], writes=[Bbk])
                        S.op("dve", lambda e, bk=bk, j4=j4, n=n: e.tensor_copy(out=clo[0:n, j4 * 512:(j4 + 1) * 512], in_=bk[0:n, :]), reads=[Bbk], writes=[Bclo])
                    S.dma("sp", lambda e, n=n, dst=dst: [e.dma_start(out=dst, in_=clo[0:n, :])], Bclo, reads=[Bclo])
                    outbufs.append(Bclo)
                for oc in range(8):
                    bk, Bbk = K.bank()
                    mm_group(bk[:, 0:W], [(woc[:, kc, oc * 128:(oc + 1) * 128], yb[:, kc, 0:W]) for kc in range(8)], Bbk, [Bwoc, Byb])
                    add_to_x(oc, t0, W, bk, Bbk)

        def phase_final():
            S.barrier()
            K.off = PERSIST
            hnf, Bhnf = K.alloc("hnff", [128, 8, 512], F32)
            hnb, Bhnb = K.alloc("hnfb", [128, 8, 512], BF16)
            sq, Bsq = K.alloc("sq", [128, 8, 512], BF16)
            rstd, Brstd = K.alloc("rstd", [128, 512], F32)
            yo = [K.alloc("yo%d" % i, [128, 4, 1024], F32) for i in range(2)]
            for ti, (t0, W) in enumerate(TILES512):
                rmsnorm(t0, W, C_GFIN, None, None, sq, Bsq, rstd, Brstd, hnf, Bhnf)
                yt, Byt = yo[ti % 2]
                nc128 = (W + 127) // 128
                for c in range(nc128):
                    rows = min(128, W - c * 128)
                    for half in range(2):
                        bk, Bbk = K.bank()

                        def tro(e, bk=bk, c=c, half=half, rows=rows):
                            ins = None
                            for kk in range(4):
                                kc = half * 4 + kk
                                ins = e.transpose(bk[0:rows, kk * 128:(kk + 1) * 128], hnf[:, kc, c * 128:c * 128 + rows], identf)
                            return ins
                        S.op("pe", tro, reads=[Bhnf, Bidf], writes=[Bbk])
                        evac_copy(yt[0:rows, c, half * 512:(half + 1) * 512], bk[0:rows, :], Bbk, Byt)
                if t0 < NP:
                    S.dma("sp", lambda e, yt=yt, t0=t0: [e.dma_start(out=yp_d[t0:t0 + 512, :].rearrange("(c p) f -> p c f", p=128), in_=yt)], Byt, reads=[Byt])
                else:
                    S.dma("sp", lambda e, yt=yt: [e.dma_start(out=ys_d, in_=yt[0:64, 0, :])], Byt, reads=[Byt])
                outbufs.append(Byt)

        seq = [("attn0", lambda: phase_attn(0)), ("ffn0", lambda: phase_ffn(False)), ("mixc", phase_mixc),
               ("attn1", lambda: phase_attn(1)), ("moe", lambda: phase_ffn(True)), ("final", phase_final)]
        for i, (nm, f) in enumerate(seq):
            if stop_after >= i + 2:
                f()
                if dbg and stop_after == i + 2:
                    dump_x()
        S.barrier(engines=("sp",))
        print("ops:", S.nops, "sems:", len(S.sems))
        with nc.Block() as block:
            S.emit(block)
    return nc


OUT_NAMES = ["yp", "ys", "ssmp", "ssms", "scvp", "scvs", "sccp", "sccs", "mkp", "mvp", "vs"]


def make_in_maps(inp, ncores=NCORES):
    f = lambda a: np.ascontiguousarray(a, dtype=np.float32)
    ident, tri, Rm, selm = _consts()
    pcol = _lay_pcol(inp)
    bsp = f(inp["b_spatial"][0].reshape(1, 1024))
    wsT = f(np.transpose(inp["w_spatial"][0], (2, 0, 1)).reshape(128, 1024))
    w4 = inp["w_spatial"][0][:, 0:4, 0:4]
    wblk = np.zeros((16, 4, 8, 16, 4), np.float32)
    for b in range(16):
        wblk[b, :, :, b, :] = np.transpose(w4, (2, 0, 1))
    wblk = wblk.reshape(64, 512)
    tri4 = np.zeros((16, 4, 16, 4), np.float32)
    for b in range(16):
        tri4[b, :, b, :] = np.triu(np.ones((4, 4), np.float32))
    tri4 = tri4.reshape(64, 64)
    bsps = f(np.broadcast_to(inp["b_spatial"][0][:, None, 0:4], (8, 16, 4)).reshape(1, 512))
    shared = dict(
        pcol=pcol, bsp=bsp, wsT=wsT, ident=ident, tri=tri, Rm=Rm, selm=selm, wblk=wblk, tri4=tri4, bsps=bsps,
        w_in_ab=f(inp["w_in_ab"][0]), w_out_ab=f(inp["w_out_ab"][0]),
        w_ffn_gate=f(inp["w_ffn_gate"][0]), w_ffn_up=f(inp["w_ffn_up"][0]), w_ffn_down=f(inp["w_ffn_down"][0]),
        w_in_c=f(inp["w_in_c"][0]), w_out_c=f(inp["w_out_c"][0]), w_router=f(inp["w_router"][0]),
        w_exp_gate=f(inp["w_exp_gate"][0].reshape(8 * 1024, DFF)), w_exp_up=f(inp["w_exp_up"][0].reshape(8 * 1024, DFF)),
        w_exp_down=f(inp["w_exp_down"][0].reshape(8 * DFF, 1024)),
        w_mem_q=f(inp["w_mem_q"].reshape(2048, 1024)), w_mem_k=f(inp["w_mem_k"].reshape(2048, 1024)),
        w_mem_v=f(inp["w_mem_v"].reshape(2048, 1024)), w_mem_o=f(inp["w_mem_o"].reshape(2048, 1024)),
    )
    maps = []
    for c in range(ncores):
        b0, b1 = 16 * c, 16 * c + 16
        m = dict(shared)
        m["xp"] = f(inp["x_prompt"][c])
        m["xs"] = f(inp["x_sample"][b0:b1].reshape(64, 1024))
        m["memp"] = f(inp["mem_prompt"][c])
        m["sssm"] = f(inp["state_ssm"][0, b0:b1].reshape(16 * 1024, 128))
        m["scv"] = f(inp["state_ssm_conv"][0, b0:b1].reshape(48, 1536))
        m["ssc"] = f(inp["state_sconv"][0, b0:b1].reshape(32, 1024))
        m["ck"] = f(inp["cache_mem_k"][:, b0:b1].reshape(2 * 16 * 256, 1024))
        m["cv"] = f(inp["cache_mem_v"][:, b0:b1].reshape(2 * 16 * 256, 1024))
        maps.append(m)
    return maps


def assemble(results):
    n = len(results)
    g = lambda k: [np.asarray(r[k]) for r in results]
    yp = np.stack(g("yp"), 0)
    ys = np.concatenate(g("ys"), 0).reshape(16 * n, 4, 1024)
    ssmp = np.stack(g("ssmp"), 0).reshape(1, n, 16, 64, 128)
    ssms = np.concatenate(g("ssms"), 0).reshape(1, 16 * n, 16, 64, 128)
    scvp = np.stack(g("scvp"), 0).reshape(1, n, 3, 1536)
    scvs = np.concatenate(g("scvs"), 0).reshape(1, 16 * n, 3, 1536)
    sccp = np.stack(g("sccp"), 0).reshape(1, n, 2, 1024)
    sccs = np.concatenate(g("sccs"), 0).reshape(1, 16 * n, 2, 1024)
    mkp = np.stack([a.reshape(2, 256, 4, 256) for a in g("mkp")], 1)
    mvp = np.stack([a.reshape(2, 256, 4, 256) for a in g("mvp")], 1)
    vs = np.concatenate(g("vs"), 0).reshape(1, 16 * n, 4, 1024)
    return tuple(np.ascontiguousarray(a, dtype=np.float32) for a in (yp, ys, ssmp, ssms, scvp, scvs, sccp, sccs, mkp, mvp, vs))


def kernel(**inputs):
    inp = {k: np.asarray(v) for k, v in inputs.items()}
    nc = build_program()
    in_maps = make_in_maps(inp)
    res = run_bass_kernel_spmd(nc, in_maps, core_ids=list(range(NCORES)))
    return assemble(res.results)
```
